# Optimizing a Trainium2 kernel written in Bass

```python
import math
import jax, jax.numpy as jnp
from jax import lax
import numpy as np

D_MODEL = 1024
BATCH = 8
SEQ = 2048
DEPTH = 2

ATTN_WIDTH = D_MODEL // 2
SSM_WIDTH = D_MODEL // 2
HEAD_DIM = 64
N_Q_HEADS = ATTN_WIDTH // HEAD_DIM
N_KV_HEADS = 2
Q_PER_KV = N_Q_HEADS // N_KV_HEADS
KV_WIDTH = N_KV_HEADS * HEAD_DIM
WINDOW = 128
ATTN_BLOCK = 128
ROPE_THETA = 10000.0

SSM_GROUP_CH = 16
SSM_GROUPS = SSM_WIDTH // SSM_GROUP_CH
SSM_STATE = 64

IN_WIDTH = ATTN_WIDTH + 2 * KV_WIDTH + SSM_WIDTH

N_EXPERTS = 16
N_EXPERT_GROUPS = 4
EXPERTS_PER_GROUP = N_EXPERTS // N_EXPERT_GROUPS
TOP_K = 2
D_FF_EXPERT = D_MODEL // 2

EPS = 1e-6

kernel_name = "hymba_swa_s5_grouped_moe_adaln"


def rmsnorm(x, g):
    xf = x.astype(jnp.float32)
    y = xf * lax.rsqrt(jnp.mean(xf * xf, axis=-1, keepdims=True) + EPS)
    return (y * g.astype(jnp.float32)).astype(x.dtype)


def rope(x, pos):
    half = HEAD_DIM // 2
    freq = ROPE_THETA ** (-jnp.arange(half, dtype=jnp.float32) / half)
    ang = pos.astype(jnp.float32)[..., None] * freq
    cos = jnp.cos(ang)[:, :, None, :]
    sin = jnp.sin(ang)[:, :, None, :]
    xf = x.astype(jnp.float32)
    x1, x2 = xf[..., :half], xf[..., half:]
    out = jnp.concatenate([x1 * cos - x2 * sin, x2 * cos + x1 * sin], axis=-1)
    return out.astype(x.dtype)


def swa_attention(q, k, v, sink):
    bsz, s = q.shape[0], q.shape[1]
    nb = s // ATTN_BLOCK
    qb = q.reshape(bsz, nb, ATTN_BLOCK, N_KV_HEADS, Q_PER_KV, HEAD_DIM)
    kb = k.reshape(bsz, nb, ATTN_BLOCK, N_KV_HEADS, HEAD_DIM)
    vb = v.reshape(bsz, nb, ATTN_BLOCK, N_KV_HEADS, HEAD_DIM)
    pad = ((0, 0), (1, 0), (0, 0), (0, 0), (0, 0))
    kk = jnp.concatenate([jnp.pad(kb, pad)[:, :-1], kb], axis=2)
    vv = jnp.concatenate([jnp.pad(vb, pad)[:, :-1], vb], axis=2)
    scores = jnp.einsum('bnqkgd,bnskd->bnkgqs', qb, kk).astype(jnp.float32) * (HEAD_DIM ** -0.5)
    qi = jnp.arange(ATTN_BLOCK)[:, None]
    sj = jnp.arange(2 * ATTN_BLOCK)[None, :]
    diff = qi + ATTN_BLOCK - sj
    band = (diff >= 0) & (diff < WINDOW)
    real_key = (jnp.arange(nb)[:, None, None] > 0) | (sj >= ATTN_BLOCK)[None]
    mask = (band[None] & real_key)[None, :, None, None]
    scores = jnp.where(mask, scores, -jnp.inf)
    sink_l = jnp.broadcast_to(
        sink.astype(jnp.float32).reshape(N_KV_HEADS, Q_PER_KV)[None, None, :, :, None, None],
        scores.shape[:-1] + (1,))
    p = jax.nn.softmax(jnp.concatenate([scores, sink_l], axis=-1), axis=-1)[..., :-1]
    out = jnp.einsum('bnkgqs,bnskd->bnqkgd', p.astype(v.dtype), vv)
    return out.reshape(bsz, s, N_Q_HEADS * HEAD_DIM)


def _ssm_combine(e1, e2):
    a1, b1 = e1
    a2, b2 = e2
    return a1 * a2, a2 * b1 + b2


def s5_mixer(u, lam_re, lam_im, b_re, b_im, c_re, c_im, d_skip, log_dt, w_glu):
    bsz, s, _ = u.shape
    f32 = jnp.float32
    uf = u.astype(f32).reshape(bsz, s, SSM_GROUPS, SSM_GROUP_CH)
    lam = lax.complex(lam_re.astype(f32), lam_im.astype(f32))
    dt = jnp.exp(log_dt.astype(f32))[:, None]
    lam_bar = jnp.exp(lam * dt)
    b_c = lax.complex(b_re.astype(f32), b_im.astype(f32))
    b_bar = ((lam_bar - 1.0) / lam)[..., None] * b_c
    bu = jnp.einsum('blgh,gph->blgp', uf.astype(jnp.complex64), b_bar)
    a = jnp.broadcast_to(lam_bar, bu.shape)
    _, states = lax.associative_scan(_ssm_combine, (a, bu), axis=1)
    c_c = lax.complex(c_re.astype(f32), c_im.astype(f32))
    y = jnp.real(jnp.einsum('blgp,ghp->blgh', states, c_c))
    y = y + d_skip.astype(f32).reshape(SSM_GROUPS, SSM_GROUP_CH) * uf
    y = jax.nn.gelu(y.reshape(bsz, s, SSM_WIDTH))
    y = y * jax.nn.sigmoid(y @ w_glu.astype(f32))
    return y.astype(u.dtype)


def moe_ffn(h, w_router, router_bias, w_gate, w_up, w_down):
    bsz, s, d = h.shape
    t = h.reshape(-1, d)
    logits = t.astype(jnp.float32) @ w_router.astype(jnp.float32)
    probs = jax.nn.softmax(logits, axis=-1)
    sel = probs + router_bias.astype(jnp.float32)
    grp = sel.reshape(-1, N_EXPERT_GROUPS, EXPERTS_PER_GROUP)
    gscore = lax.top_k(grp, TOP_K)[0].sum(-1)
    gbest = jnp.argmax(gscore, axis=-1)
    in_group = (jnp.arange(N_EXPERTS) // EXPERTS_PER_GROUP)[None, :] == gbest[:, None]
    _, idx = lax.top_k(jnp.where(in_group, sel, -jnp.inf), TOP_K)
    gates = jnp.take_along_axis(probs, idx, axis=-1)
    gates = gates / jnp.sum(gates, axis=-1, keepdims=True)
    combine = jnp.sum(jax.nn.one_hot(idx, N_EXPERTS, dtype=jnp.float32) * gates[..., None], axis=1)
    g = jnp.einsum('td,edf->tef', t, w_gate)
    up = jnp.einsum('td,edf->tef', t, w_up)
    act = jax.nn.silu(g) * up * combine.astype(t.dtype)[..., None]
    y = jnp.einsum('tef,efd->td', act, w_down)
    return y.reshape(bsz, s, d)


def setup_inputs(seed: int = 0) -> dict:
    key = jax.random.key(seed)
    ks = jax.random.split(key, 32)
    f32 = jnp.float32
    nrm = lambda k, shp, sc: jax.random.normal(k, shp, f32) * sc
    x = nrm(ks[0], (BATCH, SEQ, D_MODEL), 1.0)
    c = nrm(ks[1], (BATCH, D_MODEL), 1.0)
    offs = jax.random.randint(ks[2], (BATCH, 1), 0, 1024, dtype=jnp.int32)
    positions = offs + jnp.arange(SEQ, dtype=jnp.int32)[None, :]
    ada_w = nrm(ks[3], (DEPTH, D_MODEL, 6 * D_MODEL), 0.5 * D_MODEL ** -0.5)
    ada_b = nrm(ks[4], (DEPTH, 6 * D_MODEL), 0.02)
    norm1_g = 1.0 + nrm(ks[5], (DEPTH, D_MODEL), 0.02)
    w_in = nrm(ks[6], (DEPTH, D_MODEL, IN_WIDTH), D_MODEL ** -0.5)
    q_norm_g = 1.0 + nrm(ks[7], (DEPTH, HEAD_DIM), 0.02)
    k_norm_g = 1.0 + nrm(ks[8], (DEPTH, HEAD_DIM), 0.02)
    attn_sink = nrm(ks[9], (DEPTH, N_Q_HEADS), 0.5)
    n = jnp.arange(SSM_STATE, dtype=f32)
    lam_re = -0.5 + nrm(ks[10], (DEPTH, SSM_GROUPS, SSM_STATE), 0.01)
    lam_im = math.pi * n + nrm(ks[11], (DEPTH, SSM_GROUPS, SSM_STATE), 0.01)
    b_sc = (2.0 * SSM_GROUP_CH) ** -0.5
    ssm_b_re = nrm(ks[12], (DEPTH, SSM_GROUPS, SSM_STATE, SSM_GROUP_CH), b_sc)
    ssm_b_im = nrm(ks[13], (DEPTH, SSM_GROUPS, SSM_STATE, SSM_GROUP_CH), b_sc)
    c_sc = (2.0 * SSM_STATE) ** -0.5
    ssm_c_re = nrm(ks[14], (DEPTH, SSM_GROUPS, SSM_GROUP_CH, SSM_STATE), c_sc)
    ssm_c_im = nrm(ks[15], (DEPTH, SSM_GROUPS, SSM_GROUP_CH, SSM_STATE), c_sc)
    ssm_d = nrm(ks[16], (DEPTH, SSM_WIDTH), 1.0)
    ssm_log_dt = jax.random.uniform(ks[17], (DEPTH, SSM_GROUPS), f32, math.log(0.001), math.log(0.1))
    w_glu = nrm(ks[18], (DEPTH, SSM_WIDTH, SSM_WIDTH), SSM_WIDTH ** -0.5)
    attn_out_g = 1.0 + nrm(ks[19], (DEPTH, ATTN_WIDTH), 0.02)
    ssm_out_g = 1.0 + nrm(ks[20], (DEPTH, SSM_WIDTH), 0.02)
    w_out = nrm(ks[21], (DEPTH, ATTN_WIDTH + SSM_WIDTH, D_MODEL), (ATTN_WIDTH + SSM_WIDTH) ** -0.5)
    norm2_g = 1.0 + nrm(ks[22], (DEPTH, D_MODEL), 0.02)
    w_router = nrm(ks[23], (D_MODEL, N_EXPERTS), D_MODEL ** -0.5)
    router_bias = nrm(ks[24], (N_EXPERTS,), 0.01)
    w_exp_gate = nrm(ks[25], (DEPTH, N_EXPERTS, D_MODEL, D_FF_EXPERT), D_MODEL ** -0.5)
    w_exp_up = nrm(ks[26], (DEPTH, N_EXPERTS, D_MODEL, D_FF_EXPERT), D_MODEL ** -0.5)
    w_exp_down = nrm(ks[27], (DEPTH, N_EXPERTS, D_FF_EXPERT, D_MODEL), D_FF_EXPERT ** -0.5)
    return {"x": x, "c": c, "positions": positions, "ada_w": ada_w, "ada_b": ada_b,
            "norm1_g": norm1_g, "w_in": w_in, "q_norm_g": q_norm_g, "k_norm_g": k_norm_g,
            "attn_sink": attn_sink, "lam_re": lam_re, "lam_im": lam_im,
            "ssm_b_re": ssm_b_re, "ssm_b_im": ssm_b_im, "ssm_c_re": ssm_c_re, "ssm_c_im": ssm_c_im,
            "ssm_d": ssm_d, "ssm_log_dt": ssm_log_dt, "w_glu": w_glu,
            "attn_out_g": attn_out_g, "ssm_out_g": ssm_out_g, "w_out": w_out, "norm2_g": norm2_g,
            "w_router": w_router, "router_bias": router_bias,
            "w_exp_gate": w_exp_gate, "w_exp_up": w_exp_up, "w_exp_down": w_exp_down}


def reference(x, c, positions, ada_w, ada_b, norm1_g, w_in, q_norm_g, k_norm_g, attn_sink,
              lam_re, lam_im, ssm_b_re, ssm_b_im, ssm_c_re, ssm_c_im, ssm_d, ssm_log_dt, w_glu,
              attn_out_g, ssm_out_g, w_out, norm2_g, w_router, router_bias,
              w_exp_gate, w_exp_up, w_exp_down):
    bsz, s, _ = x.shape
    for l in range(DEPTH):
        mod = jax.nn.silu(c) @ ada_w[l] + ada_b[l]
        sh1, sc1, g1, sh2, sc2, g2 = jnp.split(mod, 6, axis=-1)
        h = rmsnorm(x, norm1_g[l]) * (1.0 + sc1[:, None]) + sh1[:, None]
        proj = h @ w_in[l]
        q = proj[..., :ATTN_WIDTH].reshape(bsz, s, N_Q_HEADS, HEAD_DIM)
        k = proj[..., ATTN_WIDTH:ATTN_WIDTH + KV_WIDTH].reshape(bsz, s, N_KV_HEADS, HEAD_DIM)
        v = proj[..., ATTN_WIDTH + KV_WIDTH:ATTN_WIDTH + 2 * KV_WIDTH].reshape(bsz, s, N_KV_HEADS, HEAD_DIM)
        u = proj[..., ATTN_WIDTH + 2 * KV_WIDTH:]
        q = rope(rmsnorm(q, q_norm_g[l]), positions)
        k = rope(rmsnorm(k, k_norm_g[l]), positions)
        attn = swa_attention(q, k, v, attn_sink[l])
        ssm = s5_mixer(u, lam_re[l], lam_im[l], ssm_b_re[l], ssm_b_im[l], ssm_c_re[l], ssm_c_im[l],
                       ssm_d[l], ssm_log_dt[l], w_glu[l])
        merged = jnp.concatenate([rmsnorm(attn, attn_out_g[l]), rmsnorm(ssm, ssm_out_g[l])], axis=-1)
        x = x + g1[:, None] * (merged @ w_out[l])
        h2 = rmsnorm(x, norm2_g[l]) * (1.0 + sc2[:, None]) + sh2[:, None]
        x = x + g2[:, None] * moe_ffn(h2, w_router, router_bias, w_exp_gate[l], w_exp_up[l], w_exp_down[l])
    return x
```

```python
import math
import contextlib
import numpy as np
import concourse.bass as bass
import concourse.mybir as mybir
from concourse.bass_utils import run_bass_kernel_spmd

F32 = mybir.dt.float32
BF16 = mybir.dt.bfloat16
I32 = mybir.dt.int32
AF = mybir.ActivationFunctionType
ALU = mybir.AluOpType
AX = mybir.AxisListType

DEPTH = 2
S = 2048
D = 1024
NT = 16
EPS = 1e-6
NEG = -30000.0
TWO_PI = 2.0 * math.pi
import os
SELF_SYNC = set(os.environ.get("KSELF", "act,dve,pool").split(","))


class KB:
    def __init__(self, nc, es):
        self.nc = nc
        self.es = es
        self.E = {"pe": nc.tensor, "act": nc.scalar, "dve": nc.vector, "pool": nc.gpsimd, "sp": nc.sync}
        self.sems = {}
        self.val = {}
        self.seen = {e: {} for e in self.E}
        self.lastw = {}
        self.readers = {}
        for e in ("pe", "act", "dve", "pool"):
            self._sem(e)

    def _sem(self, name):
        if name not in self.sems:
            self.sems[name] = self.es.enter_context(self.nc.semaphore("s_" + name.replace(":", "_")))
            self.val[name] = 0
        return self.sems[name]

    def _deps(self, R, W):
        deps = {}

        def need(d):
            if d is None:
                return
            s, v = d
            if v > deps.get(s, 0):
                deps[s] = v

        for k in R:
            need(self.lastw.get(k))
        for k in W:
            need(self.lastw.get(k))
            for r in self.readers.get(k, ()):
                need(r)
        return deps

    def _wait(self, eng, deps):
        E = self.E[eng]
        for s, v in deps.items():
            if s == eng and (eng == "pe" or eng not in SELF_SYNC):
                continue
            if self.seen[eng].get(s, 0) >= v:
                continue
            E.wait_ge(self.sems[s], v)
            self.seen[eng][s] = v

    def _record(self, tok, R, W):
        for k in R:
            self.readers.setdefault(k, []).append(tok)
        for k in W:
            self.lastw[k] = tok
            self.readers[k] = []

    def op(self, eng, fn, R=(), W=(), sig=True):
        self._wait(eng, self._deps(R, W))
        inst = fn(self.E[eng])
        if sig:
            self.val[eng] += 1
            inst.then_inc(self.sems[eng], 1)
            tok = (eng, self.val[eng])
        else:
            tok = (eng, self.val[eng] + 1)
        self._record(tok, R, W)
        return inst

    def dma(self, q, out, in_, R=(), W=(), dkey=None, **kw):
        name = "d:" + dkey
        self._sem(name)
        self._wait(q, self._deps(R, W))
        inst = self.E[q].dma_start(out=out, in_=in_, **kw)
        self.val[name] += 16
        inst.then_inc(self.sems[name], 16)
        self._record((name, self.val[name]), R, W)
        return inst

    def barrier(self):
        for eng in self.E:
            deps = {s: v for s, v in self.val.items() if v > 0}
            self._wait(eng, deps)
        self.lastw.clear()
        self.readers.clear()

    def final_wait(self, eng="sp"):
        deps = {s: v for s, v in self.val.items() if v > 0 and s.startswith("d:")}
        self._wait(eng, deps)


def run_rr(gens, width, lag=0):
    it = iter(gens)
    active = []
    steps_newest = 10 ** 9
    done = False
    while True:
        if not done and len(active) < width and steps_newest >= lag:
            try:
                active.append(next(it))
                steps_newest = 0
            except StopIteration:
                done = True
        if not active:
            if done:
                break
            steps_newest = 10 ** 9
            continue
        for g in list(active):
            try:
                next(g)
            except StopIteration:
                active.remove(g)
        steps_newest += 1


def build(depth=DEPTH, debug=None):
    nc = bass.Bass("TRN2", target_bir_lowering=False)

    def din(name, shape, dt=F32):
        return nc.dram_tensor(name, list(shape), dt, kind="ExternalInput").ap()

    x_d = din("x", [S, D])
    c_d = din("c", [8, 128])
    pos_d = din("positions", [S], I32)
    ada_w = din("ada_w", [DEPTH, D, 6 * D])
    ada_b = din("ada_b", [DEPTH, 6 * D])
    norm1_g = din("norm1_g", [DEPTH, D])
    w_in = din("w_in", [DEPTH, D, 1280])
    q_norm_g = din("q_norm_g", [DEPTH, 64])
    k_norm_g = din("k_norm_g", [DEPTH, 64])
    attn_sink = din("attn_sink", [DEPTH, 8])
    lam_re = din("lam_re", [DEPTH, 32, 64])
    lam_im = din("lam_im", [DEPTH, 32, 64])
    ssm_b_re = din("ssm_b_re", [DEPTH, 32, 64, 16])
    ssm_b_im = din("ssm_b_im", [DEPTH, 32, 64, 16])
    ssm_c_re = din("ssm_c_re", [DEPTH, 32, 16, 64])
    ssm_c_im = din("ssm_c_im", [DEPTH, 32, 16, 64])
    ssm_d = din("ssm_d", [DEPTH, 512])
    ssm_log_dt = din("ssm_log_dt", [DEPTH, 32])
    w_glu = din("w_glu", [DEPTH, 512, 512])
    attn_out_g = din("attn_out_g", [DEPTH, 512])
    ssm_out_g = din("ssm_out_g", [DEPTH, 512])
    w_out = din("w_out", [DEPTH, D, D])
    norm2_g = din("norm2_g", [DEPTH, D])
    w_router = din("w_router", [D, 16])
    router_bias = din("router_bias", [16])
    w_exp_gate = din("w_exp_gate", [DEPTH, 16, D, 512])
    w_exp_up = din("w_exp_up", [DEPTH, 16, D, 512])
    w_exp_down = din("w_exp_down", [DEPTH, 16, 512, D])
    y_d = nc.dram_tensor("y", [S, D], F32, kind="ExternalOutput").ap()
    dbg_d = None
    if debug is not None:
        dbg_d = nc.dram_tensor("dbg", [128, debug[1]], debug[2], kind="ExternalOutput").ap()

    with contextlib.ExitStack() as es:
        kb = KB(nc, es)
        op = kb.op

        def sb(name, shape, dt):
            return es.enter_context(nc.sbuf_tensor(name, list(shape), dt))

        X = sb("X", [128, NT, D], F32)
        FT = sb("FT", [128, 8 * S], BF16)
        AT = sb("AT", [128, 4 * S], BF16)
        BIG = sb("BIG", [128, 12288], F32)
        MOD = sb("MOD", [128, 6, D], BF16)
        SM = sb("SM", [128, 2560], F32)
        ident_f = sb("ident_f", [128, 128], F32)
        ident_b = sb("ident_b", [128, 128], BF16)
        colI = sb("colI", [128, 128], F32)
        rowI = sb("rowI", [128, 128], F32)
        MBp = sb("MBp", [128, 4, 128], BF16)
        MBc = sb("MBc", [128, 4, 128], BF16)
        COS = sb("COS", [128, NT, 32], F32)
        SIN = sb("SIN", [128, NT, 32], F32)
        SCrep = sb("SCrep", [128, 8, 128], BF16)
        TMP4 = sb("TMP4", [128, 1024], F32)
        TMP4b = sb("TMP4b", [128, 1024], F32)
        SS = sb("SS", [128, 64], F32)
        NHALF = sb("NHALF", [128, 64], F32)
        GQK = sb("GQK", [128, 10, 64], BF16)
        ESINK = sb("ESINK", [128, 8], F32)
        GROW = sb("GROW", [128, 8], F32)
        MASKM = sb("MASKM", [128, 256], BF16)
        DIAG0 = sb("DIAG0", [128, 256], BF16)
        REP16 = sb("REP16", [16, 128], F32)
        SGN = sb("SGN", [128, 2], F32)
        DCOL = sb("DCOL", [128, 32], F32)
        LAB = sb("LAB", [128, 2, 2, 2, 16], F32)
        WR = sb("WR", [128, 8, 16], BF16)
        RB = sb("RB", [128, 16], F32)
        SELE = sb("SELE", [16, 2, 128], BF16)

        PSALL = es.enter_context(nc.psum_tensor("psall", [128, 4096], F32))
        ps = [PSALL[:, 512 * i:512 * (i + 1)] for i in range(8)]

        def psb(i):
            return ps[i].bitcast(BF16)

        def big_bf(w0, w1):
            return BIG[:, w0:w1].bitcast(BF16)

        FT3 = FT[:].rearrange("p (k t) -> p k t", k=8)
        AT3 = AT[:].rearrange("p (k t) -> p k t", k=4)

        op("pool", lambda e: e.iota(colI[:], [[1, 128]], base=0, channel_multiplier=0,
                                    allow_small_or_imprecise_dtypes=True), W=["colI"])
        op("pool", lambda e: e.iota(rowI[:], [[0, 128]], base=0, channel_multiplier=1,
                                    allow_small_or_imprecise_dtypes=True), W=["rowI"])
        op("pool", lambda e: e.memset(NHALF[:], -0.5), W=["NHALF"])
        op("dve", lambda e: e.tensor_tensor(ident_f[:], colI[:], rowI[:], op=ALU.is_equal),
           R=["colI", "rowI"], W=["ident_f"])
        op("dve", lambda e: e.tensor_copy(ident_b[:], ident_f[:]), R=["ident_f"], W=["ident_b"])
        op("dve", lambda e: e.tensor_tensor(TMP4[:, 0:128], colI[:], rowI[:], op=ALU.is_ge),
           R=["colI", "rowI"], W=["TMP4"])
        op("dve", lambda e: e.tensor_scalar(MBp[:], TMP4[:, 0:128].unsqueeze(1).to_broadcast([128, 4, 128]),
                                            NEG, None, op0=ALU.mult), R=["TMP4"], W=["MBp"])
        op("dve", lambda e: e.tensor_tensor(TMP4[:, 128:256], colI[:], rowI[:], op=ALU.is_lt),
           R=["colI", "rowI"], W=["TMP4"])
        op("dve", lambda e: e.tensor_scalar(MBc[:], TMP4[:, 128:256].unsqueeze(1).to_broadcast([128, 4, 128]),
                                            NEG, None, op0=ALU.mult), R=["TMP4"], W=["MBc"])


        def _ssm_consts():
            PI_ = SM[:, 340:341].bitcast(I32)
            JJF = SM[:, 341:342]
            HPF = SM[:, 342:343]
            TI = SM[:, 344:345].bitcast(I32)
            IIF = TMP4[:, 0:256]
            HF = TMP4[:, 256:512]
            DLF = TMP4[:, 512:768]
            E1 = TMP4b[:, 0:256]
            E2 = TMP4b[:, 256:512]
            op("pool", lambda e: e.iota(PI_, [[0, 1]], base=0, channel_multiplier=1), W=["c_pi"])
            op("dve", lambda e: e.tensor_single_scalar(TI, PI_, 4, op=ALU.arith_shift_right), R=["c_pi"], W=["c_ti"])
            op("dve", lambda e: e.tensor_copy(JJF, TI), R=["c_ti"], W=["c_jj"])
            op("dve", lambda e: e.tensor_single_scalar(TI, PI_, 15, op=ALU.bitwise_and), R=["c_pi", "c_jj"], W=["c_ti"])
            op("dve", lambda e: e.tensor_copy(HPF, TI), R=["c_ti"], W=["c_hp"])
            op("pool", lambda e: e.iota(IIF.rearrange("p (a b c) -> p a b c", a=2, b=8), [[0, 2], [1, 8], [0, 16]],
                                        base=0, channel_multiplier=0, allow_small_or_imprecise_dtypes=True),
               W=["TMP4"])
            op("pool", lambda e: e.iota(HF.rearrange("p (a b c) -> p a b c", a=2, b=8), [[0, 2], [0, 8], [1, 16]],
                                        base=0, channel_multiplier=0, allow_small_or_imprecise_dtypes=True),
               W=["TMP4"])
            op("pool", lambda e: e.iota(DLF.rearrange("p (a b c) -> p a b c", a=2, b=8), [[1, 2], [0, 8], [0, 16]],
                                        base=0, channel_multiplier=0, allow_small_or_imprecise_dtypes=True),
               W=["TMP4"])
            op("dve", lambda e: e.tensor_scalar(E1, IIF, JJF, None, op0=ALU.is_ge), R=["TMP4", "c_jj"], W=["TMP4b"])
            op("dve", lambda e: e.tensor_tensor(MASKM[:], E1, DLF, op=ALU.max), R=["TMP4b", "TMP4"], W=["MASKM"])
            op("dve", lambda e: e.tensor_scalar(E1, IIF, JJF, None, op0=ALU.is_equal), R=["TMP4", "c_jj", "MASKM"],
               W=["TMP4b"])
            op("dve", lambda e: e.tensor_scalar(E2, HF, HPF, None, op0=ALU.is_equal), R=["TMP4", "c_hp"], W=["TMP4b"])
            op("dve", lambda e: e.tensor_tensor(E1, E1, E2, op=ALU.mult), R=["TMP4b"], W=["TMP4b"])
            op("dve", lambda e: e.tensor_scalar(E2, DLF, -1.0, 1.0, op0=ALU.mult, op1=ALU.add), R=["TMP4"], W=["TMP4b"])
            op("dve", lambda e: e.tensor_tensor(DIAG0[:], E1, E2, op=ALU.mult), R=["TMP4b"], W=["DIAG0"])
            op("dve", lambda e: e.tensor_copy(REP16[:].rearrange("k (j h) -> k j h", j=8),
                                              ident_f[0:16, 0:16].unsqueeze(1).to_broadcast([16, 8, 16])),
               R=["ident_f"], W=["REP16"])
            op("pool", lambda e: e.memset(SGN[0:64, 0:1], 1.0), W=["SGN"])
            op("pool", lambda e: e.memset(SGN[64:128, 0:1], -1.0), W=["SGN"])
            op("pool", lambda e: e.memset(SGN[0:64, 1:2], -1.0), W=["SGN"])
            op("pool", lambda e: e.memset(SGN[64:128, 1:2], 1.0), W=["SGN"])

        _ssm_consts()
        kb.dma("pool", WR[:], w_router.rearrange("(k p) e -> p k e", p=128), W=["WR"], dkey="wr")
        kb.dma("sp", RB[:], router_bias.rearrange("(o e) -> o e", o=1).partition_broadcast(128).rearrange("p o e -> p (o e)"),
               W=["RB"], dkey="rb")

        x_v = x_d.rearrange("(c i) d -> c i d", i=NT)
        for q4 in range(4):
            kb.dma("sp", X[:, 4 * q4:4 * q4 + 4, :], x_v[:, 4 * q4:4 * q4 + 4, :],
                   W=[("X", i) for i in range(4 * q4, 4 * q4 + 4)], dkey=f"x{q4}")

        POSI = SM[:, 0:16].bitcast(I32)
        with nc.allow_non_contiguous_dma(reason="tiny position load"):
            kb.dma("sp", POSI, pos_d.rearrange("(n p) -> p n", p=128), W=["POSI"], dkey="pos")
        POSF = SM[:, 16:32]
        FREQ = SM[:, 32:64]
        ANG = TMP4[:, 0:512].rearrange("p (n k) -> p n k", k=32)
        ANG2 = TMP4[:, 512:1024].rearrange("p (n k) -> p n k", k=32)
        KI = TMP4b[:, 0:512].bitcast(I32).rearrange("p (n k) -> p n k", k=32)
        KF = TMP4b[:, 512:1024].rearrange("p (n k) -> p n k", k=32)
        op("dve", lambda e: e.tensor_copy(POSF, POSI), R=["POSI"], W=["POSF"])
        op("pool", lambda e: e.iota(FREQ, [[1, 32]], base=0, channel_multiplier=0,
                                    allow_small_or_imprecise_dtypes=True), W=["FREQ"])
        op("act", lambda e: e.activation(out=FREQ, in_=FREQ, func=AF.Exp, scale=-math.log(10000.0) / 32.0),
           R=["FREQ"], W=["FREQ"])
        op("dve", lambda e: e.tensor_tensor(ANG, POSF.unsqueeze(2).to_broadcast([128, NT, 32]),
                                            FREQ.unsqueeze(1).to_broadcast([128, NT, 32]), op=ALU.mult),
           R=["POSF", "FREQ"], W=["TMP4"])

        def sin_table(dst, src_ang, shift, keyname):
            op("dve", lambda e: e.tensor_scalar(ANG2, src_ang, shift, None, op0=ALU.add), R=["TMP4"], W=["TMP4"])
            op("dve", lambda e: e.tensor_scalar(KF, ANG2, 1.0 / TWO_PI, None, op0=ALU.mult), R=["TMP4"], W=["TMP4b"])
            op("dve", lambda e: e.tensor_copy(KI, KF), R=["TMP4b"], W=["TMP4b"])
            op("dve", lambda e: e.tensor_copy(KF, KI), R=["TMP4b"], W=["TMP4b"])
            op("dve", lambda e: e.scalar_tensor_tensor(ANG2, KF, -TWO_PI, ANG2, op0=ALU.mult, op1=ALU.add),
               R=["TMP4b", "TMP4"], W=["TMP4"])
            op("dve", lambda e: e.tensor_scalar(ANG2, ANG2, -3.1415925, 3.1415925, op0=ALU.max, op1=ALU.min),
               R=["TMP4"], W=["TMP4"])
            op("act", lambda e: e.activation(out=dst, in_=ANG2, func=AF.Sin), R=["TMP4"], W=[keyname])

        sin_table(SIN[:], ANG, 0.0, "SIN")
        sin_table(COS[:], ANG, math.pi / 2.0, "COS")

        C8 = SM[0:8, 64:192]
        kb.dma("sp", C8, c_d, W=["C8"], dkey="c8")
        op("pe", lambda e: e.matmul(ps[0][:, 0:8], C8, ident_f[0:8, 0:8], start=True, stop=True),
           R=["C8", "ident_f"], W=[("ps", 0)])
        SCT = SM[:, 192:200]
        op("act", lambda e: e.activation(out=SCT, in_=ps[0][:, 0:8], func=AF.Silu), R=[("ps", 0)], W=["SCT"])
        op("dve", lambda e: e.tensor_copy(SCrep[:], SCT.unsqueeze(2).to_broadcast([128, 8, 128])),
           R=["SCT"], W=["SCrep"])

        def ring_slot(s):
            return big_bf(2048 * s, 2048 * (s + 1))

        def adaln(l):
            for blk in range(6):
                if blk in (1, 4):
                    ng = (norm1_g if blk == 1 else norm2_g)
                    kb.dma("sp", TMP4b[:], ng[l:l + 1, :].partition_broadcast(128).rearrange("p o n -> p (o n)"),
                           W=["TMP4b"], dkey="ngbc")
                BIASB = TMP4[:] if blk % 2 == 0 else SM[:, 1024:2048]
                bkey = "TMP4" if blk % 2 == 0 else "SMbias"
                kb.dma("sp", BIASB, ada_b[l:l + 1, blk * D:(blk + 1) * D].partition_broadcast(128)
                       .rearrange("p o n -> p (o n)"), W=[bkey], dkey="adb%d" % (blk % 2))
                for half in range(2):
                    s = (blk * 2 + half) % 4
                    slot = ring_slot(s).rearrange("p (k n) -> p k n", k=8)
                    col0 = blk * D + half * 512
                    kb.dma("pool", slot, ada_w[l, :, col0:col0 + 512].rearrange("(k p) n -> p k n", p=128),
                           W=[("ring", s)], dkey=f"ring{s}")
                    pb = 2 + (s % 2)
                    for k in range(8):
                        op("pe", lambda e, k=k: e.matmul(ps[pb][:], SCrep[:, k, :], slot[:, k, :],
                                                         start=(k == 0), stop=(k == 7)),
                           R=["SCrep", ("ring", s)], W=[("ps", pb)], sig=(k == 7))
                    dst = MOD[:, blk, half * 512:(half + 1) * 512]
                    bias = BIASB[:, half * 512:(half + 1) * 512]
                    if blk in (1, 4):
                        tmp = SM[:, 512:1024]
                        op("dve", lambda e: e.tensor_tensor(tmp, ps[pb][:], bias, op=ALU.add),
                           R=[("ps", pb), bkey], W=["SMtmp"])
                        op("dve", lambda e: e.scalar_tensor_tensor(dst, tmp, 1.0, TMP4b[:, half * 512:(half + 1) * 512],
                                                                   op0=ALU.add, op1=ALU.mult),
                           R=["SMtmp", "TMP4b"], W=[("MOD", blk)])
                    else:
                        op("dve", lambda e: e.tensor_tensor(dst, ps[pb][:], bias, op=ALU.add),
                           R=[("ps", pb), bkey], W=[("MOD", blk)])

        def norm_to_FT(l, which, pos_of_tile, okey="FT", pbanks=(0, 1)):
            a_i, b_i = (1, 0) if which == 0 else (4, 3)
            junk = SM[:, 2048:2560].bitcast(BF16)
            for i in range(NT):
                op("act", lambda e, i=i: e.activation(out=junk, in_=X[:, i, :], func=AF.Square,
                                                      accum_out=SS[:, i:i + 1]),
                   R=[("X", i)], W=["junk", ("SS", i)])
            op("dve", lambda e: e.tensor_scalar(SS[:, 16:32], SS[:, 0:16], 1.0 / D, EPS, op0=ALU.mult, op1=ALU.add),
               R=[("SS", i) for i in range(NT)], W=["SSb"])
            op("pool", lambda e: e.tensor_tensor(SS[:, 32:48], SS[:, 16:32], NHALF[:, 0:16], op=ALU.pow),
               R=["SSb", "NHALF"], W=["RSTD"])
            def tile(i):
                xs = i % 2
                t32 = TMP4[:] if xs == 0 else TMP4b[:]
                tkey = "TMP4" if xs == 0 else "TMP4b"
                xn = SM[:, 1024 + 512 * xs:1024 + 512 * (xs + 1)].bitcast(BF16)
                op("dve", lambda e, i=i: e.scalar_tensor_tensor(t32, X[:, i, :], SS[:, 32 + i:33 + i], MOD[:, a_i, :],
                                                                op0=ALU.mult, op1=ALU.mult),
                   R=[("X", i), "RSTD", ("MOD", a_i)], W=[tkey])
                yield
                op("dve", lambda e: e.tensor_tensor(xn, t32, MOD[:, b_i, :], op=ALU.add),
                   R=[tkey, ("MOD", b_i)], W=[("xn", xs)])
                yield
                pb = pbanks[xs]
                for k in range(8):
                    op("pe", lambda e, k=k: e.transpose(psb(pb)[:, k * 128:(k + 1) * 128],
                                                        xn[:, k * 128:(k + 1) * 128], ident_b[:]),
                       R=[("xn", xs), "ident_b"], W=[("ps", pb)], sig=(k == 7))
                    yield
                dst = pos_of_tile(i)
                op("act", lambda e: e.activation(out=dst, in_=psb(pb).rearrange("p (k c) -> p k c", k=8),
                                                 func=AF.Copy),
                   R=[("ps", pb)], W=[(okey, i)])
                yield

            run_rr([tile(i) for i in range(NT)], 2, 5)

        WIN3 = big_bf(0, 5120).rearrange("p (k n) -> p k n", k=8)
        UCM = big_bf(8192, 12288).rearrange("p (j f) -> p j f", j=16)
        UCMg = big_bf(8192, 12288).rearrange("p (g j h) -> p g j h", g=32, j=16)

        def layer_loads(l):
            kb.dma("pool", WIN3, w_in[l].rearrange("(k p) n -> p k n", p=128), W=["WIN"], dkey="win")
            QG = SM[:, 200:264]
            KG = SM[:, 264:328]
            SK = SM[:, 328:336]
            kb.dma("sp", QG, q_norm_g[l:l + 1, :].partition_broadcast(128).rearrange("p o n -> p (o n)"),
                   W=["QG"], dkey="qg")
            kb.dma("sp", KG, k_norm_g[l:l + 1, :].partition_broadcast(128).rearrange("p o n -> p (o n)"),
                   W=["KG"], dkey="kg")
            kb.dma("sp", SK, attn_sink[l:l + 1, :].partition_broadcast(128).rearrange("p o n -> p (o n)"),
                   W=["SK"], dkey="sk")
            op("dve", lambda e: e.tensor_copy(GQK[:, 0:8, :], QG.unsqueeze(1).to_broadcast([128, 8, 64])),
               R=["QG"], W=["GQK"])
            op("dve", lambda e: e.tensor_copy(GQK[:, 8:10, :], KG.unsqueeze(1).to_broadcast([128, 2, 64])),
               R=["KG"], W=["GQK"])
            op("act", lambda e: e.activation(out=ESINK[:], in_=SK, func=AF.Exp), R=["SK"], W=["ESINK"])
            with nc.allow_non_contiguous_dma(reason="tiny gain vectors"):
                kb.dma("sp", GROW[:, 0:4], attn_out_g[l].rearrange("(k p) -> p k", p=128), W=["GROW"], dkey="grow")
                kb.dma("sp", GROW[:, 4:8], ssm_out_g[l].rearrange("(k p) -> p k", p=128), W=["GROW"], dkey="grow")

        def u_proj(l):
            for j in range(NT):
                pb = 2 + (j % 2)
                for k in range(8):
                    op("pe", lambda e, k=k: e.matmul(ps[pb][:], FT3[:, k, j::16], WIN3[:, k, 768:1280],
                                                     start=(k == 0), stop=(k == 7)),
                       R=[("FT", j), "WIN"], W=[("ps", pb)], sig=(k == 7))
                eng = "act" if j % 2 == 0 else "dve"
                if eng == "act":
                    op("act", lambda e: e.activation(out=UCMg[:, :, j, :],
                                                     in_=ps[pb][:].rearrange("p (g h) -> p g h", g=32), func=AF.Copy),
                       R=[("ps", pb)], W=[("UCM", j)])
                else:
                    op("dve", lambda e: e.tensor_copy(UCMg[:, :, j, :], ps[pb][:].rearrange("p (g h) -> p g h", g=32)),
                       R=[("ps", pb)], W=[("UCM", j)])

        def attention(l):
            KT = big_bf(5120, 7168)[0:64, :].rearrange("p (n h t) -> p n h t", n=16, h=2)
            PT = [big_bf(7168 + 256 * j, 7168 + 256 * (j + 1)) for j in range(4)]
            QK = SM[:, 0:640]
            T1 = SM[:, 640:1280]
            T2 = SM[:, 1280:1920]
            QKN = SM[:, 1920:2240].bitcast(BF16)
            ST10 = SM[:, 2240:2250]
            RS10 = SM[:, 2250:2260]
            DEN = SM[:, 2260:2268]
            RDEN = SM[:, 2268:2276]
            ASS = SM[:, 2276:2277]
            ARS = SM[:, 2278:2279]
            QT = [TMP4[0:64, 512 * j:512 * (j + 1)].bitcast(BF16) for j in range(2)]
            ATT = TMP4b[:, 0:512]
            AN = TMP4b[:, 512:768].bitcast(BF16)
            VA = [TMP4b[:, 768 + 66 * j:768 + 66 * (j + 1)].bitcast(BF16).rearrange("p (h d) -> p h d", h=2)
                  for j in range(3)]
            ALLFT = [("FT", i) for i in range(NT)]
            for j in range(3):
                op("pool", lambda e, j=j: e.memset(VA[j][:, :, 64:66], 1.0), W=[("VA1", j)])
            T1v = T1.rearrange("p (h t d) -> p h t d", h=10, t=2)
            T2v = T2.rearrange("p (h t d) -> p h t d", h=10, t=2)
            def front(n):
                qs = n % 2
                for k in range(8):
                    op("pe", lambda e, k=k: e.matmul(ps[4][:], FT3[:, k, 128 * n:128 * n + 128], WIN3[:, k, 0:512],
                                                     start=(k == 0), stop=(k == 7)),
                       R=ALLFT + ["WIN"], W=[("ps", 4)], sig=(k == 7))
                    yield
                for k in range(8):
                    op("pe", lambda e, k=k: e.matmul(ps[5][:, 0:256], FT3[:, k, 128 * n:128 * n + 128],
                                                     WIN3[:, k, 512:768], start=(k == 0), stop=(k == 7)),
                       R=ALLFT + ["WIN"], W=[("ps", 5)], sig=(k == 7))
                    yield
                op("act", lambda e: e.activation(out=QK[:, 0:512], in_=ps[4][:], func=AF.Copy),
                   R=[("ps", 4)], W=["QK"])
                yield
                op("act", lambda e: e.activation(out=QK[:, 512:640], in_=ps[5][:, 0:128], func=AF.Copy),
                   R=[("ps", 5)], W=["QK"])
                yield
                op("act", lambda e: e.activation(out=VA[n % 3][:, :, 0:64],
                                                 in_=ps[5][:, 128:256].rearrange("p (h d) -> p h d", h=2),
                                                 func=AF.Copy),
                   R=[("ps", 5)], W=[("VA", n % 3)])
                yield
                junkq = T2[:, 0:32].bitcast(BF16)
                for h in range(10):
                    op("act", lambda e, h=h: e.activation(out=junkq, in_=QK[:, h * 64:(h + 1) * 64], func=AF.Square,
                                                          accum_out=ST10[:, h:h + 1]), R=["QK"], W=["T2", ("ST10", h)])
                    yield
                op("pool", lambda e: e.tensor_scalar(ST10, ST10, 1.0 / 64.0, EPS, op0=ALU.mult, op1=ALU.add),
                   R=[("ST10", h) for h in range(10)], W=["ST10"])
                yield
                op("pool", lambda e: e.tensor_tensor(RS10, ST10, NHALF[:, 0:10], op=ALU.pow),
                   R=["ST10", "NHALF"], W=["RS10"])
                yield
                op("pool", lambda e: e.tensor_tensor(T1, QK, GQK[:].rearrange("p h d -> p (h d)"), op=ALU.mult),
                   R=["QK", "GQK"], W=["T1"])
                yield
                sin_b = SIN[:, n, :].unsqueeze(1).to_broadcast([128, 10, 32])
                cos_b = COS[:, n, :].unsqueeze(1).unsqueeze(1).to_broadcast([128, 10, 2, 32])
                op("pool", lambda e: e.tensor_tensor(T2v[:, :, 0, :], T1v[:, :, 1, :], sin_b, op=ALU.mult),
                   R=["T1", "SIN"], W=["T2"])
                yield
                op("pool", lambda e: e.tensor_tensor(T2v[:, :, 1, :], T1v[:, :, 0, :], sin_b, op=ALU.mult),
                   R=["T1", "SIN"], W=["T2"])
                yield
                op("pool", lambda e: e.tensor_tensor(T1v, T1v, cos_b, op=ALU.mult), R=["T1", "COS", "T2"], W=["T1"])
                yield
                op("pool", lambda e: e.tensor_tensor(T1v[:, :, 0, :], T1v[:, :, 0, :], T2v[:, :, 0, :],
                                                     op=ALU.subtract), R=["T1", "T2"], W=["T1"])
                yield
                op("pool", lambda e: e.tensor_tensor(T1v[:, :, 1, :], T1v[:, :, 1, :], T2v[:, :, 1, :],
                                                     op=ALU.add), R=["T1", "T2"], W=["T1"])
                yield
                op("pool", lambda e: e.tensor_tensor(QKN.rearrange("p (h d) -> p h d", h=10),
                                                     T1.rearrange("p (h d) -> p h d", h=10),
                                                     RS10.unsqueeze(2).to_broadcast([128, 10, 64]), op=ALU.mult),
                   R=["T1", "RS10"], W=["QKN"])
                yield
                for h in range(8):
                    op("pe", lambda e, h=h: e.transpose(psb(0)[0:64, h * 128:(h + 1) * 128],
                                                        QKN[:, h * 64:(h + 1) * 64], ident_b[:]),
                       R=["QKN", "ident_b"], W=[("ps", 0)], sig=(h == 7))
                    yield
                for h in range(2):
                    op("pe", lambda e, h=h: e.transpose(psb(0)[64:128, h * 128:(h + 1) * 128],
                                                        QKN[:, 512 + h * 64:512 + (h + 1) * 64], ident_b[:]),
                       R=["QKN", "ident_b"], W=[("ps", 0)], sig=(h == 1))
                    yield
                op("act", lambda e: e.activation(out=QT[qs], in_=psb(0)[0:64, :], func=AF.Copy),
                   R=[("ps", 0)], W=[("QT", qs)])
                yield
                op("act", lambda e: e.activation(out=KT[:, n, :, :],
                                                 in_=psb(0)[64:128, 0:256].rearrange("p (h t) -> p h t", h=2),
                                                 func=AF.Copy),
                   R=[("ps", 0)], W=[("KT", n)])
                yield

            def back(n):
                qs = n % 2
                QT3 = QT[qs].rearrange("p (h t) -> p h t", h=8)
                halves = ([(n - 1, MBp)] if n > 0 else []) + [(n, MBc)]
                for kvh in range(2):
                    for hi, (nb, MB) in enumerate(halves):
                        cidx = 2 * kvh + hi
                        sbk = 2 + (cidx % 2)
                        pj = cidx % 4
                        op("pe", lambda e: e.matmul(ps[sbk][:], KT[:, nb, kvh, :],
                                                    QT3[:, 4 * kvh:4 * kvh + 4, :], start=True, stop=False),
                           R=[("KT", nb), ("QT", qs)], W=[("ps", sbk)], sig=False)
                        yield
                        op("pe", lambda e: e.matmul(ps[sbk][:], ident_b[:], MB[:].rearrange("p h q -> p (h q)"),
                                                    start=False, stop=True),
                           R=["ident_b", "MB"], W=[("ps", sbk)])
                        yield
                        op("act", lambda e: e.activation(out=PT[pj], in_=ps[sbk][:], func=AF.Exp, scale=0.125),
                           R=[("ps", sbk)], W=[("PT", pj)])
                        yield
                        for h in range(4):
                            op("pe", lambda e, h=h: e.matmul(ps[6 + kvh][:, h * 65:h * 65 + 65],
                                                             PT[pj][:, h * 128:(h + 1) * 128],
                                                             VA[nb % 3][:, kvh, 0:65],
                                                             start=(hi == 0 and h == 0), stop=(hi == len(halves) - 1 and h == 3),
                                                             skip_group_check=True),
                               R=[("PT", pj), ("VA", nb % 3), ("VA1", nb % 3)], W=[("ps", 6 + kvh)], sig=(h == 3))
                            yield
                pv2 = PSALL[:, 3072:4096].rearrange("p (b x) -> p b x", b=2)[:, :, 0:260].rearrange(
                    "p b (h d) -> p b h d", h=4)
                k67 = [("ps", 6), ("ps", 7)]
                op("dve", lambda e: e.tensor_tensor(DEN.rearrange("p (b h o) -> p b h o", b=2, o=1), pv2[:, :, :, 64:65],
                                                    ESINK[:].rearrange("p (b h o) -> p b h o", b=2, o=1), op=ALU.add),
                   R=k67 + ["ESINK"], W=["DEN"])
                yield
                op("dve", lambda e: e.reciprocal(RDEN, DEN), R=["DEN"], W=["RDEN"])
                yield
                op("dve", lambda e: e.tensor_tensor(
                    ATT.rearrange("p (b h d) -> p b h d", b=2, h=4), pv2[:, :, :, 0:64],
                    RDEN.rearrange("p (b h) -> p b h", b=2).unsqueeze(3).to_broadcast([128, 2, 4, 64]), op=ALU.mult),
                   R=k67 + ["RDEN"], W=["ATT"])
                yield
                op("dve", lambda e: e.scalar_tensor_tensor(AN, ATT, 1.0, ATT, op0=ALU.mult, op1=ALU.mult, accum_out=ASS),
                   R=["ATT"], W=["AN", "ASS"])
                yield
                op("dve", lambda e: e.tensor_scalar(ASS, ASS, 1.0 / 512.0, EPS, op0=ALU.mult, op1=ALU.add),
                   R=["ASS"], W=["ASS"])
                yield
                op("pool", lambda e: e.tensor_tensor(ARS, ASS, NHALF[:, 0:1], op=ALU.pow),
                   R=["ASS", "NHALF"], W=["ARS"])
                yield
                op("dve", lambda e: e.tensor_scalar(AN, ATT, ARS, None, op0=ALU.mult), R=["ATT", "ARS"], W=["AN"])
                yield
                for k in range(4):
                    op("pe", lambda e, k=k: e.transpose(psb(1)[:, 512 + k * 128:512 + (k + 1) * 128],
                                                        AN[:, k * 128:(k + 1) * 128], ident_b[:]),
                       R=["AN", "ident_b"], W=[("ps", 1)], sig=(k == 3))
                    yield
                op("act", lambda e: e.activation(out=AT3[:, :, 128 * n:128 * n + 128],
                                                 in_=psb(1)[:, 512:1024].rearrange("p (k t) -> p k t", k=4),
                                                 func=AF.Copy),
                   R=[("ps", 1)], W=[("AT", n)])
                yield

            run_rr([front(0)], 1)
            for n in range(NT):
                gs = [back(n)] + ([front(n + 1)] if n + 1 < NT else [])
                run_rr(gs, 2)

        FTf = FT[:].bitcast(F32)
        BZCL = big_bf(0, 4096).rearrange("p (g a m) -> p g a m", g=32, a=2)
        MW = big_bf(4096, 8192).rearrange("p (g a m) -> p g a m", g=32, a=2)
        UT = FT[:, 0:8192].rearrange("p (g a c) -> p g a c", g=32, a=2)
        ZS = FTf[:, 4096:8192].rearrange("p (r g c) -> p r g c", r=2, g=16)
        XSB = SM[:, 0:2048].bitcast(BF16).rearrange("p (g c) -> p g c", g=32)
        S34 = TMP4b[:].rearrange("p (gh t g h) -> p gh t g h", gh=2, t=2, g=16)

        def sincos(dst_s, dst_c, ang, t_a, t_k, t_ki, keys_ang, key_a, key_k, wkey):
            for dst, shift in ((dst_c, math.pi / 2.0), (dst_s, 0.0)):
                op("dve", lambda e: e.tensor_scalar(t_a, ang, shift, None, op0=ALU.add), R=keys_ang, W=[key_a])
                op("dve", lambda e: e.tensor_scalar(t_k, t_a, 1.0 / TWO_PI, None, op0=ALU.mult), R=[key_a], W=[key_k])
                op("dve", lambda e: e.tensor_copy(t_ki, t_k), R=[key_k], W=[key_k])
                op("dve", lambda e: e.tensor_copy(t_k, t_ki), R=[key_k], W=[key_k])
                op("dve", lambda e: e.scalar_tensor_tensor(t_a, t_k, -TWO_PI, t_a, op0=ALU.mult, op1=ALU.add),
                   R=[key_k, key_a], W=[key_a])
                op("dve", lambda e: e.tensor_scalar(t_a, t_a, -3.1415925, 3.1415925, op0=ALU.max, op1=ALU.min),
                   R=[key_a], W=[key_a])
                op("act", lambda e, dst=dst: e.activation(out=dst, in_=t_a, func=AF.Sin), R=[key_a], W=[wkey])

        def ssm_tables(l, gh, taus, PC, PS, tmpE, tmpA, tmpK, AB, ADT, ldsm):
            T = sum(c for _, _, c in taus)
            g0 = 16 * gh
            LL = ldsm[0:16, 0:256]
            kb.dma("sp", LL[:, 0:64], lam_re[l, g0:g0 + 16, :], W=["ldsm"], dkey="ll")
            kb.dma("sp", LL[:, 64:128], lam_re[l, g0:g0 + 16, :], W=["ldsm"], dkey="ll")
            kb.dma("sp", LL[:, 128:192], lam_im[l, g0:g0 + 16, :], W=["ldsm"], dkey="ll")
            kb.dma("sp", LL[:, 192:256], lam_im[l, g0:g0 + 16, :], W=["ldsm"], dkey="ll")
            op("pe", lambda e: e.matmul(ps[6][:, 0:16], LL[:, 0:128], ident_f[0:16, 0:16], start=True, stop=False),
               R=["ldsm", "ident_f"], W=[("ps", 6)])
            op("pe", lambda e: e.matmul(ps[6][:, 16:32], LL[:, 128:256], ident_f[0:16, 0:16], start=False, stop=True),
               R=["ldsm", "ident_f"], W=[("ps", 6)])
            op("act", lambda e: e.activation(out=AB[:, 0:32], in_=ps[6][:, 0:32], func=AF.Copy), R=[("ps", 6)], W=["AB"])
            DT = ADT[:, 32:48]
            kb.dma("sp", DT, ssm_log_dt[l:l + 1, g0:g0 + 16].partition_broadcast(128).rearrange("p o n -> p (o n)"),
                   W=["ADT"], dkey="dt")
            op("act", lambda e: e.activation(out=DT, in_=DT, func=AF.Exp), R=["ADT"], W=["ADT"])
            op("dve", lambda e: e.tensor_tensor(ADT[:, 0:16], AB[:, 0:16], DT, op=ALU.mult), R=["AB", "ADT"], W=["ADT"])
            op("dve", lambda e: e.tensor_tensor(ADT[:, 16:32], AB[:, 16:32], DT, op=ALU.mult), R=["AB", "ADT"], W=["ADT"])
            TAU = ADT[:, 48:48 + T]
            o = 0
            for (b0, st, c) in taus:
                op("pool", lambda e, o=o, b0=b0, st=st, c=c: e.iota(TAU[:, o:o + c], [[st, c]], base=b0,
                                                                     channel_multiplier=0,
                                                                     allow_small_or_imprecise_dtypes=True),
                   W=["ADT"])
                o += c
            sh = [128, 16, T]
            op("dve", lambda e: e.tensor_tensor(tmpE, ADT[:, 0:16].unsqueeze(2).to_broadcast(sh),
                                                TAU.unsqueeze(1).to_broadcast(sh), op=ALU.mult), R=["ADT"], W=["tmpE"])
            op("act", lambda e: e.activation(out=tmpE, in_=tmpE, func=AF.Exp), R=["tmpE"], W=["tmpE"])
            op("dve", lambda e: e.tensor_tensor(PS, ADT[:, 16:32].unsqueeze(2).to_broadcast(sh),
                                                TAU.unsqueeze(1).to_broadcast(sh), op=ALU.mult), R=["ADT"], W=["PS"])
            sincos(PS, PC, PS, tmpA, tmpK, tmpK.bitcast(I32), ["PS"], "tmpA", "tmpK", "PCS")
            op("dve", lambda e: e.tensor_tensor(PC, PC, tmpE, op=ALU.mult), R=["PCS", "tmpE"], W=["PC"])
            op("dve", lambda e: e.tensor_tensor(PS, PS, tmpE, op=ALU.mult), R=["PCS", "tmpE", "PS"], W=["PS"])

        def stack_load(l, gh, src_r, src_i, ld, ldkey, dst, dstkey, kind, psbank, scale_col):
            g0 = 16 * gh
            if kind == "C":
                L4 = ld[0:16, :].rearrange("g (h r p) -> g h r p", h=16, r=2)
                kb.dma("sp", L4[:, :, 0, :], src_r[l, g0:g0 + 16, :, :], W=[ldkey], dkey=ldkey)
                kb.dma("sp", L4[:, :, 1, :], src_i[l, g0:g0 + 16, :, :], W=[ldkey], dkey=ldkey)
                for h in range(16):
                    op("pe", lambda e, h=h: e.matmul(ps[psbank][:, h * 16:(h + 1) * 16],
                                                     L4[:, h, :, :].rearrange("g r p -> g (r p)"),
                                                     ident_f[0:16, 0:16], start=(h == 0), stop=(h == 15)),
                       R=[ldkey, "ident_f"], W=[("ps", psbank)], sig=(h == 15))
            else:
                L4 = ld[0:16, :].rearrange("g (r p h) -> g r p h", r=2, p=64)
                kb.dma("sp", L4[:, 0, :, :], src_r[l, g0:g0 + 16, :, :], W=[ldkey], dkey=ldkey)
                kb.dma("sp", L4[:, 1, :, :], src_i[l, g0:g0 + 16, :, :], W=[ldkey], dkey=ldkey)
                for h in range(16):
                    op("pe", lambda e, h=h: e.matmul(ps[psbank][:, h * 16:(h + 1) * 16],
                                                     L4[:, :, :, h].rearrange("g r p -> g (r p)"),
                                                     ident_f[0:16, 0:16], start=(h == 0), stop=(h == 15)),
                       R=[ldkey, "ident_f"], W=[("ps", psbank)], sig=(h == 15))
            op("dve", lambda e: e.tensor_scalar(dst, ps[psbank][:, 0:256].rearrange("p (h g) -> p g h", h=16),
                                                scale_col, None, op0=ALU.mult),
               R=[("ps", psbank), "SGN"], W=[dstkey])

        def outer_combine(dst_bf, PCv, PSv, Sa, Sb, nt, keysR, wkey, pbanks):
            sh = [128, 16, nt, 16]
            tmp = PSALL[:, 512 * pbanks:512 * pbanks + 16 * nt * 16].rearrange("p (g t h) -> p g t h", g=16, t=nt)
            pk = [("ps", pbanks + j) for j in range((16 * nt * 16 + 511) // 512)]
            op("dve", lambda e: e.tensor_tensor(tmp, PCv.unsqueeze(3).to_broadcast(sh),
                                                Sa.unsqueeze(2).to_broadcast(sh), op=ALU.mult), R=keysR, W=pk)
            op("pool", lambda e: e.tensor_tensor(dst_bf, PSv.unsqueeze(3).to_broadcast(sh),
                                                 Sb.unsqueeze(2).to_broadcast(sh), op=ALU.mult), R=keysR, W=[wkey])
            op("dve", lambda e: e.tensor_tensor(dst_bf, tmp, dst_bf, op=ALU.add), R=pk + [wkey], W=[wkey])

        def ssm_gen_A(l, gh):
            g0 = 16 * gh
            ldA = FTf[:, 0:2048]
            ldB = FTf[:, 2048:4096]
            ST = FTf[:, 4096:5632].rearrange("p (s g h) -> p s g h", s=6, g=16)
            BzT = FT[:, 2 * 5632:2 * 7680].rearrange("p (g a m) -> p g a m", g=16, a=2)
            CF = FT[:, 0:4096].rearrange("p (g a m) -> p g a m", g=16, a=2)
            PC = SM[:, 0:512].rearrange("p (g t) -> p g t", g=16)
            PS = SM[:, 512:1024].rearrange("p (g t) -> p g t", g=16)
            tmpE = SM[:, 1024:1536].rearrange("p (g t) -> p g t", g=16)
            tmpA = SM[:, 1536:2048].rearrange("p (g t) -> p g t", g=16)
            tmpK = SM[:, 2048:2560].rearrange("p (g t) -> p g t", g=16)
            AB = TMP4[:, 0:32]
            ADT = TMP4[:, 32:160]
            KAP = TMP4[:, 160:320]
            ssm_tables(l, gh, [(15, -1, 16), (-7, 1, 16)], PC, PS, tmpE, tmpA, tmpK, AB, ADT, TMP4[:, 512:768])
            S3p = S34[:, gh, 0, :, :]
            S4p = S34[:, gh, 1, :, :]
            stack_load(l, gh, ssm_c_re, ssm_c_im, ldA, "ldA", S3p, "S34", "C", 7, SGN[:, 0:1])
            stack_load(l, gh, ssm_c_im, ssm_c_re, ldB, "ldB", ST[:, 1], "ST1", "C", 6, -1.0)
            op("pool", lambda e: e.tensor_copy(S4p, ST[:, 1]), R=["ST1"], W=["S34"])
            stack_load(l, gh, ssm_b_re, ssm_b_im, ldA, "ldA", ST[:, 2], "ST2", "B", 7, 1.0)
            stack_load(l, gh, ssm_b_im, ssm_b_re, ldB, "ldB", ST[:, 3], "ST3", "B", 6, SGN[:, 1:2])
            a_ = AB[:, 0:16]
            b_ = AB[:, 16:32]
            L1r = PC[:, :, 24]
            L1i = PS[:, :, 24]
            nr, den, t1, t2, kr, ki = [KAP[:, 16 * j:16 * (j + 1)] for j in range(6)]
            op("dve", lambda e: e.tensor_scalar(nr, L1r, -1.0, None, op0=ALU.add), R=["PC"], W=["KAP"])
            op("dve", lambda e: e.tensor_tensor(den, a_, a_, op=ALU.mult), R=["AB"], W=["KAP"])
            op("dve", lambda e: e.tensor_tensor(t1, b_, b_, op=ALU.mult), R=["AB"], W=["KAP"])
            op("dve", lambda e: e.tensor_tensor(den, den, t1, op=ALU.add), R=["KAP"], W=["KAP"])
            op("dve", lambda e: e.reciprocal(den, den), R=["KAP"], W=["KAP"])
            op("dve", lambda e: e.tensor_tensor(t1, nr, a_, op=ALU.mult), R=["KAP", "AB"], W=["KAP"])
            op("dve", lambda e: e.tensor_tensor(t2, L1i, b_, op=ALU.mult), R=["PS", "AB"], W=["KAP"])
            op("dve", lambda e: e.tensor_tensor(t1, t1, t2, op=ALU.add), R=["KAP"], W=["KAP"])
            op("dve", lambda e: e.tensor_tensor(kr, t1, den, op=ALU.mult), R=["KAP"], W=["KAP"])
            op("dve", lambda e: e.tensor_tensor(t1, L1i, a_, op=ALU.mult), R=["PS", "AB", "KAP"], W=["KAP"])
            op("dve", lambda e: e.tensor_tensor(t2, nr, b_, op=ALU.mult), R=["KAP", "AB"], W=["KAP"])
            op("dve", lambda e: e.tensor_tensor(t1, t1, t2, op=ALU.subtract), R=["KAP"], W=["KAP"])
            op("dve", lambda e: e.tensor_tensor(ki, t1, den, op=ALU.mult), R=["KAP"], W=["KAP"])
            sh3 = [128, 16, 16]
            krb = kr.unsqueeze(2).to_broadcast(sh3)
            kib = ki.unsqueeze(2).to_broadcast(sh3)
            op("dve", lambda e: e.tensor_tensor(ST[:, 4], ST[:, 2], krb, op=ALU.mult), R=["ST2", "KAP"], W=["ST4"])
            op("dve", lambda e: e.tensor_tensor(ST[:, 0], ST[:, 3], kib, op=ALU.mult), R=["ST3", "KAP"], W=["ST0"])
            op("dve", lambda e: e.tensor_tensor(ST[:, 4], ST[:, 4], ST[:, 0], op=ALU.add), R=["ST4", "ST0"], W=["ST4"])
            op("dve", lambda e: e.tensor_tensor(ST[:, 5], ST[:, 3], krb, op=ALU.mult), R=["ST3", "KAP"], W=["ST5"])
            op("dve", lambda e: e.tensor_tensor(ST[:, 0], ST[:, 2], kib, op=ALU.mult), R=["ST2", "KAP", "ST4"], W=["ST0"])
            op("dve", lambda e: e.tensor_tensor(ST[:, 5], ST[:, 5], ST[:, 0], op=ALU.subtract), R=["ST5", "ST0"], W=["ST5"])
            outer_combine(BzT.rearrange("p g a (j h) -> p g (a j) h", h=16), PC[:, :, 0:16], PS[:, :, 0:16],
                          ST[:, 4], ST[:, 5], 16, ["PC", "PS", "ST4", "ST5"], "BzT", 0)
            outer_combine(CF.rearrange("p g a (j h) -> p g (a j) h", h=16), PC[:, :, 16:32], PS[:, :, 16:32],
                          S3p, S4p, 16, ["PC", "PS", "S34", "ldA", "ldB"], "ldA", 0)
            for gp in range(8):
                pb = 4 + (gp % 2)
                for j in range(4):
                    g, jb = 2 * gp + j // 2, j % 2
                    op("pe", lambda e, g=g, jb=jb, j=j: e.matmul(ps[pb][:, j * 128:(j + 1) * 128], BzT[:, g, jb, :],
                                                               ident_b[:], start=(j == 0), stop=(j == 3)),
                       R=["BzT", "ident_b"], W=[("ps", pb)], sig=(j == 3))
                op("act", lambda e: e.activation(
                    out=BZCL[:, g0 + 2 * gp:g0 + 2 * gp + 2, :, :].rearrange("p g a m -> p (g a m)"),
                    in_=ps[pb][:], func=AF.Copy), R=[("ps", pb)], W=["BZCL"])
            for g in range(16):
                pb = 6 + (g % 2)
                op("pe", lambda e, g=g: e.matmul(ps[pb][:, 0:256], BzT[:, g, 1, :],
                                                 CF[:, g, :, :].rearrange("p a m -> p (a m)"), start=True, stop=True),
                   R=["BzT", "ldA"], W=[("ps", pb)])
                tm = TMP4[:, 768:1024].bitcast(BF16)[:, 0:256] if g % 2 == 0 else TMP4[:, 896:1024].bitcast(BF16)
                tkey = "tmM0" if g % 2 == 0 else "tmM1"
                tm = TMP4[:, 768:896].bitcast(BF16) if g % 2 == 0 else TMP4[:, 896:1024].bitcast(BF16)
                op("dve", lambda e: e.tensor_tensor(tm, ps[pb][:, 0:256], MASKM[:], op=ALU.mult),
                   R=[("ps", pb), "MASKM"], W=[tkey])
                op("dve", lambda e, g=g: e.scalar_tensor_tensor(
                    MW[:, g0 + g, :, :].rearrange("p a m -> p (a m)"), DIAG0[:], DCOL[:, g0 + g:g0 + g + 1], tm,
                    op0=ALU.mult, op1=ALU.add), R=["DIAG0", "DCOL", tkey], W=["MW"])

        def ssm_gen_B(l, gh):
            g0 = 16 * gh
            PC = TMP4[:, 0:256].rearrange("p (g t) -> p g t", g=16)
            PS = TMP4[:, 256:512].rearrange("p (g t) -> p g t", g=16)
            tmpE = TMP4[:, 512:768].rearrange("p (g t) -> p g t", g=16)
            tmpA = TMP4[:, 768:1024].rearrange("p (g t) -> p g t", g=16)
            tmpK = SM[:, 2048:2304].rearrange("p (g t) -> p g t", g=16)
            AB = SM[:, 2304:2336]
            ADT = SM[:, 2336:2432]
            ssm_tables(l, gh, [(1, 1, 16)], PC, PS, tmpE, tmpA, tmpK, AB, ADT, SM[:, 0:256])
            outer_combine(BZCL[:, g0:g0 + 16, :, :].rearrange("p g a (j h) -> p g (a j) h", h=16), PC, PS,
                          S34[:, gh, 0, :, :], S34[:, gh, 1, :, :], 16, ["PC", "PS", "S34"], "BZCL", 0)

        PWR = SM[:, 512:768].rearrange("p (g t) -> p g t", g=16)
        PWI = SM[:, 768:1024].rearrange("p (g t) -> p g t", g=16)

        def ssm_scan_coefs(l):
            LL = SM[0:16, 0:256]
            kb.dma("sp", LL[:, 0:128].rearrange("g (a p) -> g a p", a=2),
                   lam_re[l].rearrange("(a g) p -> g a p", a=2), W=["ldsm"], dkey="scl")
            kb.dma("sp", LL[:, 128:256].rearrange("g (a p) -> g a p", a=2),
                   lam_im[l].rearrange("(a g) p -> g a p", a=2), W=["ldsm"], dkey="scl")
            op("pe", lambda e: e.matmul(ps[6][:, 32:48], LL[:, 0:128], ident_f[0:16, 0:16], start=True, stop=False),
               R=["ldsm", "ident_f"], W=[("ps", 6)])
            op("pe", lambda e: e.matmul(ps[6][:, 48:64], LL[:, 128:256], ident_f[0:16, 0:16], start=False, stop=True),
               R=["ldsm", "ident_f"], W=[("ps", 6)])
            A_ = TMP4[:, 0:16]
            B_ = TMP4[:, 16:32]
            DT = TMP4[:, 32:48]
            TAU = TMP4[:, 48:64]
            g3 = lambda ap: ap.rearrange("p (g t) -> p g t", g=16)
            EA = g3(TMP4[:, 64:320])
            AN = g3(TMP4[:, 320:576])
            tA = g3(TMP4[:, 576:832])
            tK = g3(SM[:, 256:512])
            op("act", lambda e: e.activation(out=TMP4[:, 0:32], in_=ps[6][:, 32:64], func=AF.Copy), R=[("ps", 6)], W=["TMP4"])
            for a in range(2):
                kb.dma("sp", DT[64 * a:64 * a + 64, :],
                       ssm_log_dt[l:l + 1, 16 * a:16 * a + 16].partition_broadcast(64).rearrange("p o n -> p (o n)"),
                       W=["TMP4"], dkey="dts")
            op("act", lambda e: e.activation(out=DT, in_=DT, func=AF.Exp), R=["TMP4"], W=["TMP4"])
            op("pool", lambda e: e.iota(TAU, [[16, 16]], base=16, channel_multiplier=0,
                                        allow_small_or_imprecise_dtypes=True), R=["TMP4"], W=["TMP4"])
            op("dve", lambda e: e.tensor_tensor(A_, A_, DT, op=ALU.mult), R=["TMP4"], W=["TMP4"])
            op("dve", lambda e: e.tensor_tensor(B_, B_, DT, op=ALU.mult), R=["TMP4"], W=["TMP4"])
            sh = [128, 16, 16]
            op("dve", lambda e: e.tensor_tensor(EA, A_.unsqueeze(2).to_broadcast(sh), TAU.unsqueeze(1).to_broadcast(sh),
                                                op=ALU.mult), R=["TMP4"], W=["TMP4"])
            op("act", lambda e: e.activation(out=EA, in_=EA, func=AF.Exp), R=["TMP4"], W=["TMP4"])
            op("dve", lambda e: e.tensor_tensor(AN, B_.unsqueeze(2).to_broadcast(sh), TAU.unsqueeze(1).to_broadcast(sh),
                                                op=ALU.mult), R=["TMP4"], W=["TMP4"])
            sincos(PWI, PWR, AN, tA, tK, tK.bitcast(I32), ["TMP4"], "TMP4", "tKc", "PW")
            op("dve", lambda e: e.tensor_tensor(PWR, PWR, EA, op=ALU.mult), R=["PW", "TMP4"], W=["PW"])
            op("dve", lambda e: e.tensor_tensor(PWI, PWI, EA, op=ALU.mult), R=["PW", "TMP4"], W=["PW"])
            for j, t in ((0, 0), (1, 15)):
                op("dve", lambda e, j=j, t=t: e.tensor_copy(LAB[:, j, 0, 0, :], PWR[:, :, t]), R=["PW"], W=["LAB"])
                op("dve", lambda e, j=j, t=t: e.tensor_copy(LAB[:, j, 0, 1, :], PWR[:, :, t]), R=["PW"], W=["LAB"])
                op("dve", lambda e, j=j, t=t: e.tensor_copy(LAB[:, j, 1, 1, :], PWI[:, :, t]), R=["PW"], W=["LAB"])
                op("dve", lambda e, j=j, t=t: e.tensor_scalar(LAB[:, j, 1, 0, :], PWI[:, :, t], -1.0, None, op0=ALU.mult),
                   R=["PW"], W=["LAB"])

        def ssm_main(l):
            DT_ = SS[0:16, 0:32]
            with nc.allow_non_contiguous_dma(reason="tiny D load"):
                kb.dma("sp", DT_, ssm_d[l].rearrange("(g h) -> h g", h=16), W=["dT"], dkey="dT")
            op("pe", lambda e: e.matmul(ps[5][:, 0:32], REP16[:], DT_, start=True, stop=True),
               R=["REP16", "dT"], W=[("ps", 5)])
            op("act", lambda e: e.activation(out=DCOL[:], in_=ps[5][:, 0:32], func=AF.Copy), R=[("ps", 5)], W=["DCOL"])
            ssm_gen_A(l, 0)
            if debug is not None and debug[0] == f"tab{l}":
                kb.barrier()
                kb.dma("sp", dbg_d[:, 0:1024], SM[:, 0:1024], dkey="dbg")
                kb.dma("sp", dbg_d[:, 1024:2560], FTf[:, 4096:5632], dkey="dbg")
                kb.dma("sp", dbg_d[:, 2560:2880], TMP4[:, 0:320], dkey="dbg")
                kb.dma("sp", dbg_d[:, 2880:3904], TMP4b[:, 0:1024], dkey="dbg")
                return True
            ssm_gen_A(l, 1)
            if debug is not None and debug[0] == f"bz{l}":
                dump(BIG[:, 0:8192], 8192, F32, ["BZCL", "MW"])
                return True
            kb.barrier()
            ALLU = [("UCM", j) for j in range(NT)]
            for b8 in range(8):
                pb = b8 % 2
                for j in range(8):
                    g, jb = 4 * b8 + j // 2, j % 2
                    op("pe", lambda e, g=g, jb=jb, j=j: e.transpose(
                        psb(pb)[:, j * 128:(j + 1) * 128], UCMg[:, g, 8 * jb:8 * jb + 8, :].rearrange("p j h -> p (j h)"),
                        ident_b[:]),
                       R=ALLU + ["ident_b"], W=[("ps", pb)], sig=(j == 7))
                eng = "act" if b8 % 2 == 0 else "dve"
                dst = UT[:, 4 * b8:4 * b8 + 4, :, :].rearrange("p g a c -> p (g a c)")
                if eng == "act":
                    op("act", lambda e: e.activation(out=dst, in_=psb(pb), func=AF.Copy), R=[("ps", pb)], W=["UT"])
                else:
                    op("dve", lambda e: e.tensor_copy(dst, psb(pb)), R=[("ps", pb)], W=["UT"])
            for g4 in range(8):
                pb = 2 + (g4 % 2)
                for j in range(4):
                    g = 4 * g4 + j
                    for jb in range(2):
                        op("pe", lambda e, g=g, jb=jb, j=j: e.matmul(ps[pb][:, j * 128:(j + 1) * 128], BZCL[:, g, jb, :],
                                                                   UT[:, g, jb, :], start=(j == 0 and jb == 0),
                                                                   stop=(j == 3 and jb == 1)),
                           R=["BZCL", "UT"], W=[("ps", pb)], sig=(j == 3 and jb == 1))
                ghh, gl0 = g4 // 4, 4 * (g4 % 4)
                pv = ps[pb][:].rearrange("p (g c) -> p g c", g=4)
                op("act", lambda e: e.activation(out=ZS[64 * ghh:64 * ghh + 64, 0, gl0:gl0 + 4, :], in_=pv[0:64],
                                                 func=AF.Copy), R=[("ps", pb)], W=["ZS"])
                op("dve", lambda e: e.tensor_copy(ZS[64 * ghh:64 * ghh + 64, 1, gl0:gl0 + 4, :], pv[64:128]),
                   R=[("ps", pb)], W=["ZS"])
            if debug is not None and debug[0] == f"z0{l}":
                dump(FTf[:, 4096:8192], 4096, F32, ["ZS"])
                return True
            ssm_scan_coefs(l)
            kb.barrier()
            ssm_gen_B(l, 0)
            ssm_gen_B(l, 1)
            ZS5 = FTf[:, 4096:8192].rearrange("p (r g b s) -> p r g b s", r=2, g=16, b=8)
            SCb = SM[:, 1024:1536].rearrange("p (a r g b) -> p a r g b", a=2, r=2, g=16)
            sh4 = [128, 2, 16, 8]
            sh3 = [128, 16, 8]
            for s_ in range(1, 16):
                prev = ZS5[:, :, :, :, s_ - 1]
                cur = ZS5[:, :, :, :, s_]
                op("pool", lambda e: e.tensor_tensor(SCb[:, 0], prev, LAB[:, 0, 0].unsqueeze(3).to_broadcast(sh4),
                                                     op=ALU.mult), R=["ZS", "LAB"], W=["SC0"])
                op("pool", lambda e: e.tensor_tensor(SCb[:, 1, 0], prev[:, 1], LAB[:, 0, 1, 0, :].unsqueeze(2).to_broadcast(sh3),
                                                     op=ALU.mult), R=["ZS", "LAB"], W=["SC1"])
                op("pool", lambda e: e.tensor_tensor(SCb[:, 1, 1], prev[:, 0], LAB[:, 0, 1, 1, :].unsqueeze(2).to_broadcast(sh3),
                                                     op=ALU.mult), R=["ZS", "LAB"], W=["SC1"])
                op("pool", lambda e: e.tensor_tensor(SCb[:, 0], SCb[:, 0], SCb[:, 1], op=ALU.add), R=["SC0", "SC1"], W=["SC0"])
                op("pool", lambda e: e.tensor_tensor(cur, cur, SCb[:, 0], op=ALU.add), R=["ZS", "SC0"], W=["ZS"])
            SC2 = SM[:, 1536:1600].rearrange("p (a r g) -> p a r g", a=2, r=2)
            for bk in range(1, 8):
                prev = ZS5[:, :, :, bk - 1, 15]
                cur = ZS5[:, :, :, bk, 15]
                op("pool", lambda e: e.tensor_tensor(SC2[:, 0], prev, LAB[:, 1, 0], op=ALU.mult), R=["ZS", "LAB"], W=["SC20"])
                op("pool", lambda e: e.tensor_tensor(SC2[:, 1, 0, :], prev[:, 1, :], LAB[:, 1, 1, 0, :], op=ALU.mult),
                   R=["ZS", "LAB"], W=["SC21"])
                op("pool", lambda e: e.tensor_tensor(SC2[:, 1, 1, :], prev[:, 0, :], LAB[:, 1, 1, 1, :], op=ALU.mult),
                   R=["ZS", "LAB"], W=["SC21"])
                op("pool", lambda e: e.tensor_tensor(SC2[:, 0], SC2[:, 0], SC2[:, 1], op=ALU.add), R=["SC20", "SC21"], W=["SC20"])
                op("pool", lambda e: e.tensor_tensor(cur, cur, SC2[:, 0], op=ALU.add), R=["ZS", "SC20"], W=["ZS"])
            F_ = [SM[:, 1600:1840].rearrange("p (g s) -> p g s", g=16),
                  SM[:, 1024:1264].rearrange("p (g s) -> p g s", g=16)]
            sh15 = [128, 16, 15]
            pr = PWR[:, :, 0:15]
            pi_ = PWI[:, :, 0:15]
            for bk in range(1, 8):
                cr = ZS5[:, 0, :, bk - 1, 15].unsqueeze(2).to_broadcast(sh15)
                ci = ZS5[:, 1, :, bk - 1, 15].unsqueeze(2).to_broadcast(sh15)
                re = ZS5[:, 0, :, bk, 0:15]
                im = ZS5[:, 1, :, bk, 0:15]
                for (dst, pa, ca, pb_, cb, sub) in ((re, pr, cr, pi_, ci, True), (im, pr, ci, pi_, cr, False)):
                    op("pool", lambda e, pa=pa, ca=ca: e.tensor_tensor(F_[0], pa, ca, op=ALU.mult), R=["PW", "ZS"], W=["F0"])
                    op("pool", lambda e, pb_=pb_, cb=cb: e.tensor_tensor(F_[1], pb_, cb, op=ALU.mult), R=["PW", "ZS"], W=["SC0"])
                    op("pool", lambda e, dst=dst: e.tensor_tensor(dst, dst, F_[0], op=ALU.add), R=["F0", "ZS"], W=["ZS"])
                    op("pool", lambda e, dst=dst, sub=sub: e.tensor_tensor(dst, dst, F_[1],
                                                                      op=(ALU.subtract if sub else ALU.add)),
                       R=["SC0", "ZS"], W=["ZS"])
            if debug is not None and debug[0] == f"zs{l}":
                dump(FTf[:, 4096:8192], 4096, F32, ["ZS"])
                return True
            op("pool", lambda e: e.memset(XSB[:, :, 0:1], 0.0), R=["ZS"], W=["XSB", "ldsm"])
            for ghh in range(2):
                for r in range(2):
                    eng = "act" if r == 0 else "dve"
                    src = ZS[64 * ghh:64 * ghh + 64, r, :, 0:127]
                    dst = XSB[64 * r:64 * r + 64, 16 * ghh:16 * ghh + 16, 1:128]
                    if eng == "act":
                        op("act", lambda e: e.activation(out=dst, in_=src, func=AF.Copy), R=["ZS"], W=["XSB", "ldsm"])
                    else:
                        op("dve", lambda e: e.tensor_copy(dst, src), R=["ZS"], W=["XSB", "ldsm"])
            for gp in range(16):
                pb = 4 + (gp % 4)
                for j in range(2):
                    g = 2 * gp + j
                    o = j * 256
                    op("pe", lambda e, g=g, o=o, j=j: e.matmul(ps[pb][:, o:o + 256], UT[:, g, 0, :],
                                                          MW[:, g, :, :].rearrange("p a m -> p (a m)"),
                                                          start=(j == 0), stop=False, skip_group_check=True),
                       R=["UT", "MW"], W=[("ps", pb)], sig=False)
                    op("pe", lambda e, g=g, o=o: e.matmul(ps[pb][:, o + 128:o + 256], UT[:, g, 1, :], MW[:, g, 0, :],
                                                     start=False, stop=False, skip_group_check=True),
                       R=["UT", "MW"], W=[("ps", pb)], sig=False)
                    op("pe", lambda e, g=g, o=o: e.matmul(ps[pb][:, o:o + 256], XSB[:, g, :],
                                                     BZCL[:, g, :, :].rearrange("p a m -> p (a m)"),
                                                     start=False, stop=(j == 1), skip_group_check=True),
                       R=["XSB", "BZCL"], W=[("ps", pb)], sig=(j == 1))
                dst = UCM[:, :, 32 * gp:32 * gp + 32].rearrange("p i (g h) -> p g i h", g=2)
                op("act", lambda e: e.activation(out=dst, in_=ps[pb][:].rearrange("p (g i h) -> p g i h", g=2, i=16),
                                                 func=AF.Gelu_apprx_tanh), R=[("ps", pb)], W=ALLU)

        def glu_phase(l):
            WGLU = TMP4[:].bitcast(BF16).rearrange("p (k n) -> p k n", k=4)
            kb.dma("pool", WGLU, w_glu[l].rearrange("(k p) n -> p k n", p=128), W=["TMP4"], dkey="wglu")
            ZSS = SS[:, 0:16]
            def tile(i):
                s2 = i % 2
                pb = s2
                YGT = SM[:, 2048 + 256 * s2:2048 + 256 * (s2 + 1)].bitcast(BF16)
                SG = SM[:, 1024 * s2:1024 * s2 + 512]
                ZF = SM[:, 1024 * s2 + 512:1024 * s2 + 1024]
                ZN = SG.bitcast(BF16)[:, 0:512]
                for fc in range(4):
                    op("pe", lambda e, fc=fc: e.transpose(psb(pb)[:, fc * 128:(fc + 1) * 128],
                                                          UCM[:, i, fc * 128:(fc + 1) * 128], ident_b[:]),
                       R=[("UCM", i), "ident_b"], W=[("ps", pb)], sig=(fc == 3))
                    yield
                op("act", lambda e: e.activation(out=YGT, in_=psb(pb)[:, 0:512], func=AF.Copy),
                   R=[("ps", pb)], W=[("YGT", s2)])
                yield
                pg = 2 + s2
                for fc in range(4):
                    op("pe", lambda e, fc=fc: e.matmul(ps[pg][:], YGT[:, fc * 128:(fc + 1) * 128], WGLU[:, fc, :],
                                                       start=(fc == 0), stop=(fc == 3)),
                       R=[("YGT", s2), "TMP4"], W=[("ps", pg)], sig=(fc == 3))
                    yield
                op("act", lambda e: e.activation(out=SG, in_=ps[pg][:], func=AF.Sigmoid), R=[("ps", pg)], W=[("SG", s2)])
                yield
                op("dve", lambda e: e.tensor_tensor(ZF, UCM[:, i, :], SG, op=ALU.mult),
                   R=[("UCM", i), ("SG", s2)], W=[("ZF", s2)])
                yield
                op("dve", lambda e: e.scalar_tensor_tensor(ZN, ZF, 1.0, ZF, op0=ALU.mult, op1=ALU.mult,
                                                           accum_out=ZSS[:, i:i + 1]),
                   R=[("ZF", s2), ("SG", s2)], W=[("SG", s2), ("ZSS", i)])
                yield
                op("dve", lambda e: e.tensor_scalar(SS[:, 16 + i:17 + i], ZSS[:, i:i + 1], 1.0 / 512.0, EPS,
                                                    op0=ALU.mult, op1=ALU.add), R=[("ZSS", i)], W=[("ZSb", i)])
                yield
                op("pool", lambda e: e.tensor_tensor(SS[:, 32 + i:33 + i], SS[:, 16 + i:17 + i], NHALF[:, 0:1],
                                                     op=ALU.pow), R=[("ZSb", i), "NHALF"], W=[("ZRS", i)])
                yield
                op("dve", lambda e: e.tensor_scalar(ZN, ZF, SS[:, 32 + i:33 + i], None, op0=ALU.mult),
                   R=[("ZF", s2), ("ZRS", i), ("SG", s2)], W=[("SG", s2)])
                yield
                pt = 4 + s2
                for fc in range(4):
                    op("pe", lambda e, fc=fc: e.transpose(psb(pt)[:, fc * 128:(fc + 1) * 128],
                                                          ZN[:, fc * 128:(fc + 1) * 128], ident_b[:]),
                       R=[("SG", s2), "ident_b"], W=[("ps", pt)], sig=(fc == 3))
                    yield
                op("act", lambda e: e.activation(out=FT3[:, 0:4, i::16],
                                                 in_=psb(pt)[:, 0:512].rearrange("p (k c) -> p k c", k=4),
                                                 func=AF.Copy), R=[("ps", pt)], W=[("ST", i)])
                yield

            run_rr([tile(i) for i in range(NT)], 2, 8)

        WOUT = big_bf(0, 4096).rearrange("p (k n) -> p k n", k=8)

        def wout_load(l):
            kb.dma("pool", WOUT, w_out[l].rearrange("(k p) n -> p k n", p=128), W=["WOUT"], dkey="wout")
            for k in range(8):
                op("dve", lambda e, k=k: e.scalar_tensor_tensor(WOUT[:, k, :], WOUT[:, k, :], GROW[:, k:k + 1],
                                                                MOD[:, 2, :], op0=ALU.mult, op1=ALU.mult),
                   R=["WOUT", "GROW", ("MOD", 2)], W=["WOUT"])

        def wout_phase(l):
            ALLAT = [("AT", n) for n in range(NT)]
            ALLST = [("ST", n) for n in range(NT)]
            cnt = 0
            for i in range(NT):
                for half in range(2):
                    pb = 4 + (cnt % 4)
                    cnt += 1
                    for k in range(8):
                        lhs = AT3[:, k, i::16] if k < 4 else FT3[:, k - 4, i::16]
                        op("pe", lambda e, k=k, lhs=lhs: e.matmul(ps[pb][:], lhs, WOUT[:, k, half * 512:(half + 1) * 512],
                                                                 start=(k == 0), stop=(k == 7)),
                           R=ALLAT + ALLST + ["WOUT"], W=[("ps", pb)], sig=(k == 7))
                    xs_ = X[:, i, half * 512:(half + 1) * 512]
                    op("dve", lambda e, xs_=xs_: e.tensor_tensor(xs_, xs_, ps[pb][:], op=ALU.add),
                       R=[("ps", pb), ("X", i)], W=[("X", i)])

        H2T3 = big_bf(4096, 12288).rearrange("p (k t) -> p k t", k=8)
        ACTT = big_bf(0, 4096).rearrange("p (k t) -> p k t", k=4)

        def ring(sl):
            if sl < 4:
                return FT[:, sl * 4096:(sl + 1) * 4096]
            return AT[:, (sl - 4) * 4096:(sl - 3) * 4096]

        def router(l):
            ALLH = [("H2", i) for i in range(NT)]
            for i in range(NT):
                for k in range(8):
                    op("pe", lambda e, k=k: e.matmul(ps[6][:, i * 16:(i + 1) * 16], H2T3[:, k, i * 128:(i + 1) * 128],
                                                     WR[:, k, :], start=(i == 0 and k == 0), stop=(i == NT - 1 and k == 7),
                                                     skip_group_check=True),
                       R=[("H2", i), "WR"], W=[("ps", 6)], sig=(i == NT - 1 and k == 7))
            v3 = lambda ap: ap.rearrange("p (i e) -> p i e", i=16)
            v4 = lambda ap: ap.rearrange("p (i g e) -> p i g e", i=16, g=4)
            LG = TMP4[:, 0:256]
            PR = TMP4[:, 256:512]
            SEL = TMP4[:, 512:768]
            SEL2 = TMP4[:, 768:1024]
            EQ1 = TMP4b[:, 0:256]
            EQ2 = TMP4b[:, 256:512]
            CMB = TMP4b[:, 512:768]
            CMBb = TMP4b[:, 768:896].bitcast(BF16)
            MX = SS[:, 0:16]
            SM_ = SS[:, 16:32]
            M1 = SM[:, 2048:2112]
            M2 = SM[:, 2112:2176]
            GS = SM[:, 2176:2240]
            GM = SS[:, 32:48]
            ING = SM[:, 2240:2304]
            b3 = lambda ap: ap.unsqueeze(2).to_broadcast([128, 16, 16])
            op("dve", lambda e: e.tensor_reduce(MX, v3(ps[6][:, 0:256]), axis=AX.X, op=ALU.max), R=[("ps", 6)], W=["r_mx"])
            op("dve", lambda e: e.tensor_tensor(v3(LG), v3(ps[6][:, 0:256]), b3(MX), op=ALU.subtract),
               R=[("ps", 6), "r_mx"], W=["TMP4"])
            op("act", lambda e: e.activation(out=LG, in_=LG, func=AF.Exp), R=["TMP4"], W=["TMP4"])
            op("dve", lambda e: e.tensor_reduce(SM_, v3(LG), axis=AX.X, op=ALU.add), R=["TMP4"], W=["r_sm"])
            op("dve", lambda e: e.reciprocal(SM_, SM_), R=["r_sm"], W=["r_sm"])
            op("dve", lambda e: e.tensor_tensor(v3(PR), v3(LG), b3(SM_), op=ALU.mult), R=["TMP4", "r_sm"], W=["TMP4"])
            op("dve", lambda e: e.tensor_tensor(v3(SEL), v3(PR), RB[:].unsqueeze(1).to_broadcast([128, 16, 16]),
                                                op=ALU.add), R=["TMP4", "RB"], W=["TMP4"])
            op("dve", lambda e: e.tensor_reduce(M1, SEL.rearrange("p (j e) -> p j e", e=4), axis=AX.X, op=ALU.max),
               R=["TMP4"], W=["r_m1"])
            b4 = lambda ap: ap.unsqueeze(2).to_broadcast([128, 64, 4])
            j4 = lambda ap: ap.rearrange("p (j e) -> p j e", e=4)
            op("dve", lambda e: e.tensor_tensor(j4(EQ1), j4(SEL), b4(M1), op=ALU.is_equal), R=["TMP4", "r_m1"], W=["TMP4b"])
            op("dve", lambda e: e.scalar_tensor_tensor(SEL2, EQ1, -1.0e9, SEL, op0=ALU.mult, op1=ALU.add),
               R=["TMP4b", "TMP4"], W=["TMP4"])
            op("dve", lambda e: e.tensor_reduce(M2, j4(SEL2), axis=AX.X, op=ALU.max), R=["TMP4"], W=["r_m2"])
            op("dve", lambda e: e.tensor_tensor(j4(EQ2), j4(SEL2), b4(M2), op=ALU.is_equal), R=["TMP4", "r_m2"], W=["TMP4b"])
            op("dve", lambda e: e.tensor_tensor(GS, M1, M2, op=ALU.add), R=["r_m1", "r_m2"], W=["r_gs"])
            op("dve", lambda e: e.tensor_reduce(GM, GS.rearrange("p (i g) -> p i g", g=4), axis=AX.X, op=ALU.max),
               R=["r_gs"], W=["r_gm"])
            op("dve", lambda e: e.tensor_tensor(ING.rearrange("p (i g) -> p i g", g=4), GS.rearrange("p (i g) -> p i g", g=4),
                                                GM.unsqueeze(2).to_broadcast([128, 16, 4]), op=ALU.is_equal),
               R=["r_gs", "r_gm"], W=["r_ing"])
            op("dve", lambda e: e.tensor_tensor(EQ1, EQ1, EQ2, op=ALU.add), R=["TMP4b"], W=["TMP4b"])
            op("dve", lambda e: e.tensor_tensor(j4(EQ1), j4(EQ1), b4(ING), op=ALU.mult), R=["TMP4b", "r_ing"], W=["TMP4b"])
            op("dve", lambda e: e.tensor_tensor(EQ1, EQ1, PR, op=ALU.mult), R=["TMP4b", "TMP4"], W=["TMP4b"])
            op("dve", lambda e: e.tensor_reduce(SM_, v3(EQ1), axis=AX.X, op=ALU.add), R=["TMP4b"], W=["r_sm"])
            op("dve", lambda e: e.reciprocal(SM_, SM_), R=["r_sm"], W=["r_sm"])
            op("dve", lambda e: e.tensor_tensor(v3(CMBb), v3(EQ1), b3(SM_), op=ALU.mult), R=["TMP4b", "r_sm"], W=["TMP4b"])
            if debug is not None and debug[0] == f"comb{l}":
                op("dve", lambda e: e.tensor_tensor(v3(CMB), v3(EQ1), b3(SM_), op=ALU.mult), R=["TMP4b", "r_sm"], W=["TMP4b"])
                dump(CMB, 256, F32, ["TMP4b"])
                return True
            CT = SM[0:16, 0:1024].bitcast(BF16)
            for i in range(NT):
                pb = 4 + (i // 8)
                op("pe", lambda e, i=i: e.transpose(psb(pb)[0:16, (i % 8) * 128:(i % 8 + 1) * 128],
                                                    CMBb[:, i * 16:(i + 1) * 16], ident_b[:]),
                   R=["TMP4b", "ident_b"], W=[("ps", pb)], sig=(i % 8 == 7))
            for hb in range(2):
                op("act", lambda e, hb=hb: e.activation(out=CT[:, hb * 1024:(hb + 1) * 1024], in_=psb(4 + hb)[0:16, :],
                                                        func=AF.Copy), R=[("ps", 4 + hb)], W=["CT"])
            return False

        def load_expert(l, e_):
            base = 3 * (e_ % 2)
            kb.dma("pool", ring(base).rearrange("p (k n) -> p k n", k=8),
                   w_exp_gate[l, e_].rearrange("(k p) n -> p k n", p=128), W=[("ring", base)], dkey=f"ring{base}")
            kb.dma("pool", ring(base + 1).rearrange("p (k n) -> p k n", k=8),
                   w_exp_up[l, e_].rearrange("(k p) n -> p k n", p=128), W=[("ring", base + 1)], dkey=f"ring{base + 1}")
            wd = ring(base + 2).rearrange("p (k n) -> p k n", k=4)
            kb.dma("pool", wd, w_exp_down[l, e_].rearrange("(k p) n -> p k n", p=128),
                   W=[("ring", base + 2)], dkey=f"ring{base + 2}")


        def moe_phase(l):
            CT = SM[0:16, 0:1024].bitcast(BF16)

            def scale_wd(e_):
                base = 3 * (e_ % 2)
                wd = ring(base + 2).rearrange("p (k n) -> p k n", k=4)
                op("pool", lambda e: e.tensor_tensor(wd, wd, MOD[:, 5, :].unsqueeze(1).to_broadcast([128, 4, 1024]),
                                                     op=ALU.mult), R=[("ring", base + 2), ("MOD", 5)], W=[("ring", base + 2)])

            def prep_piece(e2, tb):
                p2 = e2 % 2
                CB2 = (TMP4 if p2 == 0 else TMP4b)[:].bitcast(BF16)
                ck2 = "TMP4" if p2 == 0 else "TMP4b"
                if tb == 0:
                    op("dve", lambda e: e.tensor_copy(SELE[:, p2, :], ident_b[0:16, e2:e2 + 1].to_broadcast([16, 128])),
                       R=["ident_b"], W=[("SELE", p2)])
                op("pe", lambda e: e.matmul(ps[6][:], SELE[:, p2, :], CT[:, tb * 512:(tb + 1) * 512],
                                            start=True, stop=True), R=[("SELE", p2), "CT"], W=[("ps", 6)])
                op("act", lambda e: e.activation(out=CB2[:, tb * 512:(tb + 1) * 512], in_=ps[6][:], func=AF.Copy),
                   R=[("ps", 6)], W=[(ck2, tb)])

            gcnt = 0
            dcnt = 0
            for e_ in range(16):
                if e_ + 1 < 16:
                    load_expert(l, e_ + 1)
                base = 3 * (e_ % 2)
                WG = ring(base).rearrange("p (k n) -> p k n", k=8)
                WU = ring(base + 1).rearrange("p (k n) -> p k n", k=8)
                WD = ring(base + 2).rearrange("p (k n) -> p k n", k=4)
                es_ = e_ % 2
                CBC = (TMP4 if es_ == 0 else TMP4b)[:].bitcast(BF16)
                ckey = "TMP4" if es_ == 0 else "TMP4b"
                if e_ == 0:
                    for tb in range(4):
                        prep_piece(0, tb)
                for fc in range(4):
                    if fc == 2:
                        scale_wd(e_)
                    for tb in range(4):
                        g2 = gcnt % 2
                        gcnt += 1
                        pg, pu = g2, 2 + g2
                        hk = [("H2", i) for i in range(4 * tb, 4 * tb + 4)]
                        for k in range(8):
                            op("pe", lambda e, k=k: e.matmul(ps[pg][:], WG[:, k, fc * 128:(fc + 1) * 128],
                                                             H2T3[:, k, tb * 512:(tb + 1) * 512],
                                                             start=(k == 0), stop=(k == 7)),
                               R=[("ring", base)] + hk, W=[("ps", pg)], sig=(k == 7))
                        for k in range(8):
                            op("pe", lambda e, k=k: e.matmul(ps[pu][:], WU[:, k, fc * 128:(fc + 1) * 128],
                                                             H2T3[:, k, tb * 512:(tb + 1) * 512],
                                                             start=(k == 0), stop=(k == 7)),
                               R=[("ring", base + 1)] + hk, W=[("ps", pu)], sig=(k == 7))
                        SGt = SM[:, 1024 + 256 * g2:1280 + 256 * g2].bitcast(BF16)
                        Tt = SM[:, 1536 + 256 * g2:1792 + 256 * g2].bitcast(BF16)
                        op("act", lambda e: e.activation(out=SGt, in_=ps[pg][:], func=AF.Silu),
                           R=[("ps", pg)], W=[("SGt", g2)])
                        op("dve", lambda e: e.tensor_tensor(Tt, SGt, ps[pu][:], op=ALU.mult),
                           R=[("SGt", g2), ("ps", pu)], W=[("Tt", g2)])
                        op("pool", lambda e: e.tensor_tensor(ACTT[:, fc, tb * 512:(tb + 1) * 512], Tt,
                                                             CBC[:, tb * 512:(tb + 1) * 512], op=ALU.mult),
                           R=[("Tt", g2), (ckey, tb)], W=[("ACTT", fc, tb)])
                    if e_ + 1 < 16:
                        prep_piece(e_ + 1, fc)
                for i in range(NT):
                    for half in range(2):
                        pd = (4, 5, 7)[dcnt % 3]
                        dcnt += 1
                        for fc in range(4):
                            op("pe", lambda e, fc=fc: e.matmul(ps[pd][:], ACTT[:, fc, i * 128:(i + 1) * 128],
                                                               WD[:, fc, half * 512:(half + 1) * 512],
                                                               start=(fc == 0), stop=(fc == 3)),
                               R=[("ACTT", fc, i // 4), ("ring", base + 2)], W=[("ps", pd)], sig=(fc == 3))
                        xs_ = X[:, i, half * 512:(half + 1) * 512]
                        op("dve", lambda e, xs_=xs_: e.tensor_tensor(xs_, xs_, ps[pd][:], op=ALU.add),
                           R=[("ps", pd), ("X", i)], W=[("X", i)])

        def dump(ap, shape2d_cols, dt, keys):
            kb.dma("sp", dbg_d[:, 0:shape2d_cols], ap, R=keys, dkey="dbg")

        for l in range(depth):
            adaln(l)
            if debug is not None and debug[0] == f"mod{l}":
                dump(MOD[:].rearrange("p a n -> p (a n)"), 6 * D, BF16, [("MOD", j) for j in range(6)])
                break
            kb.barrier()
            layer_loads(l)
            norm_to_FT(l, 0, lambda i: FT3[:, :, i::16])
            if debug is not None and debug[0] == f"hT{l}":
                dump(FT[:], 8 * S, BF16, [("FT", i) for i in range(NT)])
                break
            u_proj(l)
            if debug is not None and debug[0] == f"ucm{l}":
                dump(UCM.rearrange("p j f -> p (j f)"), 16 * 512, BF16, [("UCM", j) for j in range(NT)])
                break
            kb.barrier()
            attention(l)
            if debug is not None and debug[0] == f"at{l}":
                dump(AT[:], 4 * S, BF16, [("AT", n) for n in range(NT)])
                break
            kb.barrier()
            if ssm_main(l):
                break
            if debug is not None and debug[0] == f"yg{l}":
                dump(UCM.rearrange("p j f -> p (j f)"), 16 * 512, BF16, [("UCM", j) for j in range(NT)])
                break
            kb.barrier()
            wout_load(l)
            glu_phase(l)
            if debug is not None and debug[0] == f"st{l}":
                dump(FT[:, 0:4 * S], 4 * S, BF16, [("ST", i) for i in range(NT)])
                break
            kb.barrier()
            wout_phase(l)
            if debug is not None and debug[0] == f"x1{l}":
                kb.barrier()
                dump(X[:].rearrange("p i d -> p (i d)"), NT * D, F32, [("X", i) for i in range(NT)])
                break
            kb.barrier()
            load_expert(l, 0)
            norm_to_FT(l, 1, lambda i: H2T3[:, :, i * 128:(i + 1) * 128], okey="H2", pbanks=(0, 1))
            if debug is not None and debug[0] == f"h2{l}":
                kb.barrier()
                dump(BIG[:, 4096:12288].bitcast(BF16), 8 * S, BF16, [("H2", i) for i in range(NT)])
                break
            kb.barrier()
            if router(l):
                break
            kb.barrier()
            moe_phase(l)
            kb.barrier()
            if debug is not None and debug[0] == f"x2{l}":
                dump(X[:].rearrange("p i d -> p (i d)"), NT * D, F32, [("X", i) for i in range(NT)])
                break

        if debug is None:
            y_v = y_d.rearrange("(c i) d -> c i d", i=NT)
            for q4 in range(4):
                kb.dma("sp", y_v[:, 4 * q4:4 * q4 + 4, :], X[:, 4 * q4:4 * q4 + 4, :],
                       R=[("X", i) for i in range(4 * q4, 4 * q4 + 4)], dkey=f"y{q4}")
        kb.final_wait("sp")
    return nc


_IN_NAMES = ["ada_w", "ada_b", "norm1_g", "w_in", "q_norm_g", "k_norm_g", "attn_sink", "lam_re", "lam_im",
             "ssm_b_re", "ssm_b_im", "ssm_c_re", "ssm_c_im", "ssm_d", "ssm_log_dt", "w_glu", "attn_out_g",
             "ssm_out_g", "w_out", "norm2_g", "w_router", "router_bias", "w_exp_gate", "w_exp_up", "w_exp_down"]


def make_in_maps(inputs, cores):
    maps = []
    shared = {k: np.ascontiguousarray(np.asarray(inputs[k], dtype=np.float32)) for k in _IN_NAMES}
    for b in cores:
        m = dict(shared)
        m["x"] = np.ascontiguousarray(np.asarray(inputs["x"][b], dtype=np.float32))
        m["c"] = np.ascontiguousarray(np.asarray(inputs["c"][b], dtype=np.float32).reshape(8, 128))
        m["positions"] = np.ascontiguousarray(np.asarray(inputs["positions"][b], dtype=np.int32))
        maps.append(m)
    return maps


def kernel(**inputs):
    nc = build()
    in_maps = make_in_maps(inputs, list(range(8)))
    res = run_bass_kernel_spmd(nc, in_maps, core_ids=list(range(8)))
    return np.stack([np.asarray(r["y"], dtype=np.float32) for r in res.results], axis=0)
```

```python
import math
import contextlib
import numpy as np
import concourse.bass as bass
import concourse.mybir as mybir
from concourse.bass_utils import run_bass_kernel_spmd

F32 = mybir.dt.float32
BF16 = mybir.dt.bfloat16
I32 = mybir.dt.int32
AF = mybir.ActivationFunctionType
ALU = mybir.AluOpType
AX = mybir.AxisListType

DEPTH = 2
S = 2048
D = 1024
NT = 16
EPS = 1e-6
NEG = -30000.0
TWO_PI = 2.0 * math.pi
import os
SELF_SYNC = set(os.environ.get("KSELF", "act,dve,pool").split(","))


class KB:
    def __init__(self, nc, es):
        self.nc = nc
        self.es = es
        self.E = {"pe": nc.tensor, "act": nc.scalar, "dve": nc.vector, "pool": nc.gpsimd, "sp": nc.sync}
        self.sems = {}
        self.val = {}
        self.seen = {e: {} for e in self.E}
        self.lastw = {}
        self.readers = {}
        for e in ("pe", "act", "dve", "pool"):
            self._sem(e)

    def _sem(self, name):
        if name not in self.sems:
            self.sems[name] = self.es.enter_context(self.nc.semaphore("s_" + name.replace(":", "_")))
            self.val[name] = 0
        return self.sems[name]

    def _deps(self, R, W):
        deps = {}

        def need(d):
            if d is None:
                return
            s, v = d
            if v > deps.get(s, 0):
                deps[s] = v

        for k in R:
            need(self.lastw.get(k))
        for k in W:
            need(self.lastw.get(k))
            for r in self.readers.get(k, ()):
                need(r)
        return deps

    def _wait(self, eng, deps):
        E = self.E[eng]
        for s, v in deps.items():
            if s == eng and (eng == "pe" or eng not in SELF_SYNC):
                continue
            if self.seen[eng].get(s, 0) >= v:
                continue
            E.wait_ge(self.sems[s], v)
            self.seen[eng][s] = v

    def _record(self, tok, R, W):
        for k in R:
            self.readers.setdefault(k, []).append(tok)
        for k in W:
            self.lastw[k] = tok
            self.readers[k] = []

    def op(self, eng, fn, R=(), W=(), sig=True):
        self._wait(eng, self._deps(R, W))
        inst = fn(self.E[eng])
        if sig:
            self.val[eng] += 1
            inst.then_inc(self.sems[eng], 1)
            tok = (eng, self.val[eng])
        else:
            tok = (eng, self.val[eng] + 1)
        self._record(tok, R, W)
        return inst

    def dma(self, q, out, in_, R=(), W=(), dkey=None, **kw):
        name = "d:" + dkey
        self._sem(name)
        self._wait(q, self._deps(R, W))
        inst = self.E[q].dma_start(out=out, in_=in_, **kw)
        self.val[name] += 16
        inst.then_inc(self.sems[name], 16)
        self._record((name, self.val[name]), R, W)
        return inst

    def barrier(self):
        for eng in self.E:
            deps = {s: v for s, v in self.val.items() if v > 0}
            self._wait(eng, deps)
        self.lastw.clear()
        self.readers.clear()

    def final_wait(self, eng="sp"):
        deps = {s: v for s, v in self.val.items() if v > 0 and s.startswith("d:")}
        self._wait(eng, deps)


def run_rr(gens, width, lag=0):
    it = iter(gens)
    active = []
    steps_newest = 10 ** 9
    done = False
    while True:
        if not done and len(active) < width and steps_newest >= lag:
            try:
                active.append(next(it))
                steps_newest = 0
            except StopIteration:
                done = True
        if not active:
            if done:
                break
            steps_newest = 10 ** 9
            continue
        for g in list(active):
            try:
                next(g)
            except StopIteration:
                active.remove(g)
        steps_newest += 1


def build(depth=DEPTH, debug=None):
    nc = bass.Bass("TRN2", target_bir_lowering=False)

    def din(name, shape, dt=F32):
        return nc.dram_tensor(name, list(shape), dt, kind="ExternalInput").ap()

    x_d = din("x", [S, D])
    c_d = din("c", [8, 128])
    pos_d = din("positions", [S], I32)
    ada_w = din("ada_w", [DEPTH, D, 6 * D])
    ada_b = din("ada_b", [DEPTH, 6 * D])
    norm1_g = din("norm1_g", [DEPTH, D])
    w_in = din("w_in", [DEPTH, D, 1280])
    q_norm_g = din("q_norm_g", [DEPTH, 64])
    k_norm_g = din("k_norm_g", [DEPTH, 64])
    attn_sink = din("attn_sink", [DEPTH, 8])
    lam_re = din("lam_re", [DEPTH, 32, 64])
    lam_im = din("lam_im", [DEPTH, 32, 64])
    ssm_b_re = din("ssm_b_re", [DEPTH, 32, 64, 16])
    ssm_b_im = din("ssm_b_im", [DEPTH, 32, 64, 16])
    ssm_c_re = din("ssm_c_re", [DEPTH, 32, 16, 64])
    ssm_c_im = din("ssm_c_im", [DEPTH, 32, 16, 64])
    ssm_d = din("ssm_d", [DEPTH, 512])
    ssm_log_dt = din("ssm_log_dt", [DEPTH, 32])
    w_glu = din("w_glu", [DEPTH, 512, 512])
    attn_out_g = din("attn_out_g", [DEPTH, 512])
    ssm_out_g = din("ssm_out_g", [DEPTH, 512])
    w_out = din("w_out", [DEPTH, D, D])
    norm2_g = din("norm2_g", [DEPTH, D])
    w_router = din("w_router", [D, 16])
    router_bias = din("router_bias", [16])
    w_exp_gate = din("w_exp_gate", [DEPTH, 16, D, 512])
    w_exp_up = din("w_exp_up", [DEPTH, 16, D, 512])
    w_exp_down = din("w_exp_down", [DEPTH, 16, 512, D])
    y_d = nc.dram_tensor("y", [S, D], F32, kind="ExternalOutput").ap()
    dbg_d = None
    if debug is not None:
        dbg_d = nc.dram_tensor("dbg", [128, debug[1]], debug[2], kind="ExternalOutput").ap()

    with contextlib.ExitStack() as es:
        kb = KB(nc, es)
        op = kb.op

        def sb(name, shape, dt):
            return es.enter_context(nc.sbuf_tensor(name, list(shape), dt))

        X = sb("X", [128, NT, D], F32)
        FT = sb("FT", [128, 8 * S], BF16)
        AT = sb("AT", [128, 4 * S], BF16)
        BIG = sb("BIG", [128, 12288], F32)
        MOD = sb("MOD", [128, 6, D], BF16)
        SM = sb("SM", [128, 2560], F32)
        ident_f = sb("ident_f", [128, 128], F32)
        ident_b = sb("ident_b", [128, 128], BF16)
        colI = sb("colI", [128, 128], F32)
        rowI = sb("rowI", [128, 128], F32)
        MBp = sb("MBp", [128, 4, 128], BF16)
        MBc = sb("MBc", [128, 4, 128], BF16)
        COS = sb("COS", [128, NT, 32], F32)
        SIN = sb("SIN", [128, NT, 32], F32)
        SCrep = sb("SCrep", [128, 8, 128], BF16)
        TMP4 = sb("TMP4", [128, 1024], F32)
        TMP4b = sb("TMP4b", [128, 1024], F32)
        SS = sb("SS", [128, 64], F32)
        NHALF = sb("NHALF", [128, 64], F32)
        GQK = sb("GQK", [128, 10, 64], BF16)
        ESINK = sb("ESINK", [128, 8], F32)
        GROW = sb("GROW", [128, 8], F32)
        MASKM = sb("MASKM", [128, 256], BF16)
        DIAG0 = sb("DIAG0", [128, 256], BF16)
        REP16 = sb("REP16", [16, 128], F32)
        SGN = sb("SGN", [128, 2], F32)
        DCOL = sb("DCOL", [128, 32], F32)
        LAB = sb("LAB", [128, 2, 2, 2, 16], F32)
        WR = sb("WR", [128, 8, 16], BF16)
        RB = sb("RB", [128, 16], F32)
        SELE = sb("SELE", [16, 2, 128], BF16)

        PSALL = es.enter_context(nc.psum_tensor("psall", [128, 4096], F32))
        ps = [PSALL[:, 512 * i:512 * (i + 1)] for i in range(8)]

        def psb(i):
            return ps[i].bitcast(BF16)

        def big_bf(w0, w1):
            return BIG[:, w0:w1].bitcast(BF16)

        FT3 = FT[:].rearrange("p (k t) -> p k t", k=8)
        AT3 = AT[:].rearrange("p (k t) -> p k t", k=4)

        op("pool", lambda e: e.iota(colI[:], [[1, 128]], base=0, channel_multiplier=0,
                                    allow_small_or_imprecise_dtypes=True), W=["colI"])
        op("pool", lambda e: e.iota(rowI[:], [[0, 128]], base=0, channel_multiplier=1,
                                    allow_small_or_imprecise_dtypes=True), W=["rowI"])
        op("pool", lambda e: e.memset(NHALF[:], -0.5), W=["NHALF"])
        op("dve", lambda e: e.tensor_tensor(ident_f[:], colI[:], rowI[:], op=ALU.is_equal),
           R=["colI", "rowI"], W=["ident_f"])
        op("dve", lambda e: e.tensor_copy(ident_b[:], ident_f[:]), R=["ident_f"], W=["ident_b"])
        op("dve", lambda e: e.tensor_tensor(TMP4[:, 0:128], colI[:], rowI[:], op=ALU.is_ge),
           R=["colI", "rowI"], W=["TMP4"])
        op("dve", lambda e: e.tensor_scalar(MBp[:], TMP4[:, 0:128].unsqueeze(1).to_broadcast([128, 4, 128]),
                                            NEG, None, op0=ALU.mult), R=["TMP4"], W=["MBp"])
        op("dve", lambda e: e.tensor_tensor(TMP4[:, 128:256], colI[:], rowI[:], op=ALU.is_lt),
           R=["colI", "rowI"], W=["TMP4"])
        op("dve", lambda e: e.tensor_scalar(MBc[:], TMP4[:, 128:256].unsqueeze(1).to_broadcast([128, 4, 128]),
                                            NEG, None, op0=ALU.mult), R=["TMP4"], W=["MBc"])


        def _ssm_consts():
            PI_ = SM[:, 340:341].bitcast(I32)
            JJF = SM[:, 341:342]
            HPF = SM[:, 342:343]
            TI = SM[:, 344:345].bitcast(I32)
            IIF = TMP4[:, 0:256]
            HF = TMP4[:, 256:512]
            DLF = TMP4[:, 512:768]
            E1 = TMP4b[:, 0:256]
            E2 = TMP4b[:, 256:512]
            op("pool", lambda e: e.iota(PI_, [[0, 1]], base=0, channel_multiplier=1), W=["c_pi"])
            op("dve", lambda e: e.tensor_single_scalar(TI, PI_, 4, op=ALU.arith_shift_right), R=["c_pi"], W=["c_ti"])
            op("dve", lambda e: e.tensor_copy(JJF, TI), R=["c_ti"], W=["c_jj"])
            op("dve", lambda e: e.tensor_single_scalar(TI, PI_, 15, op=ALU.bitwise_and), R=["c_pi", "c_jj"], W=["c_ti"])
            op("dve", lambda e: e.tensor_copy(HPF, TI), R=["c_ti"], W=["c_hp"])
            op("pool", lambda e: e.iota(IIF.rearrange("p (a b c) -> p a b c", a=2, b=8), [[0, 2], [1, 8], [0, 16]],
                                        base=0, channel_multiplier=0, allow_small_or_imprecise_dtypes=True),
               W=["TMP4"])
            op("pool", lambda e: e.iota(HF.rearrange("p (a b c) -> p a b c", a=2, b=8), [[0, 2], [0, 8], [1, 16]],
                                        base=0, channel_multiplier=0, allow_small_or_imprecise_dtypes=True),
               W=["TMP4"])
            op("pool", lambda e: e.iota(DLF.rearrange("p (a b c) -> p a b c", a=2, b=8), [[1, 2], [0, 8], [0, 16]],
                                        base=0, channel_multiplier=0, allow_small_or_imprecise_dtypes=True),
               W=["TMP4"])
            op("dve", lambda e: e.tensor_scalar(E1, IIF, JJF, None, op0=ALU.is_ge), R=["TMP4", "c_jj"], W=["TMP4b"])
            op("dve", lambda e: e.tensor_tensor(MASKM[:], E1, DLF, op=ALU.max), R=["TMP4b", "TMP4"], W=["MASKM"])
            op("dve", lambda e: e.tensor_scalar(E1, IIF, JJF, None, op0=ALU.is_equal), R=["TMP4", "c_jj", "MASKM"],
               W=["TMP4b"])
            op("dve", lambda e: e.tensor_scalar(E2, HF, HPF, None, op0=ALU.is_equal), R=["TMP4", "c_hp"], W=["TMP4b"])
            op("dve", lambda e: e.tensor_tensor(E1, E1, E2, op=ALU.mult), R=["TMP4b"], W=["TMP4b"])
            op("dve", lambda e: e.tensor_scalar(E2, DLF, -1.0, 1.0, op0=ALU.mult, op1=ALU.add), R=["TMP4"], W=["TMP4b"])
            op("dve", lambda e: e.tensor_tensor(DIAG0[:], E1, E2, op=ALU.mult), R=["TMP4b"], W=["DIAG0"])
            op("dve", lambda e: e.tensor_copy(REP16[:].rearrange("k (j h) -> k j h", j=8),
                                              ident_f[0:16, 0:16].unsqueeze(1).to_broadcast([16, 8, 16])),
               R=["ident_f"], W=["REP16"])
            op("pool", lambda e: e.memset(SGN[0:64, 0:1], 1.0), W=["SGN"])
            op("pool", lambda e: e.memset(SGN[64:128, 0:1], -1.0), W=["SGN"])
            op("pool", lambda e: e.memset(SGN[0:64, 1:2], -1.0), W=["SGN"])
            op("pool", lambda e: e.memset(SGN[64:128, 1:2], 1.0), W=["SGN"])

        _ssm_consts()
        kb.dma("pool", WR[:], w_router.rearrange("(k p) e -> p k e", p=128), W=["WR"], dkey="wr")
        kb.dma("sp", RB[:], router_bias.rearrange("(o e) -> o e", o=1).partition_broadcast(128).rearrange("p o e -> p (o e)"),
               W=["RB"], dkey="rb")

        x_v = x_d.rearrange("(c i) d -> c i d", i=NT)
        for q4 in range(4):
            kb.dma("sp", X[:, 4 * q4:4 * q4 + 4, :], x_v[:, 4 * q4:4 * q4 + 4, :],
                   W=[("X", i) for i in range(4 * q4, 4 * q4 + 4)], dkey=f"x{q4}")

        POSI = SM[:, 0:16].bitcast(I32)
        with nc.allow_non_contiguous_dma(reason="tiny position load"):
            kb.dma("sp", POSI, pos_d.rearrange("(n p) -> p n", p=128), W=["POSI"], dkey="pos")
        POSF = SM[:, 16:32]
        FREQ = SM[:, 32:64]
        ANG = TMP4[:, 0:512].rearrange("p (n k) -> p n k", k=32)
        ANG2 = TMP4[:, 512:1024].rearrange("p (n k) -> p n k", k=32)
        KI = TMP4b[:, 0:512].bitcast(I32).rearrange("p (n k) -> p n k", k=32)
        KF = TMP4b[:, 512:1024].rearrange("p (n k) -> p n k", k=32)
        op("dve", lambda e: e.tensor_copy(POSF, POSI), R=["POSI"], W=["POSF"])
        op("pool", lambda e: e.iota(FREQ, [[1, 32]], base=0, channel_multiplier=0,
                                    allow_small_or_imprecise_dtypes=True), W=["FREQ"])
        op("act", lambda e: e.activation(out=FREQ, in_=FREQ, func=AF.Exp, scale=-math.log(10000.0) / 32.0),
           R=["FREQ"], W=["FREQ"])
        op("dve", lambda e: e.tensor_tensor(ANG, POSF.unsqueeze(2).to_broadcast([128, NT, 32]),
                                            FREQ.unsqueeze(1).to_broadcast([128, NT, 32]), op=ALU.mult),
           R=["POSF", "FREQ"], W=["TMP4"])

        def sin_table(dst, src_ang, shift, keyname):
            op("dve", lambda e: e.tensor_scalar(ANG2, src_ang, shift, None, op0=ALU.add), R=["TMP4"], W=["TMP4"])
            op("dve", lambda e: e.tensor_scalar(KF, ANG2, 1.0 / TWO_PI, None, op0=ALU.mult), R=["TMP4"], W=["TMP4b"])
            op("dve", lambda e: e.tensor_copy(KI, KF), R=["TMP4b"], W=["TMP4b"])
            op("dve", lambda e: e.tensor_copy(KF, KI), R=["TMP4b"], W=["TMP4b"])
            op("dve", lambda e: e.scalar_tensor_tensor(ANG2, KF, -TWO_PI, ANG2, op0=ALU.mult, op1=ALU.add),
               R=["TMP4b", "TMP4"], W=["TMP4"])
            op("dve", lambda e: e.tensor_scalar(ANG2, ANG2, -3.1415925, 3.1415925, op0=ALU.max, op1=ALU.min),
               R=["TMP4"], W=["TMP4"])
            op("act", lambda e: e.activation(out=dst, in_=ANG2, func=AF.Sin), R=["TMP4"], W=[keyname])

        sin_table(SIN[:], ANG, 0.0, "SIN")
        sin_table(COS[:], ANG, math.pi / 2.0, "COS")

        C8 = SM[0:8, 64:192]
        kb.dma("sp", C8, c_d, W=["C8"], dkey="c8")
        op("pe", lambda e: e.matmul(ps[0][:, 0:8], C8, ident_f[0:8, 0:8], start=True, stop=True),
           R=["C8", "ident_f"], W=[("ps", 0)])
        SCT = SM[:, 192:200]
        op("act", lambda e: e.activation(out=SCT, in_=ps[0][:, 0:8], func=AF.Silu), R=[("ps", 0)], W=["SCT"])
        op("dve", lambda e: e.tensor_copy(SCrep[:], SCT.unsqueeze(2).to_broadcast([128, 8, 128])),
           R=["SCT"], W=["SCrep"])

        def ring_slot(s):
            return big_bf(2048 * s, 2048 * (s + 1))

        def adaln(l):
            for blk in range(6):
                if blk in (1, 4):
                    ng = (norm1_g if blk == 1 else norm2_g)
                    kb.dma("sp", TMP4b[:], ng[l:l + 1, :].partition_broadcast(128).rearrange("p o n -> p (o n)"),
                           W=["TMP4b"], dkey="ngbc")
                BIASB = TMP4[:] if blk % 2 == 0 else SM[:, 1024:2048]
                bkey = "TMP4" if blk % 2 == 0 else "SMbias"
                kb.dma("sp", BIASB, ada_b[l:l + 1, blk * D:(blk + 1) * D].partition_broadcast(128)
                       .rearrange("p o n -> p (o n)"), W=[bkey], dkey="adb%d" % (blk % 2))
                for half in range(2):
                    s = (blk * 2 + half) % 4
                    slot = ring_slot(s).rearrange("p (k n) -> p k n", k=8)
                    col0 = blk * D + half * 512
                    kb.dma("pool", slot, ada_w[l, :, col0:col0 + 512].rearrange("(k p) n -> p k n", p=128),
                           W=[("ring", s)], dkey=f"ring{s}")
                    pb = 2 + (s % 2)
                    for k in range(8):
                        op("pe", lambda e, k=k: e.matmul(ps[pb][:], SCrep[:, k, :], slot[:, k, :],
                                                         start=(k == 0), stop=(k == 7)),
                           R=["SCrep", ("ring", s)], W=[("ps", pb)], sig=(k == 7))
                    dst = MOD[:, blk, half * 512:(half + 1) * 512]
                    bias = BIASB[:, half * 512:(half + 1) * 512]
                    if blk in (1, 4):
                        tmp = SM[:, 512:1024]
                        op("dve", lambda e: e.tensor_tensor(tmp, ps[pb][:], bias, op=ALU.add),
                           R=[("ps", pb), bkey], W=["SMtmp"])
                        op("dve", lambda e: e.scalar_tensor_tensor(dst, tmp, 1.0, TMP4b[:, half * 512:(half + 1) * 512],
                                                                   op0=ALU.add, op1=ALU.mult),
                           R=["SMtmp", "TMP4b"], W=[("MOD", blk)])
                    else:
                        op("dve", lambda e: e.tensor_tensor(dst, ps[pb][:], bias, op=ALU.add),
                           R=[("ps", pb), bkey], W=[("MOD", blk)])

        def norm_to_FT(l, which, pos_of_tile, okey="FT", pbanks=(0, 1)):
            a_i, b_i = (1, 0) if which == 0 else (4, 3)
            junk = SM[:, 2048:2560].bitcast(BF16)
            for i in range(NT):
                op("act", lambda e, i=i: e.activation(out=junk, in_=X[:, i, :], func=AF.Square,
                                                      accum_out=SS[:, i:i + 1]),
                   R=[("X", i)], W=["junk", ("SS", i)])
            op("dve", lambda e: e.tensor_scalar(SS[:, 16:32], SS[:, 0:16], 1.0 / D, EPS, op0=ALU.mult, op1=ALU.add),
               R=[("SS", i) for i in range(NT)], W=["SSb"])
            op("pool", lambda e: e.tensor_tensor(SS[:, 32:48], SS[:, 16:32], NHALF[:, 0:16], op=ALU.pow),
               R=["SSb", "NHALF"], W=["RSTD"])
            def tile(i):
                xs = i % 2
                t32 = TMP4[:] if xs == 0 else TMP4b[:]
                tkey = "TMP4" if xs == 0 else "TMP4b"
                xn = SM[:, 1024 + 512 * xs:1024 + 512 * (xs + 1)].bitcast(BF16)
                op("dve", lambda e, i=i: e.scalar_tensor_tensor(t32, X[:, i, :], SS[:, 32 + i:33 + i], MOD[:, a_i, :],
                                                                op0=ALU.mult, op1=ALU.mult),
                   R=[("X", i), "RSTD", ("MOD", a_i)], W=[tkey])
                yield
                op("dve", lambda e: e.tensor_tensor(xn, t32, MOD[:, b_i, :], op=ALU.add),
                   R=[tkey, ("MOD", b_i)], W=[("xn", xs)])
                yield
                pb = pbanks[xs]
                for k in range(8):
                    op("pe", lambda e, k=k: e.transpose(psb(pb)[:, k * 128:(k + 1) * 128],
                                                        xn[:, k * 128:(k + 1) * 128], ident_b[:]),
                       R=[("xn", xs), "ident_b"], W=[("ps", pb)], sig=(k == 7))
                    yield
                dst = pos_of_tile(i)
                op("act", lambda e: e.activation(out=dst, in_=psb(pb).rearrange("p (k c) -> p k c", k=8),
                                                 func=AF.Copy),
                   R=[("ps", pb)], W=[(okey, i)])
                yield

            run_rr([tile(i) for i in range(NT)], 2, 5)

        WIN3 = big_bf(0, 5120).rearrange("p (k n) -> p k n", k=8)
        UCM = big_bf(8192, 12288).rearrange("p (j f) -> p j f", j=16)
        UCMg = big_bf(8192, 12288).rearrange("p (g j h) -> p g j h", g=32, j=16)

        def layer_loads(l):
            kb.dma("pool", WIN3, w_in[l].rearrange("(k p) n -> p k n", p=128), W=["WIN"], dkey="win")
            QG = SM[:, 200:264]
            KG = SM[:, 264:328]
            SK = SM[:, 328:336]
            kb.dma("sp", QG, q_norm_g[l:l + 1, :].partition_broadcast(128).rearrange("p o n -> p (o n)"),
                   W=["QG"], dkey="qg")
            kb.dma("sp", KG, k_norm_g[l:l + 1, :].partition_broadcast(128).rearrange("p o n -> p (o n)"),
                   W=["KG"], dkey="kg")
            kb.dma("sp", SK, attn_sink[l:l + 1, :].partition_broadcast(128).rearrange("p o n -> p (o n)"),
                   W=["SK"], dkey="sk")
            op("dve", lambda e: e.tensor_copy(GQK[:, 0:8, :], QG.unsqueeze(1).to_broadcast([128, 8, 64])),
               R=["QG"], W=["GQK"])
            op("dve", lambda e: e.tensor_copy(GQK[:, 8:10, :], KG.unsqueeze(1).to_broadcast([128, 2, 64])),
               R=["KG"], W=["GQK"])
            op("act", lambda e: e.activation(out=ESINK[:], in_=SK, func=AF.Exp), R=["SK"], W=["ESINK"])
            with nc.allow_non_contiguous_dma(reason="tiny gain vectors"):
                kb.dma("sp", GROW[:, 0:4], attn_out_g[l].rearrange("(k p) -> p k", p=128), W=["GROW"], dkey="grow")
                kb.dma("sp", GROW[:, 4:8], ssm_out_g[l].rearrange("(k p) -> p k", p=128), W=["GROW"], dkey="grow")

        def u_proj(l):
            for j in range(NT):
                pb = 2 + (j % 2)
                for k in range(8):
                    op("pe", lambda e, k=k: e.matmul(ps[pb][:], FT3[:, k, j::16], WIN3[:, k, 768:1280],
                                                     start=(k == 0), stop=(k == 7)),
                       R=[("FT", j), "WIN"], W=[("ps", pb)], sig=(k == 7))
                eng = "act" if j % 2 == 0 else "dve"
                if eng == "act":
                    op("act", lambda e: e.activation(out=UCMg[:, :, j, :],
                                                     in_=ps[pb][:].rearrange("p (g h) -> p g h", g=32), func=AF.Copy),
                       R=[("ps", pb)], W=[("UCM", j)])
                else:
                    op("dve", lambda e: e.tensor_copy(UCMg[:, :, j, :], ps[pb][:].rearrange("p (g h) -> p g h", g=32)),
                       R=[("ps", pb)], W=[("UCM", j)])

        def attention(l):
            KT = big_bf(5120, 7168)[0:64, :].rearrange("p (n h t) -> p n h t", n=16, h=2)
            PT = [big_bf(7168 + 256 * j, 7168 + 256 * (j + 1)) for j in range(4)]
            QK = SM[:, 0:640]
            T1 = SM[:, 640:1280]
            T2 = SM[:, 1280:1920]
            QKN = SM[:, 1920:2240].bitcast(BF16)
            ST10 = SM[:, 2240:2250]
            RS10 = SM[:, 2250:2260]
            DEN = SM[:, 2260:2268]
            RDEN = SM[:, 2268:2276]
            ASS = SM[:, 2276:2277]
            ARS = SM[:, 2278:2279]
            QT = [TMP4[0:64, 512 * j:512 * (j + 1)].bitcast(BF16) for j in range(2)]
            ATT = TMP4b[:, 0:512]
            AN = TMP4b[:, 512:768].bitcast(BF16)
            VA = [TMP4b[:, 768 + 66 * j:768 + 66 * (j + 1)].bitcast(BF16).rearrange("p (h d) -> p h d", h=2)
                  for j in range(3)]
            ALLFT = [("FT", i) for i in range(NT)]
            for j in range(3):
                op("pool", lambda e, j=j: e.memset(VA[j][:, :, 64:66], 1.0), W=[("VA1", j)])
            T1v = T1.rearrange("p (h t d) -> p h t d", h=10, t=2)
            T2v = T2.rearrange("p (h t d) -> p h t d", h=10, t=2)
            def front(n):
                qs = n % 2
                for k in range(8):
                    op("pe", lambda e, k=k: e.matmul(ps[4][:], FT3[:, k, 128 * n:128 * n + 128], WIN3[:, k, 0:512],
                                                     start=(k == 0), stop=(k == 7)),
                       R=ALLFT + ["WIN"], W=[("ps", 4)], sig=(k == 7))
                    yield
                for k in range(8):
                    op("pe", lambda e, k=k: e.matmul(ps[5][:, 0:256], FT3[:, k, 128 * n:128 * n + 128],
                                                     WIN3[:, k, 512:768], start=(k == 0), stop=(k == 7)),
                       R=ALLFT + ["WIN"], W=[("ps", 5)], sig=(k == 7))
                    yield
                op("act", lambda e: e.activation(out=QK[:, 0:512], in_=ps[4][:], func=AF.Copy),
                   R=[("ps", 4)], W=["QK"])
                yield
                op("act", lambda e: e.activation(out=QK[:, 512:640], in_=ps[5][:, 0:128], func=AF.Copy),
                   R=[("ps", 5)], W=["QK"])
                yield
                op("act", lambda e: e.activation(out=VA[n % 3][:, :, 0:64],
                                                 in_=ps[5][:, 128:256].rearrange("p (h d) -> p h d", h=2),
                                                 func=AF.Copy),
                   R=[("ps", 5)], W=[("VA", n % 3)])
                yield
                junkq = T2[:, 0:32].bitcast(BF16)
                for h in range(10):
                    op("act", lambda e, h=h: e.activation(out=junkq, in_=QK[:, h * 64:(h + 1) * 64], func=AF.Square,
                                                          accum_out=ST10[:, h:h + 1]), R=["QK"], W=["T2", ("ST10", h)])
                    yield
                op("pool", lambda e: e.tensor_scalar(ST10, ST10, 1.0 / 64.0, EPS, op0=ALU.mult, op1=ALU.add),
                   R=[("ST10", h) for h in range(10)], W=["ST10"])
                yield
                op("pool", lambda e: e.tensor_tensor(RS10, ST10, NHALF[:, 0:10], op=ALU.pow),
                   R=["ST10", "NHALF"], W=["RS10"])
                yield
                op("pool", lambda e: e.tensor_tensor(T1, QK, GQK[:].rearrange("p h d -> p (h d)"), op=ALU.mult),
                   R=["QK", "GQK"], W=["T1"])
                yield
                sin_b = SIN[:, n, :].unsqueeze(1).to_broadcast([128, 10, 32])
                cos_b = COS[:, n, :].unsqueeze(1).unsqueeze(1).to_broadcast([128, 10, 2, 32])
                op("pool", lambda e: e.tensor_tensor(T2v[:, :, 0, :], T1v[:, :, 1, :], sin_b, op=ALU.mult),
                   R=["T1", "SIN"], W=["T2"])
                yield
                op("pool", lambda e: e.tensor_tensor(T2v[:, :, 1, :], T1v[:, :, 0, :], sin_b, op=ALU.mult),
                   R=["T1", "SIN"], W=["T2"])
                yield
                op("pool", lambda e: e.tensor_tensor(T1v, T1v, cos_b, op=ALU.mult), R=["T1", "COS", "T2"], W=["T1"])
                yield
                op("pool", lambda e: e.tensor_tensor(T1v[:, :, 0, :], T1v[:, :, 0, :], T2v[:, :, 0, :],
                                                     op=ALU.subtract), R=["T1", "T2"], W=["T1"])
                yield
                op("pool", lambda e: e.tensor_tensor(T1v[:, :, 1, :], T1v[:, :, 1, :], T2v[:, :, 1, :],
                                                     op=ALU.add), R=["T1", "T2"], W=["T1"])
                yield
                op("pool", lambda e: e.tensor_tensor(QKN.rearrange("p (h d) -> p h d", h=10),
                                                     T1.rearrange("p (h d) -> p h d", h=10),
                                                     RS10.unsqueeze(2).to_broadcast([128, 10, 64]), op=ALU.mult),
                   R=["T1", "RS10"], W=["QKN"])
                yield
                for h in range(8):
                    op("pe", lambda e, h=h: e.transpose(psb(0)[0:64, h * 128:(h + 1) * 128],
                                                        QKN[:, h * 64:(h + 1) * 64], ident_b[:]),
                       R=["QKN", "ident_b"], W=[("ps", 0)], sig=(h == 7))
                    yield
                for h in range(2):
                    op("pe", lambda e, h=h: e.transpose(psb(0)[64:128, h * 128:(h + 1) * 128],
                                                        QKN[:, 512 + h * 64:512 + (h + 1) * 64], ident_b[:]),
                       R=["QKN", "ident_b"], W=[("ps", 0)], sig=(h == 1))
                    yield
                op("act", lambda e: e.activation(out=QT[qs], in_=psb(0)[0:64, :], func=AF.Copy),
                   R=[("ps", 0)], W=[("QT", qs)])
                yield
                op("act", lambda e: e.activation(out=KT[:, n, :, :],
                                                 in_=psb(0)[64:128, 0:256].rearrange("p (h t) -> p h t", h=2),
                                                 func=AF.Copy),
                   R=[("ps", 0)], W=[("KT", n)])
                yield

            def back(n):
                qs = n % 2
                QT3 = QT[qs].rearrange("p (h t) -> p h t", h=8)
                halves = ([(n - 1, MBp)] if n > 0 else []) + [(n, MBc)]
                for kvh in range(2):
                    for hi, (nb, MB) in enumerate(halves):
                        cidx = 2 * kvh + hi
                        sbk = 2 + (cidx % 2)
                        pj = cidx % 4
                        op("pe", lambda e: e.matmul(ps[sbk][:], KT[:, nb, kvh, :],
                                                    QT3[:, 4 * kvh:4 * kvh + 4, :], start=True, stop=False),
                           R=[("KT", nb), ("QT", qs)], W=[("ps", sbk)], sig=False)
                        yield
                        op("pe", lambda e: e.matmul(ps[sbk][:], ident_b[:], MB[:].rearrange("p h q -> p (h q)"),
                                                    start=False, stop=True),
                           R=["ident_b", "MB"], W=[("ps", sbk)])
                        yield
                        op("act", lambda e: e.activation(out=PT[pj], in_=ps[sbk][:], func=AF.Exp, scale=0.125),
                           R=[("ps", sbk)], W=[("PT", pj)])
                        yield
                        for h in range(4):
                            op("pe", lambda e, h=h: e.matmul(ps[6 + kvh][:, h * 65:h * 65 + 65],
                                                             PT[pj][:, h * 128:(h + 1) * 128],
                                                             VA[nb % 3][:, kvh, 0:65],
                                                             start=(hi == 0 and h == 0), stop=(hi == len(halves) - 1 and h == 3),
                                                             skip_group_check=True),
                               R=[("PT", pj), ("VA", nb % 3), ("VA1", nb % 3)], W=[("ps", 6 + kvh)], sig=(h == 3))
                            yield
                pv2 = PSALL[:, 3072:4096].rearrange("p (b x) -> p b x", b=2)[:, :, 0:260].rearrange(
                    "p b (h d) -> p b h d", h=4)
                k67 = [("ps", 6), ("ps", 7)]
                op("dve", lambda e: e.tensor_tensor(DEN.rearrange("p (b h o) -> p b h o", b=2, o=1), pv2[:, :, :, 64:65],
                                                    ESINK[:].rearrange("p (b h o) -> p b h o", b=2, o=1), op=ALU.add),
                   R=k67 + ["ESINK"], W=["DEN"])
                yield
                op("dve", lambda e: e.reciprocal(RDEN, DEN), R=["DEN"], W=["RDEN"])
                yield
                op("dve", lambda e: e.tensor_tensor(
                    ATT.rearrange("p (b h d) -> p b h d", b=2, h=4), pv2[:, :, :, 0:64],
                    RDEN.rearrange("p (b h) -> p b h", b=2).unsqueeze(3).to_broadcast([128, 2, 4, 64]), op=ALU.mult),
                   R=k67 + ["RDEN"], W=["ATT"])
                yield
                op("dve", lambda e: e.scalar_tensor_tensor(AN, ATT, 1.0, ATT, op0=ALU.mult, op1=ALU.mult, accum_out=ASS),
                   R=["ATT"], W=["AN", "ASS"])
                yield
                op("dve", lambda e: e.tensor_scalar(ASS, ASS, 1.0 / 512.0, EPS, op0=ALU.mult, op1=ALU.add),
                   R=["ASS"], W=["ASS"])
                yield
                op("pool", lambda e: e.tensor_tensor(ARS, ASS, NHALF[:, 0:1], op=ALU.pow),
                   R=["ASS", "NHALF"], W=["ARS"])
                yield
                op("dve", lambda e: e.tensor_scalar(AN, ATT, ARS, None, op0=ALU.mult), R=["ATT", "ARS"], W=["AN"])
                yield
                for k in range(4):
                    op("pe", lambda e, k=k: e.transpose(psb(1)[:, 512 + k * 128:512 + (k + 1) * 128],
                                                        AN[:, k * 128:(k + 1) * 128], ident_b[:]),
                       R=["AN", "ident_b"], W=[("ps", 1)], sig=(k == 3))
                    yield
                op("act", lambda e: e.activation(out=AT3[:, :, 128 * n:128 * n + 128],
                                                 in_=psb(1)[:, 512:1024].rearrange("p (k t) -> p k t", k=4),
                                                 func=AF.Copy),
                   R=[("ps", 1)], W=[("AT", n)])
                yield

            run_rr([front(0)], 1)
            for n in range(NT):
                gs = [back(n)] + ([front(n + 1)] if n + 1 < NT else [])
                run_rr(gs, 2)

        FTf = FT[:].bitcast(F32)
        BZCL = big_bf(0, 4096).rearrange("p (g a m) -> p g a m", g=32, a=2)
        MW = big_bf(4096, 8192).rearrange("p (g a m) -> p g a m", g=32, a=2)
        UT = FT[:, 0:8192].rearrange("p (g a c) -> p g a c", g=32, a=2)
        ZS = FTf[:, 4096:8192].rearrange("p (r g c) -> p r g c", r=2, g=16)
        XSB = SM[:, 0:2048].bitcast(BF16).rearrange("p (g c) -> p g c", g=32)
        S34 = TMP4b[:].rearrange("p (gh t g h) -> p gh t g h", gh=2, t=2, g=16)

        def sincos(dst_s, dst_c, ang, t_a, t_k, t_ki, keys_ang, key_a, key_k, wkey):
            for dst, shift in ((dst_c, math.pi / 2.0), (dst_s, 0.0)):
                op("dve", lambda e: e.tensor_scalar(t_a, ang, shift, None, op0=ALU.add), R=keys_ang, W=[key_a])
                op("dve", lambda e: e.tensor_scalar(t_k, t_a, 1.0 / TWO_PI, None, op0=ALU.mult), R=[key_a], W=[key_k])
                op("dve", lambda e: e.tensor_copy(t_ki, t_k), R=[key_k], W=[key_k])
                op("dve", lambda e: e.tensor_copy(t_k, t_ki), R=[key_k], W=[key_k])
                op("dve", lambda e: e.scalar_tensor_tensor(t_a, t_k, -TWO_PI, t_a, op0=ALU.mult, op1=ALU.add),
                   R=[key_k, key_a], W=[key_a])
                op("dve", lambda e: e.tensor_scalar(t_a, t_a, -3.1415925, 3.1415925, op0=ALU.max, op1=ALU.min),
                   R=[key_a], W=[key_a])
                op("act", lambda e, dst=dst: e.activation(out=dst, in_=t_a, func=AF.Sin), R=[key_a], W=[wkey])

        def ssm_tables(l, gh, taus, PC, PS, tmpE, tmpA, tmpK, AB, ADT, ldsm):
            T = sum(c for _, _, c in taus)
            g0 = 16 * gh
            LL = ldsm[0:16, 0:256]
            kb.dma("sp", LL[:, 0:64], lam_re[l, g0:g0 + 16, :], W=["ldsm"], dkey="ll")
            kb.dma("sp", LL[:, 64:128], lam_re[l, g0:g0 + 16, :], W=["ldsm"], dkey="ll")
            kb.dma("sp", LL[:, 128:192], lam_im[l, g0:g0 + 16, :], W=["ldsm"], dkey="ll")
            kb.dma("sp", LL[:, 192:256], lam_im[l, g0:g0 + 16, :], W=["ldsm"], dkey="ll")
            op("pe", lambda e: e.matmul(ps[6][:, 0:16], LL[:, 0:128], ident_f[0:16, 0:16], start=True, stop=False),
               R=["ldsm", "ident_f"], W=[("ps", 6)])
            op("pe", lambda e: e.matmul(ps[6][:, 16:32], LL[:, 128:256], ident_f[0:16, 0:16], start=False, stop=True),
               R=["ldsm", "ident_f"], W=[("ps", 6)])
            op("act", lambda e: e.activation(out=AB[:, 0:32], in_=ps[6][:, 0:32], func=AF.Copy), R=[("ps", 6)], W=["AB"])
            DT = ADT[:, 32:48]
            kb.dma("sp", DT, ssm_log_dt[l:l + 1, g0:g0 + 16].partition_broadcast(128).rearrange("p o n -> p (o n)"),
                   W=["ADT"], dkey="dt")
            op("act", lambda e: e.activation(out=DT, in_=DT, func=AF.Exp), R=["ADT"], W=["ADT"])
            op("dve", lambda e: e.tensor_tensor(ADT[:, 0:16], AB[:, 0:16], DT, op=ALU.mult), R=["AB", "ADT"], W=["ADT"])
            op("dve", lambda e: e.tensor_tensor(ADT[:, 16:32], AB[:, 16:32], DT, op=ALU.mult), R=["AB", "ADT"], W=["ADT"])
            TAU = ADT[:, 48:48 + T]
            o = 0
            for (b0, st, c) in taus:
                op("pool", lambda e, o=o, b0=b0, st=st, c=c: e.iota(TAU[:, o:o + c], [[st, c]], base=b0,
                                                                     channel_multiplier=0,
                                                                     allow_small_or_imprecise_dtypes=True),
                   W=["ADT"])
                o += c
            sh = [128, 16, T]
            op("dve", lambda e: e.tensor_tensor(tmpE, ADT[:, 0:16].unsqueeze(2).to_broadcast(sh),
                                                TAU.unsqueeze(1).to_broadcast(sh), op=ALU.mult), R=["ADT"], W=["tmpE"])
            op("act", lambda e: e.activation(out=tmpE, in_=tmpE, func=AF.Exp), R=["tmpE"], W=["tmpE"])
            op("dve", lambda e: e.tensor_tensor(PS, ADT[:, 16:32].unsqueeze(2).to_broadcast(sh),
                                                TAU.unsqueeze(1).to_broadcast(sh), op=ALU.mult), R=["ADT"], W=["PS"])
            sincos(PS, PC, PS, tmpA, tmpK, tmpK.bitcast(I32), ["PS"], "tmpA", "tmpK", "PCS")
            op("dve", lambda e: e.tensor_tensor(PC, PC, tmpE, op=ALU.mult), R=["PCS", "tmpE"], W=["PC"])
            op("dve", lambda e: e.tensor_tensor(PS, PS, tmpE, op=ALU.mult), R=["PCS", "tmpE", "PS"], W=["PS"])

        def stack_load(l, gh, src_r, src_i, ld, ldkey, dst, dstkey, kind, psbank, scale_col):
            g0 = 16 * gh
            if kind == "C":
                L4 = ld[0:16, :].rearrange("g (h r p) -> g h r p", h=16, r=2)
                kb.dma("sp", L4[:, :, 0, :], src_r[l, g0:g0 + 16, :, :], W=[ldkey], dkey=ldkey)
                kb.dma("sp", L4[:, :, 1, :], src_i[l, g0:g0 + 16, :, :], W=[ldkey], dkey=ldkey)
                for h in range(16):
                    op("pe", lambda e, h=h: e.matmul(ps[psbank][:, h * 16:(h + 1) * 16],
                                                     L4[:, h, :, :].rearrange("g r p -> g (r p)"),
                                                     ident_f[0:16, 0:16], start=(h == 0), stop=(h == 15)),
                       R=[ldkey, "ident_f"], W=[("ps", psbank)], sig=(h == 15))
            else:
                L4 = ld[0:16, :].rearrange("g (r p h) -> g r p h", r=2, p=64)
                kb.dma("sp", L4[:, 0, :, :], src_r[l, g0:g0 + 16, :, :], W=[ldkey], dkey=ldkey)
                kb.dma("sp", L4[:, 1, :, :], src_i[l, g0:g0 + 16, :, :], W=[ldkey], dkey=ldkey)
                for h in range(16):
                    op("pe", lambda e, h=h: e.matmul(ps[psbank][:, h * 16:(h + 1) * 16],
                                                     L4[:, :, :, h].rearrange("g r p -> g (r p)"),
                                                     ident_f[0:16, 0:16], start=(h == 0), stop=(h == 15)),
                       R=[ldkey, "ident_f"], W=[("ps", psbank)], sig=(h == 15))
            op("dve", lambda e: e.tensor_scalar(dst, ps[psbank][:, 0:256].rearrange("p (h g) -> p g h", h=16),
                                                scale_col, None, op0=ALU.mult),
               R=[("ps", psbank), "SGN"], W=[dstkey])

        def outer_combine(dst_bf, PCv, PSv, Sa, Sb, nt, keysR, wkey, pbanks):
            sh = [128, 16, nt, 16]
            tmp = PSALL[:, 512 * pbanks:512 * pbanks + 16 * nt * 16].rearrange("p (g t h) -> p g t h", g=16, t=nt)
            pk = [("ps", pbanks + j) for j in range((16 * nt * 16 + 511) // 512)]
            op("dve", lambda e: e.tensor_tensor(tmp, PCv.unsqueeze(3).to_broadcast(sh),
                                                Sa.unsqueeze(2).to_broadcast(sh), op=ALU.mult), R=keysR, W=pk)
            op("pool", lambda e: e.tensor_tensor(dst_bf, PSv.unsqueeze(3).to_broadcast(sh),
                                                 Sb.unsqueeze(2).to_broadcast(sh), op=ALU.mult), R=keysR, W=[wkey])
            op("dve", lambda e: e.tensor_tensor(dst_bf, tmp, dst_bf, op=ALU.add), R=pk + [wkey], W=[wkey])

        def ssm_gen_A(l, gh):
            g0 = 16 * gh
            ldA = FTf[:, 0:2048]
            ldB = FTf[:, 2048:4096]
            ST = FTf[:, 4096:5632].rearrange("p (s g h) -> p s g h", s=6, g=16)
            BzT = FT[:, 2 * 5632:2 * 7680].rearrange("p (g a m) -> p g a m", g=16, a=2)
            CF = FT[:, 0:4096].rearrange("p (g a m) -> p g a m", g=16, a=2)
            PC = SM[:, 0:512].rearrange("p (g t) -> p g t", g=16)
            PS = SM[:, 512:1024].rearrange("p (g t) -> p g t", g=16)
            tmpE = SM[:, 1024:1536].rearrange("p (g t) -> p g t", g=16)
            tmpA = SM[:, 1536:2048].rearrange("p (g t) -> p g t", g=16)
            tmpK = SM[:, 2048:2560].rearrange("p (g t) -> p g t", g=16)
            AB = TMP4[:, 0:32]
            ADT = TMP4[:, 32:160]
            KAP = TMP4[:, 160:320]
            ssm_tables(l, gh, [(15, -1, 16), (-7, 1, 16)], PC, PS, tmpE, tmpA, tmpK, AB, ADT, TMP4[:, 512:768])
            S3p = S34[:, gh, 0, :, :]
            S4p = S34[:, gh, 1, :, :]
            stack_load(l, gh, ssm_c_re, ssm_c_im, ldA, "ldA", S3p, "S34", "C", 7, SGN[:, 0:1])
            stack_load(l, gh, ssm_c_im, ssm_c_re, ldB, "ldB", ST[:, 1], "ST1", "C", 6, -1.0)
            op("pool", lambda e: e.tensor_copy(S4p, ST[:, 1]), R=["ST1"], W=["S34"])
            stack_load(l, gh, ssm_b_re, ssm_b_im, ldA, "ldA", ST[:, 2], "ST2", "B", 7, 1.0)
            stack_load(l, gh, ssm_b_im, ssm_b_re, ldB, "ldB", ST[:, 3], "ST3", "B", 6, SGN[:, 1:2])
            a_ = AB[:, 0:16]
            b_ = AB[:, 16:32]
            L1r = PC[:, :, 24]
            L1i = PS[:, :, 24]
            nr, den, t1, t2, kr, ki = [KAP[:, 16 * j:16 * (j + 1)] for j in range(6)]
            op("dve", lambda e: e.tensor_scalar(nr, L1r, -1.0, None, op0=ALU.add), R=["PC"], W=["KAP"])
            op("dve", lambda e: e.tensor_tensor(den, a_, a_, op=ALU.mult), R=["AB"], W=["KAP"])
            op("dve", lambda e: e.tensor_tensor(t1, b_, b_, op=ALU.mult), R=["AB"], W=["KAP"])
            op("dve", lambda e: e.tensor_tensor(den, den, t1, op=ALU.add), R=["KAP"], W=["KAP"])
            op("dve", lambda e: e.reciprocal(den, den), R=["KAP"], W=["KAP"])
            op("dve", lambda e: e.tensor_tensor(t1, nr, a_, op=ALU.mult), R=["KAP", "AB"], W=["KAP"])
            op("dve", lambda e: e.tensor_tensor(t2, L1i, b_, op=ALU.mult), R=["PS", "AB"], W=["KAP"])
            op("dve", lambda e: e.tensor_tensor(t1, t1, t2, op=ALU.add), R=["KAP"], W=["KAP"])
            op("dve", lambda e: e.tensor_tensor(kr, t1, den, op=ALU.mult), R=["KAP"], W=["KAP"])
            op("dve", lambda e: e.tensor_tensor(t1, L1i, a_, op=ALU.mult), R=["PS", "AB", "KAP"], W=["KAP"])
            op("dve", lambda e: e.tensor_tensor(t2, nr, b_, op=ALU.mult), R=["KAP", "AB"], W=["KAP"])
            op("dve", lambda e: e.tensor_tensor(t1, t1, t2, op=ALU.subtract), R=["KAP"], W=["KAP"])
            op("dve", lambda e: e.tensor_tensor(ki, t1, den, op=ALU.mult), R=["KAP"], W=["KAP"])
            sh3 = [128, 16, 16]
            krb = kr.unsqueeze(2).to_broadcast(sh3)
            kib = ki.unsqueeze(2).to_broadcast(sh3)
            op("dve", lambda e: e.tensor_tensor(ST[:, 4], ST[:, 2], krb, op=ALU.mult), R=["ST2", "KAP"], W=["ST4"])
            op("dve", lambda e: e.tensor_tensor(ST[:, 0], ST[:, 3], kib, op=ALU.mult), R=["ST3", "KAP"], W=["ST0"])
            op("dve", lambda e: e.tensor_tensor(ST[:, 4], ST[:, 4], ST[:, 0], op=ALU.add), R=["ST4", "ST0"], W=["ST4"])
            op("dve", lambda e: e.tensor_tensor(ST[:, 5], ST[:, 3], krb, op=ALU.mult), R=["ST3", "KAP"], W=["ST5"])
            op("dve", lambda e: e.tensor_tensor(ST[:, 0], ST[:, 2], kib, op=ALU.mult), R=["ST2", "KAP", "ST4"], W=["ST0"])
            op("dve", lambda e: e.tensor_tensor(ST[:, 5], ST[:, 5], ST[:, 0], op=ALU.subtract), R=["ST5", "ST0"], W=["ST5"])
            outer_combine(BzT.rearrange("p g a (j h) -> p g (a j) h", h=16), PC[:, :, 0:16], PS[:, :, 0:16],
                          ST[:, 4], ST[:, 5], 16, ["PC", "PS", "ST4", "ST5"], "BzT", 0)
            outer_combine(CF.rearrange("p g a (j h) -> p g (a j) h", h=16), PC[:, :, 16:32], PS[:, :, 16:32],
                          S3p, S4p, 16, ["PC", "PS", "S34", "ldA", "ldB"], "ldA", 0)
            for gp in range(8):
                pb = 4 + (gp % 2)
                for j in range(4):
                    g, jb = 2 * gp + j // 2, j % 2
                    op("pe", lambda e, g=g, jb=jb, j=j: e.matmul(ps[pb][:, j * 128:(j + 1) * 128], BzT[:, g, jb, :],
                                                               ident_b[:], start=(j == 0), stop=(j == 3)),
                       R=["BzT", "ident_b"], W=[("ps", pb)], sig=(j == 3))
                op("act", lambda e: e.activation(
                    out=BZCL[:, g0 + 2 * gp:g0 + 2 * gp + 2, :, :].rearrange("p g a m -> p (g a m)"),
                    in_=ps[pb][:], func=AF.Copy), R=[("ps", pb)], W=["BZCL"])
            for g in range(16):
                pb = 6 + (g % 2)
                op("pe", lambda e, g=g: e.matmul(ps[pb][:, 0:256], BzT[:, g, 1, :],
                                                 CF[:, g, :, :].rearrange("p a m -> p (a m)"), start=True, stop=True),
                   R=["BzT", "ldA"], W=[("ps", pb)])
                tm = TMP4[:, 768:1024].bitcast(BF16)[:, 0:256] if g % 2 == 0 else TMP4[:, 896:1024].bitcast(BF16)
                tkey = "tmM0" if g % 2 == 0 else "tmM1"
                tm = TMP4[:, 768:896].bitcast(BF16) if g % 2 == 0 else TMP4[:, 896:1024].bitcast(BF16)
                op("dve", lambda e: e.tensor_tensor(tm, ps[pb][:, 0:256], MASKM[:], op=ALU.mult),
                   R=[("ps", pb), "MASKM"], W=[tkey])
                op("dve", lambda e, g=g: e.scalar_tensor_tensor(
                    MW[:, g0 + g, :, :].rearrange("p a m -> p (a m)"), DIAG0[:], DCOL[:, g0 + g:g0 + g + 1], tm,
                    op0=ALU.mult, op1=ALU.add), R=["DIAG0", "DCOL", tkey], W=["MW"])

        def ssm_gen_B(l, gh):
            g0 = 16 * gh
            PC = TMP4[:, 0:256].rearrange("p (g t) -> p g t", g=16)
            PS = TMP4[:, 256:512].rearrange("p (g t) -> p g t", g=16)
            tmpE = TMP4[:, 512:768].rearrange("p (g t) -> p g t", g=16)
            tmpA = TMP4[:, 768:1024].rearrange("p (g t) -> p g t", g=16)
            tmpK = SM[:, 2048:2304].rearrange("p (g t) -> p g t", g=16)
            AB = SM[:, 2304:2336]
            ADT = SM[:, 2336:2432]
            ssm_tables(l, gh, [(1, 1, 16)], PC, PS, tmpE, tmpA, tmpK, AB, ADT, SM[:, 0:256])
            outer_combine(BZCL[:, g0:g0 + 16, :, :].rearrange("p g a (j h) -> p g (a j) h", h=16), PC, PS,
                          S34[:, gh, 0, :, :], S34[:, gh, 1, :, :], 16, ["PC", "PS", "S34"], "BZCL", 0)

        PWR = SM[:, 512:768].rearrange("p (g t) -> p g t", g=16)
        PWI = SM[:, 768:1024].rearrange("p (g t) -> p g t", g=16)

        def ssm_scan_coefs(l):
            LL = SM[0:16, 0:256]
            kb.dma("sp", LL[:, 0:128].rearrange("g (a p) -> g a p", a=2),
                   lam_re[l].rearrange("(a g) p -> g a p", a=2), W=["ldsm"], dkey="scl")
            kb.dma("sp", LL[:, 128:256].rearrange("g (a p) -> g a p", a=2),
                   lam_im[l].rearrange("(a g) p -> g a p", a=2), W=["ldsm"], dkey="scl")
            op("pe", lambda e: e.matmul(ps[6][:, 32:48], LL[:, 0:128], ident_f[0:16, 0:16], start=True, stop=False),
               R=["ldsm", "ident_f"], W=[("ps", 6)])
            op("pe", lambda e: e.matmul(ps[6][:, 48:64], LL[:, 128:256], ident_f[0:16, 0:16], start=False, stop=True),
               R=["ldsm", "ident_f"], W=[("ps", 6)])
            A_ = TMP4[:, 0:16]
            B_ = TMP4[:, 16:32]
            DT = TMP4[:, 32:48]
            TAU = TMP4[:, 48:64]
            g3 = lambda ap: ap.rearrange("p (g t) -> p g t", g=16)
            EA = g3(TMP4[:, 64:320])
            AN = g3(TMP4[:, 320:576])
            tA = g3(TMP4[:, 576:832])
            tK = g3(SM[:, 256:512])
            op("act", lambda e: e.activation(out=TMP4[:, 0:32], in_=ps[6][:, 32:64], func=AF.Copy), R=[("ps", 6)], W=["TMP4"])
            for a in range(2):
                kb.dma("sp", DT[64 * a:64 * a + 64, :],
                       ssm_log_dt[l:l + 1, 16 * a:16 * a + 16].partition_broadcast(64).rearrange("p o n -> p (o n)"),
                       W=["TMP4"], dkey="dts")
            op("act", lambda e: e.activation(out=DT, in_=DT, func=AF.Exp), R=["TMP4"], W=["TMP4"])
            op("pool", lambda e: e.iota(TAU, [[16, 16]], base=16, channel_multiplier=0,
                                        allow_small_or_imprecise_dtypes=True), R=["TMP4"], W=["TMP4"])
            op("dve", lambda e: e.tensor_tensor(A_, A_, DT, op=ALU.mult), R=["TMP4"], W=["TMP4"])
            op("dve", lambda e: e.tensor_tensor(B_, B_, DT, op=ALU.mult), R=["TMP4"], W=["TMP4"])
            sh = [128, 16, 16]
            op("dve", lambda e: e.tensor_tensor(EA, A_.unsqueeze(2).to_broadcast(sh), TAU.unsqueeze(1).to_broadcast(sh),
                                                op=ALU.mult), R=["TMP4"], W=["TMP4"])
            op("act", lambda e: e.activation(out=EA, in_=EA, func=AF.Exp), R=["TMP4"], W=["TMP4"])
            op("dve", lambda e: e.tensor_tensor(AN, B_.unsqueeze(2).to_broadcast(sh), TAU.unsqueeze(1).to_broadcast(sh),
                                                op=ALU.mult), R=["TMP4"], W=["TMP4"])
            sincos(PWI, PWR, AN, tA, tK, tK.bitcast(I32), ["TMP4"], "TMP4", "tKc", "PW")
            op("dve", lambda e: e.tensor_tensor(PWR, PWR, EA, op=ALU.mult), R=["PW", "TMP4"], W=["PW"])
            op("dve", lambda e: e.tensor_tensor(PWI, PWI, EA, op=ALU.mult), R=["PW", "TMP4"], W=["PW"])
            for j, t in ((0, 0), (1, 15)):
                op("dve", lambda e, j=j, t=t: e.tensor_copy(LAB[:, j, 0, 0, :], PWR[:, :, t]), R=["PW"], W=["LAB"])
                op("dve", lambda e, j=j, t=t: e.tensor_copy(LAB[:, j, 0, 1, :], PWR[:, :, t]), R=["PW"], W=["LAB"])
                op("dve", lambda e, j=j, t=t: e.tensor_copy(LAB[:, j, 1, 1, :], PWI[:, :, t]), R=["PW"], W=["LAB"])
                op("dve", lambda e, j=j, t=t: e.tensor_scalar(LAB[:, j, 1, 0, :], PWI[:, :, t], -1.0, None, op0=ALU.mult),
                   R=["PW"], W=["LAB"])

        def ssm_main(l):
            DT_ = SS[0:16, 0:32]
            with nc.allow_non_contiguous_dma(reason="tiny D load"):
                kb.dma("sp", DT_, ssm_d[l].rearrange("(g h) -> h g", h=16), W=["dT"], dkey="dT")
            op("pe", lambda e: e.matmul(ps[5][:, 0:32], REP16[:], DT_, start=True, stop=True),
               R=["REP16", "dT"], W=[("ps", 5)])
            op("act", lambda e: e.activation(out=DCOL[:], in_=ps[5][:, 0:32], func=AF.Copy), R=[("ps", 5)], W=["DCOL"])
            ssm_gen_A(l, 0)
            if debug is not None and debug[0] == f"tab{l}":
                kb.barrier()
                kb.dma("sp", dbg_d[:, 0:1024], SM[:, 0:1024], dkey="dbg")
                kb.dma("sp", dbg_d[:, 1024:2560], FTf[:, 4096:5632], dkey="dbg")
                kb.dma("sp", dbg_d[:, 2560:2880], TMP4[:, 0:320], dkey="dbg")
                kb.dma("sp", dbg_d[:, 2880:3904], TMP4b[:, 0:1024], dkey="dbg")
                return True
            ssm_gen_A(l, 1)
            if debug is not None and debug[0] == f"bz{l}":
                dump(BIG[:, 0:8192], 8192, F32, ["BZCL", "MW"])
                return True
            kb.barrier()
            ALLU = [("UCM", j) for j in range(NT)]
            for b8 in range(8):
                pb = b8 % 2
                for j in range(8):
                    g, jb = 4 * b8 + j // 2, j % 2
                    op("pe", lambda e, g=g, jb=jb, j=j: e.transpose(
                        psb(pb)[:, j * 128:(j + 1) * 128], UCMg[:, g, 8 * jb:8 * jb + 8, :].rearrange("p j h -> p (j h)"),
                        ident_b[:]),
                       R=ALLU + ["ident_b"], W=[("ps", pb)], sig=(j == 7))
                eng = "act" if b8 % 2 == 0 else "dve"
                dst = UT[:, 4 * b8:4 * b8 + 4, :, :].rearrange("p g a c -> p (g a c)")
                if eng == "act":
                    op("act", lambda e: e.activation(out=dst, in_=psb(pb), func=AF.Copy), R=[("ps", pb)], W=["UT"])
                else:
                    op("dve", lambda e: e.tensor_copy(dst, psb(pb)), R=[("ps", pb)], W=["UT"])
            for g4 in range(8):
                pb = 2 + (g4 % 2)
                for j in range(4):
                    g = 4 * g4 + j
                    for jb in range(2):
                        op("pe", lambda e, g=g, jb=jb, j=j: e.matmul(ps[pb][:, j * 128:(j + 1) * 128], BZCL[:, g, jb, :],
                                                                   UT[:, g, jb, :], start=(j == 0 and jb == 0),
                                                                   stop=(j == 3 and jb == 1)),
                           R=["BZCL", "UT"], W=[("ps", pb)], sig=(j == 3 and jb == 1))
                ghh, gl0 = g4 // 4, 4 * (g4 % 4)
                pv = ps[pb][:].rearrange("p (g c) -> p g c", g=4)
                op("act", lambda e: e.activation(out=ZS[64 * ghh:64 * ghh + 64, 0, gl0:gl0 + 4, :], in_=pv[0:64],
                                                 func=AF.Copy), R=[("ps", pb)], W=["ZS"])
                op("dve", lambda e: e.tensor_copy(ZS[64 * ghh:64 * ghh + 64, 1, gl0:gl0 + 4, :], pv[64:128]),
                   R=[("ps", pb)], W=["ZS"])
            if debug is not None and debug[0] == f"z0{l}":
                dump(FTf[:, 4096:8192], 4096, F32, ["ZS"])
                return True
            ssm_scan_coefs(l)
            kb.barrier()
            ssm_gen_B(l, 0)
            ssm_gen_B(l, 1)
            ZS5 = FTf[:, 4096:8192].rearrange("p (r g b s) -> p r g b s", r=2, g=16, b=8)
            SCb = SM[:, 1024:1536].rearrange("p (a r g b) -> p a r g b", a=2, r=2, g=16)
            sh4 = [128, 2, 16, 8]
            sh3 = [128, 16, 8]
            for s_ in range(1, 16):
                prev = ZS5[:, :, :, :, s_ - 1]
                cur = ZS5[:, :, :, :, s_]
                op("pool", lambda e: e.tensor_tensor(SCb[:, 0], prev, LAB[:, 0, 0].unsqueeze(3).to_broadcast(sh4),
                                                     op=ALU.mult), R=["ZS", "LAB"], W=["SC0"])
                op("pool", lambda e: e.tensor_tensor(SCb[:, 1, 0], prev[:, 1], LAB[:, 0, 1, 0, :].unsqueeze(2).to_broadcast(sh3),
                                                     op=ALU.mult), R=["ZS", "LAB"], W=["SC1"])
                op("pool", lambda e: e.tensor_tensor(SCb[:, 1, 1], prev[:, 0], LAB[:, 0, 1, 1, :].unsqueeze(2).to_broadcast(sh3),
                                                     op=ALU.mult), R=["ZS", "LAB"], W=["SC1"])
                op("pool", lambda e: e.tensor_tensor(SCb[:, 0], SCb[:, 0], SCb[:, 1], op=ALU.add), R=["SC0", "SC1"], W=["SC0"])
                op("pool", lambda e: e.tensor_tensor(cur, cur, SCb[:, 0], op=ALU.add), R=["ZS", "SC0"], W=["ZS"])
            SC2 = SM[:, 1536:1600].rearrange("p (a r g) -> p a r g", a=2, r=2)
            for bk in range(1, 8):
                prev = ZS5[:, :, :, bk - 1, 15]
                cur = ZS5[:, :, :, bk, 15]
                op("pool", lambda e: e.tensor_tensor(SC2[:, 0], prev, LAB[:, 1, 0], op=ALU.mult), R=["ZS", "LAB"], W=["SC20"])
                op("pool", lambda e: e.tensor_tensor(SC2[:, 1, 0, :], prev[:, 1, :], LAB[:, 1, 1, 0, :], op=ALU.mult),
                   R=["ZS", "LAB"], W=["SC21"])
                op("pool", lambda e: e.tensor_tensor(SC2[:, 1, 1, :], prev[:, 0, :], LAB[:, 1, 1, 1, :], op=ALU.mult),
                   R=["ZS", "LAB"], W=["SC21"])
                op("pool", lambda e: e.tensor_tensor(SC2[:, 0], SC2[:, 0], SC2[:, 1], op=ALU.add), R=["SC20", "SC21"], W=["SC20"])
                op("pool", lambda e: e.tensor_tensor(cur, cur, SC2[:, 0], op=ALU.add), R=["ZS", "SC20"], W=["ZS"])
            F_ = [SM[:, 1600:1840].rearrange("p (g s) -> p g s", g=16),
                  SM[:, 1024:1264].rearrange("p (g s) -> p g s", g=16)]
            sh15 = [128, 16, 15]
            pr = PWR[:, :, 0:15]
            pi_ = PWI[:, :, 0:15]
            for bk in range(1, 8):
                cr = ZS5[:, 0, :, bk - 1, 15].unsqueeze(2).to_broadcast(sh15)
                ci = ZS5[:, 1, :, bk - 1, 15].unsqueeze(2).to_broadcast(sh15)
                re = ZS5[:, 0, :, bk, 0:15]
                im = ZS5[:, 1, :, bk, 0:15]
                for (dst, pa, ca, pb_, cb, sub) in ((re, pr, cr, pi_, ci, True), (im, pr, ci, pi_, cr, False)):
                    op("pool", lambda e, pa=pa, ca=ca: e.tensor_tensor(F_[0], pa, ca, op=ALU.mult), R=["PW", "ZS"], W=["F0"])
                    op("pool", lambda e, pb_=pb_, cb=cb: e.tensor_tensor(F_[1], pb_, cb, op=ALU.mult), R=["PW", "ZS"], W=["SC0"])
                    op("pool", lambda e, dst=dst: e.tensor_tensor(dst, dst, F_[0], op=ALU.add), R=["F0", "ZS"], W=["ZS"])
                    op("pool", lambda e, dst=dst, sub=sub: e.tensor_tensor(dst, dst, F_[1],
                                                                      op=(ALU.subtract if sub else ALU.add)),
                       R=["SC0", "ZS"], W=["ZS"])
            if debug is not None and debug[0] == f"zs{l}":
                dump(FTf[:, 4096:8192], 4096, F32, ["ZS"])
                return True
            op("pool", lambda e: e.memset(XSB[:, :, 0:1], 0.0), R=["ZS"], W=["XSB", "ldsm"])
            for ghh in range(2):
                for r in range(2):
                    eng = "act" if r == 0 else "dve"
                    src = ZS[64 * ghh:64 * ghh + 64, r, :, 0:127]
                    dst = XSB[64 * r:64 * r + 64, 16 * ghh:16 * ghh + 16, 1:128]
                    if eng == "act":
                        op("act", lambda e: e.activation(out=dst, in_=src, func=AF.Copy), R=["ZS"], W=["XSB", "ldsm"])
                    else:
                        op("dve", lambda e: e.tensor_copy(dst, src), R=["ZS"], W=["XSB", "ldsm"])
            for gp in range(16):
                pb = 4 + (gp % 4)
                for j in range(2):
                    g = 2 * gp + j
                    o = j * 256
                    op("pe", lambda e, g=g, o=o, j=j: e.matmul(ps[pb][:, o:o + 256], UT[:, g, 0, :],
                                                          MW[:, g, :, :].rearrange("p a m -> p (a m)"),
                                                          start=(j == 0), stop=False, skip_group_check=True),
                       R=["UT", "MW"], W=[("ps", pb)], sig=False)
                    op("pe", lambda e, g=g, o=o: e.matmul(ps[pb][:, o + 128:o + 256], UT[:, g, 1, :], MW[:, g, 0, :],
                                                     start=False, stop=False, skip_group_check=True),
                       R=["UT", "MW"], W=[("ps", pb)], sig=False)
                    op("pe", lambda e, g=g, o=o: e.matmul(ps[pb][:, o:o + 256], XSB[:, g, :],
                                                     BZCL[:, g, :, :].rearrange("p a m -> p (a m)"),
                                                     start=False, stop=(j == 1), skip_group_check=True),
                       R=["XSB", "BZCL"], W=[("ps", pb)], sig=(j == 1))
                dst = UCM[:, :, 32 * gp:32 * gp + 32].rearrange("p i (g h) -> p g i h", g=2)
                op("act", lambda e: e.activation(out=dst, in_=ps[pb][:].rearrange("p (g i h) -> p g i h", g=2, i=16),
                                                 func=AF.Gelu_apprx_tanh), R=[("ps", pb)], W=ALLU)

        def glu_phase(l):
            WGLU = TMP4[:].bitcast(BF16).rearrange("p (k n) -> p k n", k=4)
            kb.dma("pool", WGLU, w_glu[l].rearrange("(k p) n -> p k n", p=128), W=["TMP4"], dkey="wglu")
            ZSS = SS[:, 0:16]
            def tile(i):
                s2 = i % 2
                pb = s2
                YGT = SM[:, 2048 + 256 * s2:2048 + 256 * (s2 + 1)].bitcast(BF16)
                SG = SM[:, 1024 * s2:1024 * s2 + 512]
                ZF = SM[:, 1024 * s2 + 512:1024 * s2 + 1024]
                ZN = SG.bitcast(BF16)[:, 0:512]
                for fc in range(4):
                    op("pe", lambda e, fc=fc: e.transpose(psb(pb)[:, fc * 128:(fc + 1) * 128],
                                                          UCM[:, i, fc * 128:(fc + 1) * 128], ident_b[:]),
                       R=[("UCM", i), "ident_b"], W=[("ps", pb)], sig=(fc == 3))
                    yield
                op("act", lambda e: e.activation(out=YGT, in_=psb(pb)[:, 0:512], func=AF.Copy),
                   R=[("ps", pb)], W=[("YGT", s2)])
                yield
                pg = 2 + s2
                for fc in range(4):
                    op("pe", lambda e, fc=fc: e.matmul(ps[pg][:], YGT[:, fc * 128:(fc + 1) * 128], WGLU[:, fc, :],
                                                       start=(fc == 0), stop=(fc == 3)),
                       R=[("YGT", s2), "TMP4"], W=[("ps", pg)], sig=(fc == 3))
                    yield
                op("act", lambda e: e.activation(out=SG, in_=ps[pg][:], func=AF.Sigmoid), R=[("ps", pg)], W=[("SG", s2)])
                yield
                op("dve", lambda e: e.tensor_tensor(ZF, UCM[:, i, :], SG, op=ALU.mult),
                   R=[("UCM", i), ("SG", s2)], W=[("ZF", s2)])
                yield
                op("dve", lambda e: e.scalar_tensor_tensor(ZN, ZF, 1.0, ZF, op0=ALU.mult, op1=ALU.mult,
                                                           accum_out=ZSS[:, i:i + 1]),
                   R=[("ZF", s2), ("SG", s2)], W=[("SG", s2), ("ZSS", i)])
                yield
                op("dve", lambda e: e.tensor_scalar(SS[:, 16 + i:17 + i], ZSS[:, i:i + 1], 1.0 / 512.0, EPS,
                                                    op0=ALU.mult, op1=ALU.add), R=[("ZSS", i)], W=[("ZSb", i)])
                yield
                op("pool", lambda e: e.tensor_tensor(SS[:, 32 + i:33 + i], SS[:, 16 + i:17 + i], NHALF[:, 0:1],
                                                     op=ALU.pow), R=[("ZSb", i), "NHALF"], W=[("ZRS", i)])
                yield
                op("dve", lambda e: e.tensor_scalar(ZN, ZF, SS[:, 32 + i:33 + i], None, op0=ALU.mult),
                   R=[("ZF", s2), ("ZRS", i), ("SG", s2)], W=[("SG", s2)])
                yield
                pt = 4 + s2
                for fc in range(4):
                    op("pe", lambda e, fc=fc: e.transpose(psb(pt)[:, fc * 128:(fc + 1) * 128],
                                                          ZN[:, fc * 128:(fc + 1) * 128], ident_b[:]),
                       R=[("SG", s2), "ident_b"], W=[("ps", pt)], sig=(fc == 3))
                    yield
                op("act", lambda e: e.activation(out=FT3[:, 0:4, i::16],
                                                 in_=psb(pt)[:, 0:512].rearrange("p (k c) -> p k c", k=4),
                                                 func=AF.Copy), R=[("ps", pt)], W=[("ST", i)])
                yield

            run_rr([tile(i) for i in range(NT)], 2, 8)

        WOUT = big_bf(0, 4096).rearrange("p (k n) -> p k n", k=8)

        def wout_load(l):
            kb.dma("pool", WOUT, w_out[l].rearrange("(k p) n -> p k n", p=128), W=["WOUT"], dkey="wout")
            for k in range(8):
                op("dve", lambda e, k=k: e.scalar_tensor_tensor(WOUT[:, k, :], WOUT[:, k, :], GROW[:, k:k + 1],
                                                                MOD[:, 2, :], op0=ALU.mult, op1=ALU.mult),
                   R=["WOUT", "GROW", ("MOD", 2)], W=["WOUT"])

        def wout_phase(l):
            ALLAT = [("AT", n) for n in range(NT)]
            ALLST = [("ST", n) for n in range(NT)]
            cnt = 0
            for i in range(NT):
                for half in range(2):
                    pb = 4 + (cnt % 4)
                    cnt += 1
                    for k in range(8):
                        lhs = AT3[:, k, i::16] if k < 4 else FT3[:, k - 4, i::16]
                        op("pe", lambda e, k=k, lhs=lhs: e.matmul(ps[pb][:], lhs, WOUT[:, k, half * 512:(half + 1) * 512],
                                                                 start=(k == 0), stop=(k == 7)),
                           R=ALLAT + ALLST + ["WOUT"], W=[("ps", pb)], sig=(k == 7))
                    xs_ = X[:, i, half * 512:(half + 1) * 512]
                    op("dve", lambda e, xs_=xs_: e.tensor_tensor(xs_, xs_, ps[pb][:], op=ALU.add),
                       R=[("ps", pb), ("X", i)], W=[("X", i)])

        H2T3 = big_bf(4096, 12288).rearrange("p (k t) -> p k t", k=8)
        ACTT = big_bf(0, 4096).rearrange("p (k t) -> p k t", k=4)

        def ring(sl):
            if sl < 4:
                return FT[:, sl * 4096:(sl + 1) * 4096]
            return AT[:, (sl - 4) * 4096:(sl - 3) * 4096]

        def router(l):
            ALLH = [("H2", i) for i in range(NT)]
            for i in range(NT):
                for k in range(8):
                    op("pe", lambda e, k=k: e.matmul(ps[6][:, i * 16:(i + 1) * 16], H2T3[:, k, i * 128:(i + 1) * 128],
                                                     WR[:, k, :], start=(i == 0 and k == 0), stop=(i == NT - 1 and k == 7),
                                                     skip_group_check=True),
                       R=[("H2", i), "WR"], W=[("ps", 6)], sig=(i == NT - 1 and k == 7))
            v3 = lambda ap: ap.rearrange("p (i e) -> p i e", i=16)
            v4 = lambda ap: ap.rearrange("p (i g e) -> p i g e", i=16, g=4)
            LG = TMP4[:, 0:256]
            PR = TMP4[:, 256:512]
            SEL = TMP4[:, 512:768]
            SEL2 = TMP4[:, 768:1024]
            EQ1 = TMP4b[:, 0:256]
            EQ2 = TMP4b[:, 256:512]
            CMB = TMP4b[:, 512:768]
            CMBb = TMP4b[:, 768:896].bitcast(BF16)
            MX = SS[:, 0:16]
            SM_ = SS[:, 16:32]
            M1 = SM[:, 2048:2112]
            M2 = SM[:, 2112:2176]
            GS = SM[:, 2176:2240]
            GM = SS[:, 32:48]
            ING = SM[:, 2240:2304]
            b3 = lambda ap: ap.unsqueeze(2).to_broadcast([128, 16, 16])
            op("dve", lambda e: e.tensor_reduce(MX, v3(ps[6][:, 0:256]), axis=AX.X, op=ALU.max), R=[("ps", 6)], W=["r_mx"])
            op("dve", lambda e: e.tensor_tensor(v3(LG), v3(ps[6][:, 0:256]), b3(MX), op=ALU.subtract),
               R=[("ps", 6), "r_mx"], W=["TMP4"])
            op("act", lambda e: e.activation(out=LG, in_=LG, func=AF.Exp), R=["TMP4"], W=["TMP4"])
            op("dve", lambda e: e.tensor_reduce(SM_, v3(LG), axis=AX.X, op=ALU.add), R=["TMP4"], W=["r_sm"])
            op("dve", lambda e: e.reciprocal(SM_, SM_), R=["r_sm"], W=["r_sm"])
            op("dve", lambda e: e.tensor_tensor(v3(PR), v3(LG), b3(SM_), op=ALU.mult), R=["TMP4", "r_sm"], W=["TMP4"])
            op("dve", lambda e: e.tensor_tensor(v3(SEL), v3(PR), RB[:].unsqueeze(1).to_broadcast([128, 16, 16]),
                                                op=ALU.add), R=["TMP4", "RB"], W=["TMP4"])
            op("dve", lambda e: e.tensor_reduce(M1, SEL.rearrange("p (j e) -> p j e", e=4), axis=AX.X, op=ALU.max),
               R=["TMP4"], W=["r_m1"])
            b4 = lambda ap: ap.unsqueeze(2).to_broadcast([128, 64, 4])
            j4 = lambda ap: ap.rearrange("p (j e) -> p j e", e=4)
            op("dve", lambda e: e.tensor_tensor(j4(EQ1), j4(SEL), b4(M1), op=ALU.is_equal), R=["TMP4", "r_m1"], W=["TMP4b"])
            op("dve", lambda e: e.scalar_tensor_tensor(SEL2, EQ1, -1.0e9, SEL, op0=ALU.mult, op1=ALU.add),
               R=["TMP4b", "TMP4"], W=["TMP4"])
            op("dve", lambda e: e.tensor_reduce(M2, j4(SEL2), axis=AX.X, op=ALU.max), R=["TMP4"], W=["r_m2"])
            op("dve", lambda e: e.tensor_tensor(j4(EQ2), j4(SEL2), b4(M2), op=ALU.is_equal), R=["TMP4", "r_m2"], W=["TMP4b"])
            op("dve", lambda e: e.tensor_tensor(GS, M1, M2, op=ALU.add), R=["r_m1", "r_m2"], W=["r_gs"])
            op("dve", lambda e: e.tensor_reduce(GM, GS.rearrange("p (i g) -> p i g", g=4), axis=AX.X, op=ALU.max),
               R=["r_gs"], W=["r_gm"])
            op("dve", lambda e: e.tensor_tensor(ING.rearrange("p (i g) -> p i g", g=4), GS.rearrange("p (i g) -> p i g", g=4),
                                                GM.unsqueeze(2).to_broadcast([128, 16, 4]), op=ALU.is_equal),
               R=["r_gs", "r_gm"], W=["r_ing"])
            op("dve", lambda e: e.tensor_tensor(EQ1, EQ1, EQ2, op=ALU.add), R=["TMP4b"], W=["TMP4b"])
            op("dve", lambda e: e.tensor_tensor(j4(EQ1), j4(EQ1), b4(ING), op=ALU.mult), R=["TMP4b", "r_ing"], W=["TMP4b"])
            op("dve", lambda e: e.tensor_tensor(EQ1, EQ1, PR, op=ALU.mult), R=["TMP4b", "TMP4"], W=["TMP4b"])
            op("dve", lambda e: e.tensor_reduce(SM_, v3(EQ1), axis=AX.X, op=ALU.add), R=["TMP4b"], W=["r_sm"])
            op("dve", lambda e: e.reciprocal(SM_, SM_), R=["r_sm"], W=["r_sm"])
            op("dve", lambda e: e.tensor_tensor(v3(CMBb), v3(EQ1), b3(SM_), op=ALU.mult), R=["TMP4b", "r_sm"], W=["TMP4b"])
            if debug is not None and debug[0] == f"comb{l}":
                op("dve", lambda e: e.tensor_tensor(v3(CMB), v3(EQ1), b3(SM_), op=ALU.mult), R=["TMP4b", "r_sm"], W=["TMP4b"])
                dump(CMB, 256, F32, ["TMP4b"])
                return True
            CT = SM[0:16, 0:1024].bitcast(BF16)
            for i in range(NT):
                pb = 4 + (i // 8)
                op("pe", lambda e, i=i: e.transpose(psb(pb)[0:16, (i % 8) * 128:(i % 8 + 1) * 128],
                                                    CMBb[:, i * 16:(i + 1) * 16], ident_b[:]),
                   R=["TMP4b", "ident_b"], W=[("ps", pb)], sig=(i % 8 == 7))
            for hb in range(2):
                op("act", lambda e, hb=hb: e.activation(out=CT[:, hb * 1024:(hb + 1) * 1024], in_=psb(4 + hb)[0:16, :],
                                                        func=AF.Copy), R=[("ps", 4 + hb)], W=["CT"])
            return False

        def load_expert(l, e_):
            base = 3 * (e_ % 2)
            kb.dma("pool", ring(base).rearrange("p (k n) -> p k n", k=8),
                   w_exp_gate[l, e_].rearrange("(k p) n -> p k n", p=128), W=[("ring", base)], dkey=f"ring{base}")
            kb.dma("pool", ring(base + 1).rearrange("p (k n) -> p k n", k=8),
                   w_exp_up[l, e_].rearrange("(k p) n -> p k n", p=128), W=[("ring", base + 1)], dkey=f"ring{base + 1}")
            wd = ring(base + 2).rearrange("p (k n) -> p k n", k=4)
            kb.dma("pool", wd, w_exp_down[l, e_].rearrange("(k p) n -> p k n", p=128),
                   W=[("ring", base + 2)], dkey=f"ring{base + 2}")


        def moe_phase(l):
            CT = SM[0:16, 0:1024].bitcast(BF16)

            def scale_wd(e_):
                base = 3 * (e_ % 2)
                wd = ring(base + 2).rearrange("p (k n) -> p k n", k=4)
                op("pool", lambda e: e.tensor_tensor(wd, wd, MOD[:, 5, :].unsqueeze(1).to_broadcast([128, 4, 1024]),
                                                     op=ALU.mult), R=[("ring", base + 2), ("MOD", 5)], W=[("ring", base + 2)])

            def prep_piece(e2, tb):
                p2 = e2 % 2
                CB2 = (TMP4 if p2 == 0 else TMP4b)[:].bitcast(BF16)
                ck2 = "TMP4" if p2 == 0 else "TMP4b"
                if tb == 0:
                    op("dve", lambda e: e.tensor_copy(SELE[:, p2, :], ident_b[0:16, e2:e2 + 1].to_broadcast([16, 128])),
                       R=["ident_b"], W=[("SELE", p2)])
                op("pe", lambda e: e.matmul(ps[6][:], SELE[:, p2, :], CT[:, tb * 512:(tb + 1) * 512],
                                            start=True, stop=True), R=[("SELE", p2), "CT"], W=[("ps", 6)])
                op("act", lambda e: e.activation(out=CB2[:, tb * 512:(tb + 1) * 512], in_=ps[6][:], func=AF.Copy),
                   R=[("ps", 6)], W=[(ck2, tb)])

            gcnt = 0
            dcnt = 0
            for e_ in range(16):
                if e_ + 1 < 16:
                    load_expert(l, e_ + 1)
                base = 3 * (e_ % 2)
                WG = ring(base).rearrange("p (k n) -> p k n", k=8)
                WU = ring(base + 1).rearrange("p (k n) -> p k n", k=8)
                WD = ring(base + 2).rearrange("p (k n) -> p k n", k=4)
                es_ = e_ % 2
                CBC = (TMP4 if es_ == 0 else TMP4b)[:].bitcast(BF16)
                ckey = "TMP4" if es_ == 0 else "TMP4b"
                if e_ == 0:
                    for tb in range(4):
                        prep_piece(0, tb)
                for fc in range(4):
                    if fc == 2:
                        scale_wd(e_)
                    for tb in range(4):
                        g2 = gcnt % 2
                        gcnt += 1
                        pg, pu = g2, 2 + g2
                        hk = [("H2", i) for i in range(4 * tb, 4 * tb + 4)]
                        for k in range(8):
                            op("pe", lambda e, k=k: e.matmul(ps[pg][:], WG[:, k, fc * 128:(fc + 1) * 128],
                                                             H2T3[:, k, tb * 512:(tb + 1) * 512],
                                                             start=(k == 0), stop=(k == 7)),
                               R=[("ring", base)] + hk, W=[("ps", pg)], sig=(k == 7))
                        for k in range(8):
                            op("pe", lambda e, k=k: e.matmul(ps[pu][:], WU[:, k, fc * 128:(fc + 1) * 128],
                                                             H2T3[:, k, tb * 512:(tb + 1) * 512],
                                                             start=(k == 0), stop=(k == 7)),
                               R=[("ring", base + 1)] + hk, W=[("ps", pu)], sig=(k == 7))
                        SGt = SM[:, 1024 + 256 * g2:1280 + 256 * g2].bitcast(BF16)
                        Tt = SM[:, 1536 + 256 * g2:1792 + 256 * g2].bitcast(BF16)
                        op("act", lambda e: e.activation(out=SGt, in_=ps[pg][:], func=AF.Silu),
                           R=[("ps", pg)], W=[("SGt", g2)])
                        op("dve", lambda e: e.tensor_tensor(Tt, SGt, ps[pu][:], op=ALU.mult),
                           R=[("SGt", g2), ("ps", pu)], W=[("Tt", g2)])
                        op("pool", lambda e: e.tensor_tensor(ACTT[:, fc, tb * 512:(tb + 1) * 512], Tt,
                                                             CBC[:, tb * 512:(tb + 1) * 512], op=ALU.mult),
                           R=[("Tt", g2), (ckey, tb)], W=[("ACTT", fc, tb)])
                    if e_ + 1 < 16:
                        prep_piece(e_ + 1, fc)
                for i in range(NT):
                    for half in range(2):
                        pd = (4, 5, 7)[dcnt % 3]
                        dcnt += 1
                        for fc in range(4):
                            op("pe", lambda e, fc=fc: e.matmul(ps[pd][:], ACTT[:, fc, i * 128:(i + 1) * 128],
                                                               WD[:, fc, half * 512:(half + 1) * 512],
                                                               start=(fc == 0), stop=(fc == 3)),
                               R=[("ACTT", fc, i // 4), ("ring", base + 2)], W=[("ps", pd)], sig=(fc == 3))
                        xs_ = X[:, i, half * 512:(half + 1) * 512]
                        op("dve", lambda e, xs_=xs_: e.tensor_tensor(xs_, xs_, ps[pd][:], op=ALU.add),
                           R=[("ps", pd), ("X", i, half)], W=[("X", i, half)])

        def dump(ap, shape2d_cols, dt, keys):
            kb.dma("sp", dbg_d[:, 0:shape2d_cols], ap, R=keys, dkey="dbg")

        for l in range(depth):
            adaln(l)
            if debug is not None and debug[0] == f"mod{l}":
                dump(MOD[:].rearrange("p a n -> p (a n)"), 6 * D, BF16, [("MOD", j) for j in range(6)])
                break
            kb.barrier()
            layer_loads(l)
            norm_to_FT(l, 0, lambda i: FT3[:, :, i::16])
            if debug is not None and debug[0] == f"hT{l}":
                dump(FT[:], 8 * S, BF16, [("FT", i) for i in range(NT)])
                break
            u_proj(l)
            if debug is not None and debug[0] == f"ucm{l}":
                dump(UCM.rearrange("p j f -> p (j f)"), 16 * 512, BF16, [("UCM", j) for j in range(NT)])
                break
            kb.barrier()
            attention(l)
            if debug is not None and debug[0] == f"at{l}":
                dump(AT[:], 4 * S, BF16, [("AT", n) for n in range(NT)])
                break
            kb.barrier()
            if ssm_main(l):
                break
            if debug is not None and debug[0] == f"yg{l}":
                dump(UCM.rearrange("p j f -> p (j f)"), 16 * 512, BF16, [("UCM", j) for j in range(NT)])
                break
            kb.barrier()
            wout_load(l)
            glu_phase(l)
            if debug is not None and debug[0] == f"st{l}":
                dump(FT[:, 0:4 * S], 4 * S, BF16, [("ST", i) for i in range(NT)])
                break
            kb.barrier()
            wout_phase(l)
            if debug is not None and debug[0] == f"x1{l}":
                kb.barrier()
                dump(X[:].rearrange("p i d -> p (i d)"), NT * D, F32, [("X", i) for i in range(NT)])
                break
            kb.barrier()
            load_expert(l, 0)
            norm_to_FT(l, 1, lambda i: H2T3[:, :, i * 128:(i + 1) * 128], okey="H2", pbanks=(0, 1))
            if debug is not None and debug[0] == f"h2{l}":
                kb.barrier()
                dump(BIG[:, 4096:12288].bitcast(BF16), 8 * S, BF16, [("H2", i) for i in range(NT)])
                break
            kb.barrier()
            if router(l):
                break
            kb.barrier()
            moe_phase(l)
            kb.barrier()
            if debug is not None and debug[0] == f"x2{l}":
                dump(X[:].rearrange("p i d -> p (i d)"), NT * D, F32, [("X", i) for i in range(NT)])
                break

        if debug is None:
            y_v = y_d.rearrange("(c i) d -> c i d", i=NT)
            for q4 in range(4):
                kb.dma("sp", y_v[:, 4 * q4:4 * q4 + 4, :], X[:, 4 * q4:4 * q4 + 4, :],
                       R=[("X", i) for i in range(4 * q4, 4 * q4 + 4)], dkey=f"y{q4}")
        kb.final_wait("sp")
    return nc


_IN_NAMES = ["ada_w", "ada_b", "norm1_g", "w_in", "q_norm_g", "k_norm_g", "attn_sink", "lam_re", "lam_im",
             "ssm_b_re", "ssm_b_im", "ssm_c_re", "ssm_c_im", "ssm_d", "ssm_log_dt", "w_glu", "attn_out_g",
             "ssm_out_g", "w_out", "norm2_g", "w_router", "router_bias", "w_exp_gate", "w_exp_up", "w_exp_down"]


def make_in_maps(inputs, cores):
    maps = []
    shared = {k: np.ascontiguousarray(np.asarray(inputs[k], dtype=np.float32)) for k in _IN_NAMES}
    for b in cores:
        m = dict(shared)
        m["x"] = np.ascontiguousarray(np.asarray(inputs["x"][b], dtype=np.float32))
        m["c"] = np.ascontiguousarray(np.asarray(inputs["c"][b], dtype=np.float32).reshape(8, 128))
        m["positions"] = np.ascontiguousarray(np.asarray(inputs["positions"][b], dtype=np.int32))
        maps.append(m)
    return maps


def kernel(**inputs):
    nc = build()
    in_maps = make_in_maps(inputs, list(range(8)))
    res = run_bass_kernel_spmd(nc, in_maps, core_ids=list(range(8)))
    return np.stack([np.asarray(r["y"], dtype=np.float32) for r in res.results], axis=0)
```

```python
import math
import contextlib
import numpy as np
import concourse.bass as bass
import concourse.mybir as mybir
from concourse.bass_utils import run_bass_kernel_spmd

F32 = mybir.dt.float32
BF16 = mybir.dt.bfloat16
I32 = mybir.dt.int32
AF = mybir.ActivationFunctionType
ALU = mybir.AluOpType
AX = mybir.AxisListType

DEPTH = 2
S = 2048
D = 1024
NT = 16
EPS = 1e-6
NEG = -30000.0
TWO_PI = 2.0 * math.pi
import os
SELF_SYNC = set(os.environ.get("KSELF", "dve").split(","))


class KB:
    def __init__(self, nc, es):
        self.nc = nc
        self.es = es
        self.E = {"pe": nc.tensor, "act": nc.scalar, "dve": nc.vector, "pool": nc.gpsimd, "sp": nc.sync}
        self.sems = {}
        self.val = {}
        self.seen = {e: {} for e in self.E}
        self.lastw = {}
        self.readers = {}
        for e in ("pe", "act", "dve", "pool"):
            self._sem(e)

    def _sem(self, name):
        if name not in self.sems:
            self.sems[name] = self.es.enter_context(self.nc.semaphore("s_" + name.replace(":", "_")))
            self.val[name] = 0
        return self.sems[name]

    def _deps(self, R, W):
        deps = {}

        def need(d):
            if d is None:
                return
            s, v = d
            if v > deps.get(s, 0):
                deps[s] = v

        for k in R:
            need(self.lastw.get(k))
        for k in W:
            need(self.lastw.get(k))
            for r in self.readers.get(k, ()):
                need(r)
        return deps

    def _wait(self, eng, deps):
        E = self.E[eng]
        for s, v in deps.items():
            if s == eng and (eng == "pe" or eng not in SELF_SYNC):
                continue
            if self.seen[eng].get(s, 0) >= v:
                continue
            E.wait_ge(self.sems[s], v)
            self.seen[eng][s] = v

    def _record(self, tok, R, W):
        for k in R:
            self.readers.setdefault(k, []).append(tok)
        for k in W:
            self.lastw[k] = tok
            self.readers[k] = []

    def op(self, eng, fn, R=(), W=(), sig=True):
        self._wait(eng, self._deps(R, W))
        inst = fn(self.E[eng])
        if sig:
            self.val[eng] += 1
            inst.then_inc(self.sems[eng], 1)
            tok = (eng, self.val[eng])
        else:
            tok = (eng, self.val[eng] + 1)
        self._record(tok, R, W)
        return inst

    def dma(self, q, out, in_, R=(), W=(), dkey=None, **kw):
        name = "d:" + dkey
        self._sem(name)
        self._wait(q, self._deps(R, W))
        inst = self.E[q].dma_start(out=out, in_=in_, **kw)
        self.val[name] += 16
        inst.then_inc(self.sems[name], 16)
        self._record((name, self.val[name]), R, W)
        return inst

    def barrier(self):
        for eng in self.E:
            deps = {s: v for s, v in self.val.items() if v > 0}
            self._wait(eng, deps)
        self.lastw.clear()
        self.readers.clear()

    def final_wait(self, eng="sp"):
        deps = {s: v for s, v in self.val.items() if v > 0 and s.startswith("d:")}
        self._wait(eng, deps)


def run_rr(gens, width, lag=0):
    it = iter(gens)
    active = []
    steps_newest = 10 ** 9
    done = False
    while True:
        if not done and len(active) < width and steps_newest >= lag:
            try:
                active.append(next(it))
                steps_newest = 0
            except StopIteration:
                done = True
        if not active:
            if done:
                break
            steps_newest = 10 ** 9
            continue
        for g in list(active):
            try:
                next(g)
            except StopIteration:
                active.remove(g)
        steps_newest += 1


def build(depth=DEPTH, debug=None):
    nc = bass.Bass("TRN2", target_bir_lowering=False)

    def din(name, shape, dt=F32):
        return nc.dram_tensor(name, list(shape), dt, kind="ExternalInput").ap()

    x_d = din("x", [S, D])
    c_d = din("c", [8, 128])
    pos_d = din("positions", [S], I32)
    ada_w = din("ada_w", [DEPTH, D, 6 * D])
    ada_b = din("ada_b", [DEPTH, 6 * D])
    norm1_g = din("norm1_g", [DEPTH, D])
    w_in = din("w_in", [DEPTH, D, 1280])
    q_norm_g = din("q_norm_g", [DEPTH, 64])
    k_norm_g = din("k_norm_g", [DEPTH, 64])
    attn_sink = din("attn_sink", [DEPTH, 8])
    lam_re = din("lam_re", [DEPTH, 32, 64])
    lam_im = din("lam_im", [DEPTH, 32, 64])
    ssm_b_re = din("ssm_b_re", [DEPTH, 32, 64, 16])
    ssm_b_im = din("ssm_b_im", [DEPTH, 32, 64, 16])
    ssm_c_re = din("ssm_c_re", [DEPTH, 32, 16, 64])
    ssm_c_im = din("ssm_c_im", [DEPTH, 32, 16, 64])
    ssm_d = din("ssm_d", [DEPTH, 512])
    ssm_log_dt = din("ssm_log_dt", [DEPTH, 32])
    w_glu = din("w_glu", [DEPTH, 512, 512])
    attn_out_g = din("attn_out_g", [DEPTH, 512])
    ssm_out_g = din("ssm_out_g", [DEPTH, 512])
    w_out = din("w_out", [DEPTH, D, D])
    norm2_g = din("norm2_g", [DEPTH, D])
    w_router = din("w_router", [D, 16])
    router_bias = din("router_bias", [16])
    w_exp_gate = din("w_exp_gate", [DEPTH, 16, D, 512])
    w_exp_up = din("w_exp_up", [DEPTH, 16, D, 512])
    w_exp_down = din("w_exp_down", [DEPTH, 16, 512, D])
    y_d = nc.dram_tensor("y", [S, D], F32, kind="ExternalOutput").ap()
    dbg_d = None
    if debug is not None:
        dbg_d = nc.dram_tensor("dbg", [128, debug[1]], debug[2], kind="ExternalOutput").ap()

    with contextlib.ExitStack() as es:
        kb = KB(nc, es)
        op = kb.op

        def sb(name, shape, dt):
            return es.enter_context(nc.sbuf_tensor(name, list(shape), dt))

        X = sb("X", [128, NT, D], F32)
        FT = sb("FT", [128, 8 * S], BF16)
        AT = sb("AT", [128, 4 * S], BF16)
        BIG = sb("BIG", [128, 12288], F32)
        MOD = sb("MOD", [128, 6, D], BF16)
        SM = sb("SM", [128, 2560], F32)
        ident_f = sb("ident_f", [128, 128], F32)
        ident_b = sb("ident_b", [128, 128], BF16)
        colI = sb("colI", [128, 128], F32)
        rowI = sb("rowI", [128, 128], F32)
        MBp = sb("MBp", [128, 4, 128], BF16)
        MBc = sb("MBc", [128, 4, 128], BF16)
        COS = sb("COS", [128, NT, 32], F32)
        SIN = sb("SIN", [128, NT, 32], F32)
        SCrep = sb("SCrep", [128, 8, 128], BF16)
        TMP4 = sb("TMP4", [128, 1024], F32)
        TMP4b = sb("TMP4b", [128, 1024], F32)
        SS = sb("SS", [128, 64], F32)
        NHALF = sb("NHALF", [128, 64], F32)
        GQK = sb("GQK", [128, 10, 64], BF16)
        ESINK = sb("ESINK", [128, 8], F32)
        GROW = sb("GROW", [128, 8], F32)
        MASKM = sb("MASKM", [128, 256], BF16)
        DIAG0 = sb("DIAG0", [128, 256], BF16)
        REP16 = sb("REP16", [16, 128], F32)
        SGN = sb("SGN", [128, 2], F32)
        DCOL = sb("DCOL", [128, 32], F32)
        LAB = sb("LAB", [128, 2, 2, 2, 16], F32)
        WR = sb("WR", [128, 8, 16], BF16)
        RB = sb("RB", [128, 16], F32)
        SELE = sb("SELE", [16, 2, 128], BF16)

        PSALL = es.enter_context(nc.psum_tensor("psall", [128, 4096], F32))
        ps = [PSALL[:, 512 * i:512 * (i + 1)] for i in range(8)]

        def psb(i):
            return ps[i].bitcast(BF16)

        def big_bf(w0, w1):
            return BIG[:, w0:w1].bitcast(BF16)

        FT3 = FT[:].rearrange("p (k t) -> p k t", k=8)
        AT3 = AT[:].rearrange("p (k t) -> p k t", k=4)

        op("pool", lambda e: e.iota(colI[:], [[1, 128]], base=0, channel_multiplier=0,
                                    allow_small_or_imprecise_dtypes=True), W=["colI"])
        op("pool", lambda e: e.iota(rowI[:], [[0, 128]], base=0, channel_multiplier=1,
                                    allow_small_or_imprecise_dtypes=True), W=["rowI"])
        op("pool", lambda e: e.memset(NHALF[:], -0.5), W=["NHALF"])
        op("dve", lambda e: e.tensor_tensor(ident_f[:], colI[:], rowI[:], op=ALU.is_equal),
           R=["colI", "rowI"], W=["ident_f"])
        op("dve", lambda e: e.tensor_copy(ident_b[:], ident_f[:]), R=["ident_f"], W=["ident_b"])
        op("dve", lambda e: e.tensor_tensor(TMP4[:, 0:128], colI[:], rowI[:], op=ALU.is_ge),
           R=["colI", "rowI"], W=["TMP4"])
        op("dve", lambda e: e.tensor_scalar(MBp[:], TMP4[:, 0:128].unsqueeze(1).to_broadcast([128, 4, 128]),
                                            NEG, None, op0=ALU.mult), R=["TMP4"], W=["MBp"])
        op("dve", lambda e: e.tensor_tensor(TMP4[:, 128:256], colI[:], rowI[:], op=ALU.is_lt),
           R=["colI", "rowI"], W=["TMP4"])
        op("dve", lambda e: e.tensor_scalar(MBc[:], TMP4[:, 128:256].unsqueeze(1).to_broadcast([128, 4, 128]),
                                            NEG, None, op0=ALU.mult), R=["TMP4"], W=["MBc"])


        def _ssm_consts():
            PI_ = SM[:, 340:341].bitcast(I32)
            JJF = SM[:, 341:342]
            HPF = SM[:, 342:343]
            TI = SM[:, 344:345].bitcast(I32)
            IIF = TMP4[:, 0:256]
            HF = TMP4[:, 256:512]
            DLF = TMP4[:, 512:768]
            E1 = TMP4b[:, 0:256]
            E2 = TMP4b[:, 256:512]
            op("pool", lambda e: e.iota(PI_, [[0, 1]], base=0, channel_multiplier=1), W=["c_pi"])
            op("dve", lambda e: e.tensor_single_scalar(TI, PI_, 4, op=ALU.arith_shift_right), R=["c_pi"], W=["c_ti"])
            op("dve", lambda e: e.tensor_copy(JJF, TI), R=["c_ti"], W=["c_jj"])
            op("dve", lambda e: e.tensor_single_scalar(TI, PI_, 15, op=ALU.bitwise_and), R=["c_pi", "c_jj"], W=["c_ti"])
            op("dve", lambda e: e.tensor_copy(HPF, TI), R=["c_ti"], W=["c_hp"])
            op("pool", lambda e: e.iota(IIF.rearrange("p (a b c) -> p a b c", a=2, b=8), [[0, 2], [1, 8], [0, 16]],
                                        base=0, channel_multiplier=0, allow_small_or_imprecise_dtypes=True),
               W=["TMP4"])
            op("pool", lambda e: e.iota(HF.rearrange("p (a b c) -> p a b c", a=2, b=8), [[0, 2], [0, 8], [1, 16]],
                                        base=0, channel_multiplier=0, allow_small_or_imprecise_dtypes=True),
               W=["TMP4"])
            op("pool", lambda e: e.iota(DLF.rearrange("p (a b c) -> p a b c", a=2, b=8), [[1, 2], [0, 8], [0, 16]],
                                        base=0, channel_multiplier=0, allow_small_or_imprecise_dtypes=True),
               W=["TMP4"])
            op("dve", lambda e: e.tensor_scalar(E1, IIF, JJF, None, op0=ALU.is_ge), R=["TMP4", "c_jj"], W=["TMP4b"])
            op("dve", lambda e: e.tensor_tensor(MASKM[:], E1, DLF, op=ALU.max), R=["TMP4b", "TMP4"], W=["MASKM"])
            op("dve", lambda e: e.tensor_scalar(E1, IIF, JJF, None, op0=ALU.is_equal), R=["TMP4", "c_jj", "MASKM"],
               W=["TMP4b"])
            op("dve", lambda e: e.tensor_scalar(E2, HF, HPF, None, op0=ALU.is_equal), R=["TMP4", "c_hp"], W=["TMP4b"])
            op("dve", lambda e: e.tensor_tensor(E1, E1, E2, op=ALU.mult), R=["TMP4b"], W=["TMP4b"])
            op("dve", lambda e: e.tensor_scalar(E2, DLF, -1.0, 1.0, op0=ALU.mult, op1=ALU.add), R=["TMP4"], W=["TMP4b"])
            op("dve", lambda e: e.tensor_tensor(DIAG0[:], E1, E2, op=ALU.mult), R=["TMP4b"], W=["DIAG0"])
            op("dve", lambda e: e.tensor_copy(REP16[:].rearrange("k (j h) -> k j h", j=8),
                                              ident_f[0:16, 0:16].unsqueeze(1).to_broadcast([16, 8, 16])),
               R=["ident_f"], W=["REP16"])
            op("pool", lambda e: e.memset(SGN[0:64, 0:1], 1.0), W=["SGN"])
            op("pool", lambda e: e.memset(SGN[64:128, 0:1], -1.0), W=["SGN"])
            op("pool", lambda e: e.memset(SGN[0:64, 1:2], -1.0), W=["SGN"])
            op("pool", lambda e: e.memset(SGN[64:128, 1:2], 1.0), W=["SGN"])

        _ssm_consts()
        kb.dma("pool", WR[:], w_router.rearrange("(k p) e -> p k e", p=128), W=["WR"], dkey="wr")
        kb.dma("sp", RB[:], router_bias.rearrange("(o e) -> o e", o=1).partition_broadcast(128).rearrange("p o e -> p (o e)"),
               W=["RB"], dkey="rb")

        x_v = x_d.rearrange("(c i) d -> c i d", i=NT)
        for q4 in range(4):
            kb.dma("sp", X[:, 4 * q4:4 * q4 + 4, :], x_v[:, 4 * q4:4 * q4 + 4, :],
                   W=[("X", i) for i in range(4 * q4, 4 * q4 + 4)], dkey=f"x{q4}")

        POSI = SM[:, 0:16].bitcast(I32)
        with nc.allow_non_contiguous_dma(reason="tiny position load"):
            kb.dma("sp", POSI, pos_d.rearrange("(n p) -> p n", p=128), W=["POSI"], dkey="pos")
        POSF = SM[:, 16:32]
        FREQ = SM[:, 32:64]
        ANG = TMP4[:, 0:512].rearrange("p (n k) -> p n k", k=32)
        ANG2 = TMP4[:, 512:1024].rearrange("p (n k) -> p n k", k=32)
        KI = TMP4b[:, 0:512].bitcast(I32).rearrange("p (n k) -> p n k", k=32)
        KF = TMP4b[:, 512:1024].rearrange("p (n k) -> p n k", k=32)
        op("dve", lambda e: e.tensor_copy(POSF, POSI), R=["POSI"], W=["POSF"])
        op("pool", lambda e: e.iota(FREQ, [[1, 32]], base=0, channel_multiplier=0,
                                    allow_small_or_imprecise_dtypes=True), W=["FREQ"])
        op("act", lambda e: e.activation(out=FREQ, in_=FREQ, func=AF.Exp, scale=-math.log(10000.0) / 32.0),
           R=["FREQ"], W=["FREQ"])
        op("dve", lambda e: e.tensor_tensor(ANG, POSF.unsqueeze(2).to_broadcast([128, NT, 32]),
                                            FREQ.unsqueeze(1).to_broadcast([128, NT, 32]), op=ALU.mult),
           R=["POSF", "FREQ"], W=["TMP4"])

        def sin_table(dst, src_ang, shift, keyname):
            op("dve", lambda e: e.tensor_scalar(ANG2, src_ang, shift, None, op0=ALU.add), R=["TMP4"], W=["TMP4"])
            op("dve", lambda e: e.tensor_scalar(KF, ANG2, 1.0 / TWO_PI, None, op0=ALU.mult), R=["TMP4"], W=["TMP4b"])
            op("dve", lambda e: e.tensor_copy(KI, KF), R=["TMP4b"], W=["TMP4b"])
            op("dve", lambda e: e.tensor_copy(KF, KI), R=["TMP4b"], W=["TMP4b"])
            op("dve", lambda e: e.scalar_tensor_tensor(ANG2, KF, -TWO_PI, ANG2, op0=ALU.mult, op1=ALU.add),
               R=["TMP4b", "TMP4"], W=["TMP4"])
            op("dve", lambda e: e.tensor_scalar(ANG2, ANG2, -3.1415925, 3.1415925, op0=ALU.max, op1=ALU.min),
               R=["TMP4"], W=["TMP4"])
            op("act", lambda e: e.activation(out=dst, in_=ANG2, func=AF.Sin), R=["TMP4"], W=[keyname])

        sin_table(SIN[:], ANG, 0.0, "SIN")
        sin_table(COS[:], ANG, math.pi / 2.0, "COS")

        C8 = SM[0:8, 64:192]
        kb.dma("sp", C8, c_d, W=["C8"], dkey="c8")
        op("pe", lambda e: e.matmul(ps[0][:, 0:8], C8, ident_f[0:8, 0:8], start=True, stop=True),
           R=["C8", "ident_f"], W=[("ps", 0)])
        SCT = SM[:, 192:200]
        op("act", lambda e: e.activation(out=SCT, in_=ps[0][:, 0:8], func=AF.Silu), R=[("ps", 0)], W=["SCT"])
        op("dve", lambda e: e.tensor_copy(SCrep[:], SCT.unsqueeze(2).to_broadcast([128, 8, 128])),
           R=["SCT"], W=["SCrep"])

        def ring_slot(s):
            return big_bf(2048 * s, 2048 * (s + 1))

        def adaln(l):
            for blk in range(6):
                if blk in (1, 4):
                    ng = (norm1_g if blk == 1 else norm2_g)
                    kb.dma("sp", TMP4b[:], ng[l:l + 1, :].partition_broadcast(128).rearrange("p o n -> p (o n)"),
                           W=["TMP4b"], dkey="ngbc")
                BIASB = TMP4[:] if blk % 2 == 0 else SM[:, 1024:2048]
                bkey = "TMP4" if blk % 2 == 0 else "SMbias"
                kb.dma("sp", BIASB, ada_b[l:l + 1, blk * D:(blk + 1) * D].partition_broadcast(128)
                       .rearrange("p o n -> p (o n)"), W=[bkey], dkey="adb%d" % (blk % 2))
                for half in range(2):
                    s = (blk * 2 + half) % 4
                    slot = ring_slot(s).rearrange("p (k n) -> p k n", k=8)
                    col0 = blk * D + half * 512
                    kb.dma("pool", slot, ada_w[l, :, col0:col0 + 512].rearrange("(k p) n -> p k n", p=128),
                           W=[("ring", s)], dkey=f"ring{s}")
                    pb = 2 + (s % 2)
                    for k in range(8):
                        op("pe", lambda e, k=k: e.matmul(ps[pb][:], SCrep[:, k, :], slot[:, k, :],
                                                         start=(k == 0), stop=(k == 7)),
                           R=["SCrep", ("ring", s)], W=[("ps", pb)], sig=(k == 7))
                    dst = MOD[:, blk, half * 512:(half + 1) * 512]
                    bias = BIASB[:, half * 512:(half + 1) * 512]
                    if blk in (1, 4):
                        tmp = SM[:, 512:1024]
                        op("dve", lambda e: e.tensor_tensor(tmp, ps[pb][:], bias, op=ALU.add),
                           R=[("ps", pb), bkey], W=["SMtmp"])
                        op("dve", lambda e: e.scalar_tensor_tensor(dst, tmp, 1.0, TMP4b[:, half * 512:(half + 1) * 512],
                                                                   op0=ALU.add, op1=ALU.mult),
                           R=["SMtmp", "TMP4b"], W=[("MOD", blk)])
                    else:
                        op("dve", lambda e: e.tensor_tensor(dst, ps[pb][:], bias, op=ALU.add),
                           R=[("ps", pb), bkey], W=[("MOD", blk)])

        def norm_to_FT(l, which, pos_of_tile, okey="FT", pbanks=(0, 1)):
            a_i, b_i = (1, 0) if which == 0 else (4, 3)
            junk = SM[:, 2048:2560].bitcast(BF16)
            for i in range(NT):
                op("act", lambda e, i=i: e.activation(out=junk, in_=X[:, i, :], func=AF.Square,
                                                      accum_out=SS[:, i:i + 1]),
                   R=[("X", i)], W=["junk", ("SS", i)])
            op("dve", lambda e: e.tensor_scalar(SS[:, 16:32], SS[:, 0:16], 1.0 / D, EPS, op0=ALU.mult, op1=ALU.add),
               R=[("SS", i) for i in range(NT)], W=["SSb"])
            op("pool", lambda e: e.tensor_tensor(SS[:, 32:48], SS[:, 16:32], NHALF[:, 0:16], op=ALU.pow),
               R=["SSb", "NHALF"], W=["RSTD"])
            def tile(i):
                xs = i % 2
                t32 = TMP4[:] if xs == 0 else TMP4b[:]
                tkey = "TMP4" if xs == 0 else "TMP4b"
                xn = SM[:, 1024 + 512 * xs:1024 + 512 * (xs + 1)].bitcast(BF16)
                op("dve", lambda e, i=i: e.scalar_tensor_tensor(t32, X[:, i, :], SS[:, 32 + i:33 + i], MOD[:, a_i, :],
                                                                op0=ALU.mult, op1=ALU.mult),
                   R=[("X", i), "RSTD", ("MOD", a_i)], W=[tkey])
                yield
                op("dve", lambda e: e.tensor_tensor(xn, t32, MOD[:, b_i, :], op=ALU.add),
                   R=[tkey, ("MOD", b_i)], W=[("xn", xs)])
                yield
                pb = pbanks[xs]
                for k in range(8):
                    op("pe", lambda e, k=k: e.transpose(psb(pb)[:, k * 128:(k + 1) * 128],
                                                        xn[:, k * 128:(k + 1) * 128], ident_b[:]),
                       R=[("xn", xs), "ident_b"], W=[("ps", pb)], sig=(k == 7))
                    yield
                dst = pos_of_tile(i)
                op("act", lambda e: e.activation(out=dst, in_=psb(pb).rearrange("p (k c) -> p k c", k=8),
                                                 func=AF.Copy),
                   R=[("ps", pb)], W=[(okey, i)])
                yield

            run_rr([tile(i) for i in range(NT)], 2, 5)

        WIN3 = big_bf(0, 5120).rearrange("p (k n) -> p k n", k=8)
        UCM = big_bf(8192, 12288).rearrange("p (j f) -> p j f", j=16)
        UCMg = big_bf(8192, 12288).rearrange("p (g j h) -> p g j h", g=32, j=16)

        def layer_loads(l):
            kb.dma("pool", WIN3, w_in[l].rearrange("(k p) n -> p k n", p=128), W=["WIN"], dkey="win")
            QG = SM[:, 200:264]
            KG = SM[:, 264:328]
            SK = SM[:, 328:336]
            kb.dma("sp", QG, q_norm_g[l:l + 1, :].partition_broadcast(128).rearrange("p o n -> p (o n)"),
                   W=["QG"], dkey="qg")
            kb.dma("sp", KG, k_norm_g[l:l + 1, :].partition_broadcast(128).rearrange("p o n -> p (o n)"),
                   W=["KG"], dkey="kg")
            kb.dma("sp", SK, attn_sink[l:l + 1, :].partition_broadcast(128).rearrange("p o n -> p (o n)"),
                   W=["SK"], dkey="sk")
            op("dve", lambda e: e.tensor_copy(GQK[:, 0:8, :], QG.unsqueeze(1).to_broadcast([128, 8, 64])),
               R=["QG"], W=["GQK"])
            op("dve", lambda e: e.tensor_copy(GQK[:, 8:10, :], KG.unsqueeze(1).to_broadcast([128, 2, 64])),
               R=["KG"], W=["GQK"])
            op("act", lambda e: e.activation(out=ESINK[:], in_=SK, func=AF.Exp), R=["SK"], W=["ESINK"])
            with nc.allow_non_contiguous_dma(reason="tiny gain vectors"):
                kb.dma("sp", GROW[:, 0:4], attn_out_g[l].rearrange("(k p) -> p k", p=128), W=["GROW"], dkey="grow")
                kb.dma("sp", GROW[:, 4:8], ssm_out_g[l].rearrange("(k p) -> p k", p=128), W=["GROW"], dkey="grow")

        def u_proj(l):
            for j in range(NT):
                pb = 2 + (j % 2)
                for k in range(8):
                    op("pe", lambda e, k=k: e.matmul(ps[pb][:], FT3[:, k, j::16], WIN3[:, k, 768:1280],
                                                     start=(k == 0), stop=(k == 7)),
                       R=[("FT", j), "WIN"], W=[("ps", pb)], sig=(k == 7))
                eng = "act" if j % 2 == 0 else "dve"
                if eng == "act":
                    op("act", lambda e: e.activation(out=UCMg[:, :, j, :],
                                                     in_=ps[pb][:].rearrange("p (g h) -> p g h", g=32), func=AF.Copy),
                       R=[("ps", pb)], W=[("UCM", j)])
                else:
                    op("dve", lambda e: e.tensor_copy(UCMg[:, :, j, :], ps[pb][:].rearrange("p (g h) -> p g h", g=32)),
                       R=[("ps", pb)], W=[("UCM", j)])

        def attention(l):
            KT = big_bf(5120, 7168)[0:64, :].rearrange("p (n h t) -> p n h t", n=16, h=2)
            PT = [big_bf(7168 + 256 * j, 7168 + 256 * (j + 1)) for j in range(4)]
            QK = SM[:, 0:640]
            T1 = SM[:, 640:1280]
            T2 = SM[:, 1280:1920]
            QKN = SM[:, 1920:2240].bitcast(BF16)
            ST10 = SM[:, 2240:2250]
            RS10 = SM[:, 2250:2260]
            DEN = SM[:, 2260:2268]
            RDEN = SM[:, 2268:2276]
            ASS = SM[:, 2276:2277]
            ARS = SM[:, 2278:2279]
            QT = [TMP4[0:64, 512 * j:512 * (j + 1)].bitcast(BF16) for j in range(2)]
            ATT = TMP4b[:, 0:512]
            AN = TMP4b[:, 512:768].bitcast(BF16)
            VA = [TMP4b[:, 768 + 66 * j:768 + 66 * (j + 1)].bitcast(BF16).rearrange("p (h d) -> p h d", h=2)
                  for j in range(3)]
            ALLFT = [("FT", i) for i in range(NT)]
            for j in range(3):
                op("pool", lambda e, j=j: e.memset(VA[j][:, :, 64:66], 1.0), W=[("VA1", j)])
            T1v = T1.rearrange("p (h t d) -> p h t d", h=10, t=2)
            T2v = T2.rearrange("p (h t d) -> p h t d", h=10, t=2)
            def front(n):
                qs = n % 2
                for k in range(8):
                    op("pe", lambda e, k=k: e.matmul(ps[4][:], FT3[:, k, 128 * n:128 * n + 128], WIN3[:, k, 0:512],
                                                     start=(k == 0), stop=(k == 7)),
                       R=ALLFT + ["WIN"], W=[("ps", 4)], sig=(k == 7))
                    yield
                for k in range(8):
                    op("pe", lambda e, k=k: e.matmul(ps[5][:, 0:256], FT3[:, k, 128 * n:128 * n + 128],
                                                     WIN3[:, k, 512:768], start=(k == 0), stop=(k == 7)),
                       R=ALLFT + ["WIN"], W=[("ps", 5)], sig=(k == 7))
                    yield
                op("act", lambda e: e.activation(out=QK[:, 0:512], in_=ps[4][:], func=AF.Copy),
                   R=[("ps", 4)], W=["QK"])
                yield
                op("act", lambda e: e.activation(out=QK[:, 512:640], in_=ps[5][:, 0:128], func=AF.Copy),
                   R=[("ps", 5)], W=["QK"])
                yield
                op("act", lambda e: e.activation(out=VA[n % 3][:, :, 0:64],
                                                 in_=ps[5][:, 128:256].rearrange("p (h d) -> p h d", h=2),
                                                 func=AF.Copy),
                   R=[("ps", 5)], W=[("VA", n % 3)])
                yield
                junkq = T2[:, 0:32].bitcast(BF16)
                for h in range(10):
                    op("act", lambda e, h=h: e.activation(out=junkq, in_=QK[:, h * 64:(h + 1) * 64], func=AF.Square,
                                                          accum_out=ST10[:, h:h + 1]), R=["QK"], W=["T2", ("ST10", h)])
                    yield
                op("pool", lambda e: e.tensor_scalar(ST10, ST10, 1.0 / 64.0, EPS, op0=ALU.mult, op1=ALU.add),
                   R=[("ST10", h) for h in range(10)], W=["ST10"])
                yield
                op("pool", lambda e: e.tensor_tensor(RS10, ST10, NHALF[:, 0:10], op=ALU.pow),
                   R=["ST10", "NHALF"], W=["RS10"])
                yield
                op("pool", lambda e: e.tensor_tensor(T1, QK, GQK[:].rearrange("p h d -> p (h d)"), op=ALU.mult),
                   R=["QK", "GQK"], W=["T1"])
                yield
                sin_b = SIN[:, n, :].unsqueeze(1).to_broadcast([128, 10, 32])
                cos_b = COS[:, n, :].unsqueeze(1).unsqueeze(1).to_broadcast([128, 10, 2, 32])
                op("pool", lambda e: e.tensor_tensor(T2v[:, :, 0, :], T1v[:, :, 1, :], sin_b, op=ALU.mult),
                   R=["T1", "SIN"], W=["T2"])
                yield
                op("pool", lambda e: e.tensor_tensor(T2v[:, :, 1, :], T1v[:, :, 0, :], sin_b, op=ALU.mult),
                   R=["T1", "SIN"], W=["T2"])
                yield
                op("pool", lambda e: e.tensor_tensor(T1v, T1v, cos_b, op=ALU.mult), R=["T1", "COS", "T2"], W=["T1"])
                yield
                op("pool", lambda e: e.tensor_tensor(T1v[:, :, 0, :], T1v[:, :, 0, :], T2v[:, :, 0, :],
                                                     op=ALU.subtract), R=["T1", "T2"], W=["T1"])
                yield
                op("pool", lambda e: e.tensor_tensor(T1v[:, :, 1, :], T1v[:, :, 1, :], T2v[:, :, 1, :],
                                                     op=ALU.add), R=["T1", "T2"], W=["T1"])
                yield
                op("pool", lambda e: e.tensor_tensor(QKN.rearrange("p (h d) -> p h d", h=10),
                                                     T1.rearrange("p (h d) -> p h d", h=10),
                                                     RS10.unsqueeze(2).to_broadcast([128, 10, 64]), op=ALU.mult),
                   R=["T1", "RS10"], W=["QKN"])
                yield
                for h in range(8):
                    op("pe", lambda e, h=h: e.transpose(psb(0)[0:64, h * 128:(h + 1) * 128],
                                                        QKN[:, h * 64:(h + 1) * 64], ident_b[:]),
                       R=["QKN", "ident_b"], W=[("ps", 0)], sig=(h == 7))
                    yield
                for h in range(2):
                    op("pe", lambda e, h=h: e.transpose(psb(0)[64:128, h * 128:(h + 1) * 128],
                                                        QKN[:, 512 + h * 64:512 + (h + 1) * 64], ident_b[:]),
                       R=["QKN", "ident_b"], W=[("ps", 0)], sig=(h == 1))
                    yield
                op("act", lambda e: e.activation(out=QT[qs], in_=psb(0)[0:64, :], func=AF.Copy),
                   R=[("ps", 0)], W=[("QT", qs)])
                yield
                op("act", lambda e: e.activation(out=KT[:, n, :, :],
                                                 in_=psb(0)[64:128, 0:256].rearrange("p (h t) -> p h t", h=2),
                                                 func=AF.Copy),
                   R=[("ps", 0)], W=[("KT", n)])
                yield

            def back(n):
                qs = n % 2
                QT3 = QT[qs].rearrange("p (h t) -> p h t", h=8)
                halves = ([(n - 1, MBp)] if n > 0 else []) + [(n, MBc)]
                for kvh in range(2):
                    for hi, (nb, MB) in enumerate(halves):
                        cidx = 2 * kvh + hi
                        sbk = 2 + (cidx % 2)
                        pj = cidx % 4
                        op("pe", lambda e: e.matmul(ps[sbk][:], KT[:, nb, kvh, :],
                                                    QT3[:, 4 * kvh:4 * kvh + 4, :], start=True, stop=False),
                           R=[("KT", nb), ("QT", qs)], W=[("ps", sbk)], sig=False)
                        yield
                        op("pe", lambda e: e.matmul(ps[sbk][:], ident_b[:], MB[:].rearrange("p h q -> p (h q)"),
                                                    start=False, stop=True),
                           R=["ident_b", "MB"], W=[("ps", sbk)])
                        yield
                        op("act", lambda e: e.activation(out=PT[pj], in_=ps[sbk][:], func=AF.Exp, scale=0.125),
                           R=[("ps", sbk)], W=[("PT", pj)])
                        yield
                        for h in range(4):
                            op("pe", lambda e, h=h: e.matmul(ps[6 + kvh][:, h * 65:h * 65 + 65],
                                                             PT[pj][:, h * 128:(h + 1) * 128],
                                                             VA[nb % 3][:, kvh, 0:65],
                                                             start=(hi == 0 and h == 0), stop=(hi == len(halves) - 1 and h == 3),
                                                             skip_group_check=True),
                               R=[("PT", pj), ("VA", nb % 3), ("VA1", nb % 3)], W=[("ps", 6 + kvh)], sig=(h == 3))
                            yield
                pv2 = PSALL[:, 3072:4096].rearrange("p (b x) -> p b x", b=2)[:, :, 0:260].rearrange(
                    "p b (h d) -> p b h d", h=4)
                k67 = [("ps", 6), ("ps", 7)]
                op("dve", lambda e: e.tensor_tensor(DEN.rearrange("p (b h o) -> p b h o", b=2, o=1), pv2[:, :, :, 64:65],
                                                    ESINK[:].rearrange("p (b h o) -> p b h o", b=2, o=1), op=ALU.add),
                   R=k67 + ["ESINK"], W=["DEN"])
                yield
                op("dve", lambda e: e.reciprocal(RDEN, DEN), R=["DEN"], W=["RDEN"])
                yield
                op("dve", lambda e: e.tensor_tensor(
                    ATT.rearrange("p (b h d) -> p b h d", b=2, h=4), pv2[:, :, :, 0:64],
                    RDEN.rearrange("p (b h) -> p b h", b=2).unsqueeze(3).to_broadcast([128, 2, 4, 64]), op=ALU.mult),
                   R=k67 + ["RDEN"], W=["ATT"])
                yield
                op("dve", lambda e: e.scalar_tensor_tensor(AN, ATT, 1.0, ATT, op0=ALU.mult, op1=ALU.mult, accum_out=ASS),
                   R=["ATT"], W=["AN", "ASS"])
                yield
                op("dve", lambda e: e.tensor_scalar(ASS, ASS, 1.0 / 512.0, EPS, op0=ALU.mult, op1=ALU.add),
                   R=["ASS"], W=["ASS"])
                yield
                op("pool", lambda e: e.tensor_tensor(ARS, ASS, NHALF[:, 0:1], op=ALU.pow),
                   R=["ASS", "NHALF"], W=["ARS"])
                yield
                op("dve", lambda e: e.tensor_scalar(AN, ATT, ARS, None, op0=ALU.mult), R=["ATT", "ARS"], W=["AN"])
                yield
                for k in range(4):
                    op("pe", lambda e, k=k: e.transpose(psb(1)[:, 512 + k * 128:512 + (k + 1) * 128],
                                                        AN[:, k * 128:(k + 1) * 128], ident_b[:]),
                       R=["AN", "ident_b"], W=[("ps", 1)], sig=(k == 3))
                    yield
                op("act", lambda e: e.activation(out=AT3[:, :, 128 * n:128 * n + 128],
                                                 in_=psb(1)[:, 512:1024].rearrange("p (k t) -> p k t", k=4),
                                                 func=AF.Copy),
                   R=[("ps", 1)], W=[("AT", n)])
                yield

            run_rr([front(0)], 1)
            for n in range(NT):
                gs = [back(n)] + ([front(n + 1)] if n + 1 < NT else [])
                run_rr(gs, 2)

        FTf = FT[:].bitcast(F32)
        BZCL = big_bf(0, 4096).rearrange("p (g a m) -> p g a m", g=32, a=2)
        MW = big_bf(4096, 8192).rearrange("p (g a m) -> p g a m", g=32, a=2)
        UT = FT[:, 0:8192].rearrange("p (g a c) -> p g a c", g=32, a=2)
        ZS = FTf[:, 4096:8192].rearrange("p (r g c) -> p r g c", r=2, g=16)
        XSB = SM[:, 0:2048].bitcast(BF16).rearrange("p (g c) -> p g c", g=32)
        S34 = TMP4b[:].rearrange("p (gh t g h) -> p gh t g h", gh=2, t=2, g=16)

        def sincos(dst_s, dst_c, ang, t_a, t_k, t_ki, keys_ang, key_a, key_k, wkey):
            for dst, shift in ((dst_c, math.pi / 2.0), (dst_s, 0.0)):
                op("dve", lambda e: e.tensor_scalar(t_a, ang, shift, None, op0=ALU.add), R=keys_ang, W=[key_a])
                op("dve", lambda e: e.tensor_scalar(t_k, t_a, 1.0 / TWO_PI, None, op0=ALU.mult), R=[key_a], W=[key_k])
                op("dve", lambda e: e.tensor_copy(t_ki, t_k), R=[key_k], W=[key_k])
                op("dve", lambda e: e.tensor_copy(t_k, t_ki), R=[key_k], W=[key_k])
                op("dve", lambda e: e.scalar_tensor_tensor(t_a, t_k, -TWO_PI, t_a, op0=ALU.mult, op1=ALU.add),
                   R=[key_k, key_a], W=[key_a])
                op("dve", lambda e: e.tensor_scalar(t_a, t_a, -3.1415925, 3.1415925, op0=ALU.max, op1=ALU.min),
                   R=[key_a], W=[key_a])
                op("act", lambda e, dst=dst: e.activation(out=dst, in_=t_a, func=AF.Sin), R=[key_a], W=[wkey])

        def ssm_tables(l, gh, taus, PC, PS, tmpE, tmpA, tmpK, AB, ADT, ldsm):
            T = sum(c for _, _, c in taus)
            g0 = 16 * gh
            LL = ldsm[0:16, 0:256]
            kb.dma("sp", LL[:, 0:64], lam_re[l, g0:g0 + 16, :], W=["ldsm"], dkey="ll")
            kb.dma("sp", LL[:, 64:128], lam_re[l, g0:g0 + 16, :], W=["ldsm"], dkey="ll")
            kb.dma("sp", LL[:, 128:192], lam_im[l, g0:g0 + 16, :], W=["ldsm"], dkey="ll")
            kb.dma("sp", LL[:, 192:256], lam_im[l, g0:g0 + 16, :], W=["ldsm"], dkey="ll")
            op("pe", lambda e: e.matmul(ps[6][:, 0:16], LL[:, 0:128], ident_f[0:16, 0:16], start=True, stop=False),
               R=["ldsm", "ident_f"], W=[("ps", 6)])
            op("pe", lambda e: e.matmul(ps[6][:, 16:32], LL[:, 128:256], ident_f[0:16, 0:16], start=False, stop=True),
               R=["ldsm", "ident_f"], W=[("ps", 6)])
            op("act", lambda e: e.activation(out=AB[:, 0:32], in_=ps[6][:, 0:32], func=AF.Copy), R=[("ps", 6)], W=["AB"])
            DT = ADT[:, 32:48]
            kb.dma("sp", DT, ssm_log_dt[l:l + 1, g0:g0 + 16].partition_broadcast(128).rearrange("p o n -> p (o n)"),
                   W=["ADT"], dkey="dt")
            op("act", lambda e: e.activation(out=DT, in_=DT, func=AF.Exp), R=["ADT"], W=["ADT"])
            op("dve", lambda e: e.tensor_tensor(ADT[:, 0:16], AB[:, 0:16], DT, op=ALU.mult), R=["AB", "ADT"], W=["ADT"])
            op("dve", lambda e: e.tensor_tensor(ADT[:, 16:32], AB[:, 16:32], DT, op=ALU.mult), R=["AB", "ADT"], W=["ADT"])
            TAU = ADT[:, 48:48 + T]
            o = 0
            for (b0, st, c) in taus:
                op("pool", lambda e, o=o, b0=b0, st=st, c=c: e.iota(TAU[:, o:o + c], [[st, c]], base=b0,
                                                                     channel_multiplier=0,
                                                                     allow_small_or_imprecise_dtypes=True),
                   W=["ADT"])
                o += c
            sh = [128, 16, T]
            op("dve", lambda e: e.tensor_tensor(tmpE, ADT[:, 0:16].unsqueeze(2).to_broadcast(sh),
                                                TAU.unsqueeze(1).to_broadcast(sh), op=ALU.mult), R=["ADT"], W=["tmpE"])
            op("act", lambda e: e.activation(out=tmpE, in_=tmpE, func=AF.Exp), R=["tmpE"], W=["tmpE"])
            op("dve", lambda e: e.tensor_tensor(PS, ADT[:, 16:32].unsqueeze(2).to_broadcast(sh),
                                                TAU.unsqueeze(1).to_broadcast(sh), op=ALU.mult), R=["ADT"], W=["PS"])
            sincos(PS, PC, PS, tmpA, tmpK, tmpK.bitcast(I32), ["PS"], "tmpA", "tmpK", "PCS")
            op("dve", lambda e: e.tensor_tensor(PC, PC, tmpE, op=ALU.mult), R=["PCS", "tmpE"], W=["PC"])
            op("dve", lambda e: e.tensor_tensor(PS, PS, tmpE, op=ALU.mult), R=["PCS", "tmpE", "PS"], W=["PS"])

        def stack_load(l, gh, src_r, src_i, ld, ldkey, dst, dstkey, kind, psbank, scale_col):
            g0 = 16 * gh
            if kind == "C":
                L4 = ld[0:16, :].rearrange("g (h r p) -> g h r p", h=16, r=2)
                kb.dma("sp", L4[:, :, 0, :], src_r[l, g0:g0 + 16, :, :], W=[ldkey], dkey=ldkey)
                kb.dma("sp", L4[:, :, 1, :], src_i[l, g0:g0 + 16, :, :], W=[ldkey], dkey=ldkey)
                for h in range(16):
                    op("pe", lambda e, h=h: e.matmul(ps[psbank][:, h * 16:(h + 1) * 16],
                                                     L4[:, h, :, :].rearrange("g r p -> g (r p)"),
                                                     ident_f[0:16, 0:16], start=(h == 0), stop=(h == 15)),
                       R=[ldkey, "ident_f"], W=[("ps", psbank)], sig=(h == 15))
            else:
                L4 = ld[0:16, :].rearrange("g (r p h) -> g r p h", r=2, p=64)
                kb.dma("sp", L4[:, 0, :, :], src_r[l, g0:g0 + 16, :, :], W=[ldkey], dkey=ldkey)
                kb.dma("sp", L4[:, 1, :, :], src_i[l, g0:g0 + 16, :, :], W=[ldkey], dkey=ldkey)
                for h in range(16):
                    op("pe", lambda e, h=h: e.matmul(ps[psbank][:, h * 16:(h + 1) * 16],
                                                     L4[:, :, :, h].rearrange("g r p -> g (r p)"),
                                                     ident_f[0:16, 0:16], start=(h == 0), stop=(h == 15)),
                       R=[ldkey, "ident_f"], W=[("ps", psbank)], sig=(h == 15))
            op("dve", lambda e: e.tensor_scalar(dst, ps[psbank][:, 0:256].rearrange("p (h g) -> p g h", h=16),
                                                scale_col, None, op0=ALU.mult),
               R=[("ps", psbank), "SGN"], W=[dstkey])

        def outer_combine(dst_bf, PCv, PSv, Sa, Sb, nt, keysR, wkey, pbanks):
            sh = [128, 16, nt, 16]
            tmp = PSALL[:, 512 * pbanks:512 * pbanks + 16 * nt * 16].rearrange("p (g t h) -> p g t h", g=16, t=nt)
            pk = [("ps", pbanks + j) for j in range((16 * nt * 16 + 511) // 512)]
            op("dve", lambda e: e.tensor_tensor(tmp, PCv.unsqueeze(3).to_broadcast(sh),
                                                Sa.unsqueeze(2).to_broadcast(sh), op=ALU.mult), R=keysR, W=pk)
            op("pool", lambda e: e.tensor_tensor(dst_bf, PSv.unsqueeze(3).to_broadcast(sh),
                                                 Sb.unsqueeze(2).to_broadcast(sh), op=ALU.mult), R=keysR, W=[wkey])
            op("dve", lambda e: e.tensor_tensor(dst_bf, tmp, dst_bf, op=ALU.add), R=pk + [wkey], W=[wkey])

        def ssm_gen_A(l, gh):
            g0 = 16 * gh
            ldA = FTf[:, 0:2048]
            ldB = FTf[:, 2048:4096]
            ST = FTf[:, 4096:5632].rearrange("p (s g h) -> p s g h", s=6, g=16)
            BzT = FT[:, 2 * 5632:2 * 7680].rearrange("p (g a m) -> p g a m", g=16, a=2)
            CF = FT[:, 0:4096].rearrange("p (g a m) -> p g a m", g=16, a=2)
            PC = SM[:, 0:512].rearrange("p (g t) -> p g t", g=16)
            PS = SM[:, 512:1024].rearrange("p (g t) -> p g t", g=16)
            tmpE = SM[:, 1024:1536].rearrange("p (g t) -> p g t", g=16)
            tmpA = SM[:, 1536:2048].rearrange("p (g t) -> p g t", g=16)
            tmpK = SM[:, 2048:2560].rearrange("p (g t) -> p g t", g=16)
            AB = TMP4[:, 0:32]
            ADT = TMP4[:, 32:160]
            KAP = TMP4[:, 160:320]
            ssm_tables(l, gh, [(15, -1, 16), (-7, 1, 16)], PC, PS, tmpE, tmpA, tmpK, AB, ADT, TMP4[:, 512:768])
            S3p = S34[:, gh, 0, :, :]
            S4p = S34[:, gh, 1, :, :]
            stack_load(l, gh, ssm_c_re, ssm_c_im, ldA, "ldA", S3p, "S34", "C", 7, SGN[:, 0:1])
            stack_load(l, gh, ssm_c_im, ssm_c_re, ldB, "ldB", ST[:, 1], "ST1", "C", 6, -1.0)
            op("pool", lambda e: e.tensor_copy(S4p, ST[:, 1]), R=["ST1"], W=["S34"])
            stack_load(l, gh, ssm_b_re, ssm_b_im, ldA, "ldA", ST[:, 2], "ST2", "B", 7, 1.0)
            stack_load(l, gh, ssm_b_im, ssm_b_re, ldB, "ldB", ST[:, 3], "ST3", "B", 6, SGN[:, 1:2])
            a_ = AB[:, 0:16]
            b_ = AB[:, 16:32]
            L1r = PC[:, :, 24]
            L1i = PS[:, :, 24]
            nr, den, t1, t2, kr, ki = [KAP[:, 16 * j:16 * (j + 1)] for j in range(6)]
            op("dve", lambda e: e.tensor_scalar(nr, L1r, -1.0, None, op0=ALU.add), R=["PC"], W=["KAP"])
            op("dve", lambda e: e.tensor_tensor(den, a_, a_, op=ALU.mult), R=["AB"], W=["KAP"])
            op("dve", lambda e: e.tensor_tensor(t1, b_, b_, op=ALU.mult), R=["AB"], W=["KAP"])
            op("dve", lambda e: e.tensor_tensor(den, den, t1, op=ALU.add), R=["KAP"], W=["KAP"])
            op("dve", lambda e: e.reciprocal(den, den), R=["KAP"], W=["KAP"])
            op("dve", lambda e: e.tensor_tensor(t1, nr, a_, op=ALU.mult), R=["KAP", "AB"], W=["KAP"])
            op("dve", lambda e: e.tensor_tensor(t2, L1i, b_, op=ALU.mult), R=["PS", "AB"], W=["KAP"])
            op("dve", lambda e: e.tensor_tensor(t1, t1, t2, op=ALU.add), R=["KAP"], W=["KAP"])
            op("dve", lambda e: e.tensor_tensor(kr, t1, den, op=ALU.mult), R=["KAP"], W=["KAP"])
            op("dve", lambda e: e.tensor_tensor(t1, L1i, a_, op=ALU.mult), R=["PS", "AB", "KAP"], W=["KAP"])
            op("dve", lambda e: e.tensor_tensor(t2, nr, b_, op=ALU.mult), R=["KAP", "AB"], W=["KAP"])
            op("dve", lambda e: e.tensor_tensor(t1, t1, t2, op=ALU.subtract), R=["KAP"], W=["KAP"])
            op("dve", lambda e: e.tensor_tensor(ki, t1, den, op=ALU.mult), R=["KAP"], W=["KAP"])
            sh3 = [128, 16, 16]
            krb = kr.unsqueeze(2).to_broadcast(sh3)
            kib = ki.unsqueeze(2).to_broadcast(sh3)
            op("dve", lambda e: e.tensor_tensor(ST[:, 4], ST[:, 2], krb, op=ALU.mult), R=["ST2", "KAP"], W=["ST4"])
            op("dve", lambda e: e.tensor_tensor(ST[:, 0], ST[:, 3], kib, op=ALU.mult), R=["ST3", "KAP"], W=["ST0"])
            op("dve", lambda e: e.tensor_tensor(ST[:, 4], ST[:, 4], ST[:, 0], op=ALU.add), R=["ST4", "ST0"], W=["ST4"])
            op("dve", lambda e: e.tensor_tensor(ST[:, 5], ST[:, 3], krb, op=ALU.mult), R=["ST3", "KAP"], W=["ST5"])
            op("dve", lambda e: e.tensor_tensor(ST[:, 0], ST[:, 2], kib, op=ALU.mult), R=["ST2", "KAP", "ST4"], W=["ST0"])
            op("dve", lambda e: e.tensor_tensor(ST[:, 5], ST[:, 5], ST[:, 0], op=ALU.subtract), R=["ST5", "ST0"], W=["ST5"])
            outer_combine(BzT.rearrange("p g a (j h) -> p g (a j) h", h=16), PC[:, :, 0:16], PS[:, :, 0:16],
                          ST[:, 4], ST[:, 5], 16, ["PC", "PS", "ST4", "ST5"], "BzT", 0)
            outer_combine(CF.rearrange("p g a (j h) -> p g (a j) h", h=16), PC[:, :, 16:32], PS[:, :, 16:32],
                          S3p, S4p, 16, ["PC", "PS", "S34", "ldA", "ldB"], "ldA", 0)
            for gp in range(8):
                pb = 4 + (gp % 2)
                for j in range(4):
                    g, jb = 2 * gp + j // 2, j % 2
                    op("pe", lambda e, g=g, jb=jb, j=j: e.matmul(ps[pb][:, j * 128:(j + 1) * 128], BzT[:, g, jb, :],
                                                               ident_b[:], start=(j == 0), stop=(j == 3)),
                       R=["BzT", "ident_b"], W=[("ps", pb)], sig=(j == 3))
                op("act", lambda e: e.activation(
                    out=BZCL[:, g0 + 2 * gp:g0 + 2 * gp + 2, :, :].rearrange("p g a m -> p (g a m)"),
                    in_=ps[pb][:], func=AF.Copy), R=[("ps", pb)], W=["BZCL"])
            for g in range(16):
                pb = 6 + (g % 2)
                op("pe", lambda e, g=g: e.matmul(ps[pb][:, 0:256], BzT[:, g, 1, :],
                                                 CF[:, g, :, :].rearrange("p a m -> p (a m)"), start=True, stop=True),
                   R=["BzT", "ldA"], W=[("ps", pb)])
                tm = TMP4[:, 768:1024].bitcast(BF16)[:, 0:256] if g % 2 == 0 else TMP4[:, 896:1024].bitcast(BF16)
                tkey = "tmM0" if g % 2 == 0 else "tmM1"
                tm = TMP4[:, 768:896].bitcast(BF16) if g % 2 == 0 else TMP4[:, 896:1024].bitcast(BF16)
                op("dve", lambda e: e.tensor_tensor(tm, ps[pb][:, 0:256], MASKM[:], op=ALU.mult),
                   R=[("ps", pb), "MASKM"], W=[tkey])
                op("dve", lambda e, g=g: e.scalar_tensor_tensor(
                    MW[:, g0 + g, :, :].rearrange("p a m -> p (a m)"), DIAG0[:], DCOL[:, g0 + g:g0 + g + 1], tm,
                    op0=ALU.mult, op1=ALU.add), R=["DIAG0", "DCOL", tkey], W=["MW"])

        def ssm_gen_B(l, gh):
            g0 = 16 * gh
            PC = TMP4[:, 0:256].rearrange("p (g t) -> p g t", g=16)
            PS = TMP4[:, 256:512].rearrange("p (g t) -> p g t", g=16)
            tmpE = TMP4[:, 512:768].rearrange("p (g t) -> p g t", g=16)
            tmpA = TMP4[:, 768:1024].rearrange("p (g t) -> p g t", g=16)
            tmpK = SM[:, 2048:2304].rearrange("p (g t) -> p g t", g=16)
            AB = SM[:, 2304:2336]
            ADT = SM[:, 2336:2432]
            ssm_tables(l, gh, [(1, 1, 16)], PC, PS, tmpE, tmpA, tmpK, AB, ADT, SM[:, 0:256])
            outer_combine(BZCL[:, g0:g0 + 16, :, :].rearrange("p g a (j h) -> p g (a j) h", h=16), PC, PS,
                          S34[:, gh, 0, :, :], S34[:, gh, 1, :, :], 16, ["PC", "PS", "S34"], "BZCL", 0)

        PWR = SM[:, 512:768].rearrange("p (g t) -> p g t", g=16)
        PWI = SM[:, 768:1024].rearrange("p (g t) -> p g t", g=16)

        def ssm_scan_coefs(l):
            LL = SM[0:16, 0:256]
            kb.dma("sp", LL[:, 0:128].rearrange("g (a p) -> g a p", a=2),
                   lam_re[l].rearrange("(a g) p -> g a p", a=2), W=["ldsm"], dkey="scl")
            kb.dma("sp", LL[:, 128:256].rearrange("g (a p) -> g a p", a=2),
                   lam_im[l].rearrange("(a g) p -> g a p", a=2), W=["ldsm"], dkey="scl")
            op("pe", lambda e: e.matmul(ps[6][:, 32:48], LL[:, 0:128], ident_f[0:16, 0:16], start=True, stop=False),
               R=["ldsm", "ident_f"], W=[("ps", 6)])
            op("pe", lambda e: e.matmul(ps[6][:, 48:64], LL[:, 128:256], ident_f[0:16, 0:16], start=False, stop=True),
               R=["ldsm", "ident_f"], W=[("ps", 6)])
            A_ = TMP4[:, 0:16]
            B_ = TMP4[:, 16:32]
            DT = TMP4[:, 32:48]
            TAU = TMP4[:, 48:64]
            g3 = lambda ap: ap.rearrange("p (g t) -> p g t", g=16)
            EA = g3(TMP4[:, 64:320])
            AN = g3(TMP4[:, 320:576])
            tA = g3(TMP4[:, 576:832])
            tK = g3(SM[:, 256:512])
            op("act", lambda e: e.activation(out=TMP4[:, 0:32], in_=ps[6][:, 32:64], func=AF.Copy), R=[("ps", 6)], W=["TMP4"])
            for a in range(2):
                kb.dma("sp", DT[64 * a:64 * a + 64, :],
                       ssm_log_dt[l:l + 1, 16 * a:16 * a + 16].partition_broadcast(64).rearrange("p o n -> p (o n)"),
                       W=["TMP4"], dkey="dts")
            op("act", lambda e: e.activation(out=DT, in_=DT, func=AF.Exp), R=["TMP4"], W=["TMP4"])
            op("pool", lambda e: e.iota(TAU, [[16, 16]], base=16, channel_multiplier=0,
                                        allow_small_or_imprecise_dtypes=True), R=["TMP4"], W=["TMP4"])
            op("dve", lambda e: e.tensor_tensor(A_, A_, DT, op=ALU.mult), R=["TMP4"], W=["TMP4"])
            op("dve", lambda e: e.tensor_tensor(B_, B_, DT, op=ALU.mult), R=["TMP4"], W=["TMP4"])
            sh = [128, 16, 16]
            op("dve", lambda e: e.tensor_tensor(EA, A_.unsqueeze(2).to_broadcast(sh), TAU.unsqueeze(1).to_broadcast(sh),
                                                op=ALU.mult), R=["TMP4"], W=["TMP4"])
            op("act", lambda e: e.activation(out=EA, in_=EA, func=AF.Exp), R=["TMP4"], W=["TMP4"])
            op("dve", lambda e: e.tensor_tensor(AN, B_.unsqueeze(2).to_broadcast(sh), TAU.unsqueeze(1).to_broadcast(sh),
                                                op=ALU.mult), R=["TMP4"], W=["TMP4"])
            sincos(PWI, PWR, AN, tA, tK, tK.bitcast(I32), ["TMP4"], "TMP4", "tKc", "PW")
            op("dve", lambda e: e.tensor_tensor(PWR, PWR, EA, op=ALU.mult), R=["PW", "TMP4"], W=["PW"])
            op("dve", lambda e: e.tensor_tensor(PWI, PWI, EA, op=ALU.mult), R=["PW", "TMP4"], W=["PW"])
            for j, t in ((0, 0), (1, 15)):
                op("dve", lambda e, j=j, t=t: e.tensor_copy(LAB[:, j, 0, 0, :], PWR[:, :, t]), R=["PW"], W=["LAB"])
                op("dve", lambda e, j=j, t=t: e.tensor_copy(LAB[:, j, 0, 1, :], PWR[:, :, t]), R=["PW"], W=["LAB"])
                op("dve", lambda e, j=j, t=t: e.tensor_copy(LAB[:, j, 1, 1, :], PWI[:, :, t]), R=["PW"], W=["LAB"])
                op("dve", lambda e, j=j, t=t: e.tensor_scalar(LAB[:, j, 1, 0, :], PWI[:, :, t], -1.0, None, op0=ALU.mult),
                   R=["PW"], W=["LAB"])

        def ssm_main(l):
            DT_ = SS[0:16, 0:32]
            with nc.allow_non_contiguous_dma(reason="tiny D load"):
                kb.dma("sp", DT_, ssm_d[l].rearrange("(g h) -> h g", h=16), W=["dT"], dkey="dT")
            op("pe", lambda e: e.matmul(ps[5][:, 0:32], REP16[:], DT_, start=True, stop=True),
               R=["REP16", "dT"], W=[("ps", 5)])
            op("act", lambda e: e.activation(out=DCOL[:], in_=ps[5][:, 0:32], func=AF.Copy), R=[("ps", 5)], W=["DCOL"])
            ssm_gen_A(l, 0)
            if debug is not None and debug[0] == f"tab{l}":
                kb.barrier()
                kb.dma("sp", dbg_d[:, 0:1024], SM[:, 0:1024], dkey="dbg")
                kb.dma("sp", dbg_d[:, 1024:2560], FTf[:, 4096:5632], dkey="dbg")
                kb.dma("sp", dbg_d[:, 2560:2880], TMP4[:, 0:320], dkey="dbg")
                kb.dma("sp", dbg_d[:, 2880:3904], TMP4b[:, 0:1024], dkey="dbg")
                return True
            ssm_gen_A(l, 1)
            if debug is not None and debug[0] == f"bz{l}":
                dump(BIG[:, 0:8192], 8192, F32, ["BZCL", "MW"])
                return True
            kb.barrier()
            ALLU = [("UCM", j) for j in range(NT)]
            for b8 in range(8):
                pb = b8 % 2
                for j in range(8):
                    g, jb = 4 * b8 + j // 2, j % 2
                    op("pe", lambda e, g=g, jb=jb, j=j: e.transpose(
                        psb(pb)[:, j * 128:(j + 1) * 128], UCMg[:, g, 8 * jb:8 * jb + 8, :].rearrange("p j h -> p (j h)"),
                        ident_b[:]),
                       R=ALLU + ["ident_b"], W=[("ps", pb)], sig=(j == 7))
                eng = "act" if b8 % 2 == 0 else "dve"
                dst = UT[:, 4 * b8:4 * b8 + 4, :, :].rearrange("p g a c -> p (g a c)")
                if eng == "act":
                    op("act", lambda e: e.activation(out=dst, in_=psb(pb), func=AF.Copy), R=[("ps", pb)], W=["UT"])
                else:
                    op("dve", lambda e: e.tensor_copy(dst, psb(pb)), R=[("ps", pb)], W=["UT"])
            for g4 in range(8):
                pb = 2 + (g4 % 2)
                for j in range(4):
                    g = 4 * g4 + j
                    for jb in range(2):
                        op("pe", lambda e, g=g, jb=jb, j=j: e.matmul(ps[pb][:, j * 128:(j + 1) * 128], BZCL[:, g, jb, :],
                                                                   UT[:, g, jb, :], start=(j == 0 and jb == 0),
                                                                   stop=(j == 3 and jb == 1)),
                           R=["BZCL", "UT"], W=[("ps", pb)], sig=(j == 3 and jb == 1))
                ghh, gl0 = g4 // 4, 4 * (g4 % 4)
                pv = ps[pb][:].rearrange("p (g c) -> p g c", g=4)
                op("act", lambda e: e.activation(out=ZS[64 * ghh:64 * ghh + 64, 0, gl0:gl0 + 4, :], in_=pv[0:64],
                                                 func=AF.Copy), R=[("ps", pb)], W=["ZS"])
                op("dve", lambda e: e.tensor_copy(ZS[64 * ghh:64 * ghh + 64, 1, gl0:gl0 + 4, :], pv[64:128]),
                   R=[("ps", pb)], W=["ZS"])
            if debug is not None and debug[0] == f"z0{l}":
                dump(FTf[:, 4096:8192], 4096, F32, ["ZS"])
                return True
            ssm_scan_coefs(l)
            kb.barrier()
            ssm_gen_B(l, 0)
            ssm_gen_B(l, 1)
            ZS5 = FTf[:, 4096:8192].rearrange("p (r g b s) -> p r g b s", r=2, g=16, b=8)
            SCb = SM[:, 1024:1536].rearrange("p (a r g b) -> p a r g b", a=2, r=2, g=16)
            sh4 = [128, 2, 16, 8]
            sh3 = [128, 16, 8]
            for s_ in range(1, 16):
                prev = ZS5[:, :, :, :, s_ - 1]
                cur = ZS5[:, :, :, :, s_]
                op("pool", lambda e: e.tensor_tensor(SCb[:, 0], prev, LAB[:, 0, 0].unsqueeze(3).to_broadcast(sh4),
                                                     op=ALU.mult), R=["ZS", "LAB"], W=["SC0"])
                op("pool", lambda e: e.tensor_tensor(SCb[:, 1, 0], prev[:, 1], LAB[:, 0, 1, 0, :].unsqueeze(2).to_broadcast(sh3),
                                                     op=ALU.mult), R=["ZS", "LAB"], W=["SC1"])
                op("pool", lambda e: e.tensor_tensor(SCb[:, 1, 1], prev[:, 0], LAB[:, 0, 1, 1, :].unsqueeze(2).to_broadcast(sh3),
                                                     op=ALU.mult), R=["ZS", "LAB"], W=["SC1"])
                op("pool", lambda e: e.tensor_tensor(SCb[:, 0], SCb[:, 0], SCb[:, 1], op=ALU.add), R=["SC0", "SC1"], W=["SC0"])
                op("pool", lambda e: e.tensor_tensor(cur, cur, SCb[:, 0], op=ALU.add), R=["ZS", "SC0"], W=["ZS"])
            SC2 = SM[:, 1536:1600].rearrange("p (a r g) -> p a r g", a=2, r=2)
            for bk in range(1, 8):
                prev = ZS5[:, :, :, bk - 1, 15]
                cur = ZS5[:, :, :, bk, 15]
                op("pool", lambda e: e.tensor_tensor(SC2[:, 0], prev, LAB[:, 1, 0], op=ALU.mult), R=["ZS", "LAB"], W=["SC20"])
                op("pool", lambda e: e.tensor_tensor(SC2[:, 1, 0, :], prev[:, 1, :], LAB[:, 1, 1, 0, :], op=ALU.mult),
                   R=["ZS", "LAB"], W=["SC21"])
                op("pool", lambda e: e.tensor_tensor(SC2[:, 1, 1, :], prev[:, 0, :], LAB[:, 1, 1, 1, :], op=ALU.mult),
                   R=["ZS", "LAB"], W=["SC21"])
                op("pool", lambda e: e.tensor_tensor(SC2[:, 0], SC2[:, 0], SC2[:, 1], op=ALU.add), R=["SC20", "SC21"], W=["SC20"])
                op("pool", lambda e: e.tensor_tensor(cur, cur, SC2[:, 0], op=ALU.add), R=["ZS", "SC20"], W=["ZS"])
            F_ = [SM[:, 1600:1840].rearrange("p (g s) -> p g s", g=16),
                  SM[:, 1024:1264].rearrange("p (g s) -> p g s", g=16)]
            sh15 = [128, 16, 15]
            pr = PWR[:, :, 0:15]
            pi_ = PWI[:, :, 0:15]
            for bk in range(1, 8):
                cr = ZS5[:, 0, :, bk - 1, 15].unsqueeze(2).to_broadcast(sh15)
                ci = ZS5[:, 1, :, bk - 1, 15].unsqueeze(2).to_broadcast(sh15)
                re = ZS5[:, 0, :, bk, 0:15]
                im = ZS5[:, 1, :, bk, 0:15]
                for (dst, pa, ca, pb_, cb, sub) in ((re, pr, cr, pi_, ci, True), (im, pr, ci, pi_, cr, False)):
                    op("pool", lambda e, pa=pa, ca=ca: e.tensor_tensor(F_[0], pa, ca, op=ALU.mult), R=["PW", "ZS"], W=["F0"])
                    op("pool", lambda e, pb_=pb_, cb=cb: e.tensor_tensor(F_[1], pb_, cb, op=ALU.mult), R=["PW", "ZS"], W=["SC0"])
                    op("pool", lambda e, dst=dst: e.tensor_tensor(dst, dst, F_[0], op=ALU.add), R=["F0", "ZS"], W=["ZS"])
                    op("pool", lambda e, dst=dst, sub=sub: e.tensor_tensor(dst, dst, F_[1],
                                                                      op=(ALU.subtract if sub else ALU.add)),
                       R=["SC0", "ZS"], W=["ZS"])
            if debug is not None and debug[0] == f"zs{l}":
                dump(FTf[:, 4096:8192], 4096, F32, ["ZS"])
                return True
            op("pool", lambda e: e.memset(XSB[:, :, 0:1], 0.0), R=["ZS"], W=["XSB", "ldsm"])
            for ghh in range(2):
                for r in range(2):
                    eng = "act" if r == 0 else "dve"
                    src = ZS[64 * ghh:64 * ghh + 64, r, :, 0:127]
                    dst = XSB[64 * r:64 * r + 64, 16 * ghh:16 * ghh + 16, 1:128]
                    if eng == "act":
                        op("act", lambda e: e.activation(out=dst, in_=src, func=AF.Copy), R=["ZS"], W=["XSB", "ldsm"])
                    else:
                        op("dve", lambda e: e.tensor_copy(dst, src), R=["ZS"], W=["XSB", "ldsm"])
            for gp in range(16):
                pb = 4 + (gp % 4)
                for j in range(2):
                    g = 2 * gp + j
                    o = j * 256
                    op("pe", lambda e, g=g, o=o, j=j: e.matmul(ps[pb][:, o:o + 256], UT[:, g, 0, :],
                                                          MW[:, g, :, :].rearrange("p a m -> p (a m)"),
                                                          start=(j == 0), stop=False, skip_group_check=True),
                       R=["UT", "MW"], W=[("ps", pb)], sig=False)
                    op("pe", lambda e, g=g, o=o: e.matmul(ps[pb][:, o + 128:o + 256], UT[:, g, 1, :], MW[:, g, 0, :],
                                                     start=False, stop=False, skip_group_check=True),
                       R=["UT", "MW"], W=[("ps", pb)], sig=False)
                    op("pe", lambda e, g=g, o=o: e.matmul(ps[pb][:, o:o + 256], XSB[:, g, :],
                                                     BZCL[:, g, :, :].rearrange("p a m -> p (a m)"),
                                                     start=False, stop=(j == 1), skip_group_check=True),
                       R=["XSB", "BZCL"], W=[("ps", pb)], sig=(j == 1))
                dst = UCM[:, :, 32 * gp:32 * gp + 32].rearrange("p i (g h) -> p g i h", g=2)
                op("act", lambda e: e.activation(out=dst, in_=ps[pb][:].rearrange("p (g i h) -> p g i h", g=2, i=16),
                                                 func=AF.Gelu_apprx_tanh), R=[("ps", pb)], W=ALLU)

        def glu_phase(l):
            WGLU = TMP4[:].bitcast(BF16).rearrange("p (k n) -> p k n", k=4)
            kb.dma("pool", WGLU, w_glu[l].rearrange("(k p) n -> p k n", p=128), W=["TMP4"], dkey="wglu")
            ZSS = SS[:, 0:16]
            def tile(i):
                s2 = i % 2
                pb = s2
                YGT = SM[:, 2048 + 256 * s2:2048 + 256 * (s2 + 1)].bitcast(BF16)
                SG = SM[:, 1024 * s2:1024 * s2 + 512]
                ZF = SM[:, 1024 * s2 + 512:1024 * s2 + 1024]
                ZN = SG.bitcast(BF16)[:, 0:512]
                for fc in range(4):
                    op("pe", lambda e, fc=fc: e.transpose(psb(pb)[:, fc * 128:(fc + 1) * 128],
                                                          UCM[:, i, fc * 128:(fc + 1) * 128], ident_b[:]),
                       R=[("UCM", i), "ident_b"], W=[("ps", pb)], sig=(fc == 3))
                    yield
                op("act", lambda e: e.activation(out=YGT, in_=psb(pb)[:, 0:512], func=AF.Copy),
                   R=[("ps", pb)], W=[("YGT", s2)])
                yield
                pg = 2 + s2
                for fc in range(4):
                    op("pe", lambda e, fc=fc: e.matmul(ps[pg][:], YGT[:, fc * 128:(fc + 1) * 128], WGLU[:, fc, :],
                                                       start=(fc == 0), stop=(fc == 3)),
                       R=[("YGT", s2), "TMP4"], W=[("ps", pg)], sig=(fc == 3))
                    yield
                op("act", lambda e: e.activation(out=SG, in_=ps[pg][:], func=AF.Sigmoid), R=[("ps", pg)], W=[("SG", s2)])
                yield
                op("dve", lambda e: e.tensor_tensor(ZF, UCM[:, i, :], SG, op=ALU.mult),
                   R=[("UCM", i), ("SG", s2)], W=[("ZF", s2)])
                yield
                op("dve", lambda e: e.scalar_tensor_tensor(ZN, ZF, 1.0, ZF, op0=ALU.mult, op1=ALU.mult,
                                                           accum_out=ZSS[:, i:i + 1]),
                   R=[("ZF", s2), ("SG", s2)], W=[("SG", s2), ("ZSS", i)])
                yield
                op("dve", lambda e: e.tensor_scalar(SS[:, 16 + i:17 + i], ZSS[:, i:i + 1], 1.0 / 512.0, EPS,
                                                    op0=ALU.mult, op1=ALU.add), R=[("ZSS", i)], W=[("ZSb", i)])
                yield
                op("pool", lambda e: e.tensor_tensor(SS[:, 32 + i:33 + i], SS[:, 16 + i:17 + i], NHALF[:, 0:1],
                                                     op=ALU.pow), R=[("ZSb", i), "NHALF"], W=[("ZRS", i)])
                yield
                op("dve", lambda e: e.tensor_scalar(ZN, ZF, SS[:, 32 + i:33 + i], None, op0=ALU.mult),
                   R=[("ZF", s2), ("ZRS", i), ("SG", s2)], W=[("SG", s2)])
                yield
                pt = 4 + s2
                for fc in range(4):
                    op("pe", lambda e, fc=fc: e.transpose(psb(pt)[:, fc * 128:(fc + 1) * 128],
                                                          ZN[:, fc * 128:(fc + 1) * 128], ident_b[:]),
                       R=[("SG", s2), "ident_b"], W=[("ps", pt)], sig=(fc == 3))
                    yield
                op("act", lambda e: e.activation(out=FT3[:, 0:4, i::16],
                                                 in_=psb(pt)[:, 0:512].rearrange("p (k c) -> p k c", k=4),
                                                 func=AF.Copy), R=[("ps", pt)], W=[("ST", i)])
                yield

            run_rr([tile(i) for i in range(NT)], 2, 8)

        WOUT = big_bf(0, 4096).rearrange("p (k n) -> p k n", k=8)

        def wout_load(l):
            kb.dma("pool", WOUT, w_out[l].rearrange("(k p) n -> p k n", p=128), W=["WOUT"], dkey="wout")
            for k in range(8):
                op("dve", lambda e, k=k: e.scalar_tensor_tensor(WOUT[:, k, :], WOUT[:, k, :], GROW[:, k:k + 1],
                                                                MOD[:, 2, :], op0=ALU.mult, op1=ALU.mult),
                   R=["WOUT", "GROW", ("MOD", 2)], W=["WOUT"])

        def wout_phase(l):
            ALLAT = [("AT", n) for n in range(NT)]
            ALLST = [("ST", n) for n in range(NT)]
            cnt = 0
            for i in range(NT):
                for half in range(2):
                    pb = 4 + (cnt % 4)
                    cnt += 1
                    for k in range(8):
                        lhs = AT3[:, k, i::16] if k < 4 else FT3[:, k - 4, i::16]
                        op("pe", lambda e, k=k, lhs=lhs: e.matmul(ps[pb][:], lhs, WOUT[:, k, half * 512:(half + 1) * 512],
                                                                 start=(k == 0), stop=(k == 7)),
                           R=ALLAT + ALLST + ["WOUT"], W=[("ps", pb)], sig=(k == 7))
                    xs_ = X[:, i, half * 512:(half + 1) * 512]
                    op("dve", lambda e, xs_=xs_: e.tensor_tensor(xs_, xs_, ps[pb][:], op=ALU.add),
                       R=[("ps", pb), ("X", i)], W=[("X", i)])

        H2T3 = big_bf(4096, 12288).rearrange("p (k t) -> p k t", k=8)
        ACTT = big_bf(0, 4096).rearrange("p (k t) -> p k t", k=4)

        def ring(sl):
            if sl < 4:
                return FT[:, sl * 4096:(sl + 1) * 4096]
            return AT[:, (sl - 4) * 4096:(sl - 3) * 4096]

        def router(l):
            ALLH = [("H2", i) for i in range(NT)]
            for i in range(NT):
                for k in range(8):
                    op("pe", lambda e, k=k: e.matmul(ps[6][:, i * 16:(i + 1) * 16], H2T3[:, k, i * 128:(i + 1) * 128],
                                                     WR[:, k, :], start=(i == 0 and k == 0), stop=(i == NT - 1 and k == 7),
                                                     skip_group_check=True),
                       R=[("H2", i), "WR"], W=[("ps", 6)], sig=(i == NT - 1 and k == 7))
            v3 = lambda ap: ap.rearrange("p (i e) -> p i e", i=16)
            v4 = lambda ap: ap.rearrange("p (i g e) -> p i g e", i=16, g=4)
            LG = TMP4[:, 0:256]
            PR = TMP4[:, 256:512]
            SEL = TMP4[:, 512:768]
            SEL2 = TMP4[:, 768:1024]
            EQ1 = TMP4b[:, 0:256]
            EQ2 = TMP4b[:, 256:512]
            CMB = TMP4b[:, 512:768]
            CMBb = TMP4b[:, 768:896].bitcast(BF16)
            MX = SS[:, 0:16]
            SM_ = SS[:, 16:32]
            M1 = SM[:, 2048:2112]
            M2 = SM[:, 2112:2176]
            GS = SM[:, 2176:2240]
            GM = SS[:, 32:48]
            ING = SM[:, 2240:2304]
            b3 = lambda ap: ap.unsqueeze(2).to_broadcast([128, 16, 16])
            op("dve", lambda e: e.tensor_reduce(MX, v3(ps[6][:, 0:256]), axis=AX.X, op=ALU.max), R=[("ps", 6)], W=["r_mx"])
            op("dve", lambda e: e.tensor_tensor(v3(LG), v3(ps[6][:, 0:256]), b3(MX), op=ALU.subtract),
               R=[("ps", 6), "r_mx"], W=["TMP4"])
            op("act", lambda e: e.activation(out=LG, in_=LG, func=AF.Exp), R=["TMP4"], W=["TMP4"])
            op("dve", lambda e: e.tensor_reduce(SM_, v3(LG), axis=AX.X, op=ALU.add), R=["TMP4"], W=["r_sm"])
            op("dve", lambda e: e.reciprocal(SM_, SM_), R=["r_sm"], W=["r_sm"])
            op("dve", lambda e: e.tensor_tensor(v3(PR), v3(LG), b3(SM_), op=ALU.mult), R=["TMP4", "r_sm"], W=["TMP4"])
            op("dve", lambda e: e.tensor_tensor(v3(SEL), v3(PR), RB[:].unsqueeze(1).to_broadcast([128, 16, 16]),
                                                op=ALU.add), R=["TMP4", "RB"], W=["TMP4"])
            op("dve", lambda e: e.tensor_reduce(M1, SEL.rearrange("p (j e) -> p j e", e=4), axis=AX.X, op=ALU.max),
               R=["TMP4"], W=["r_m1"])
            b4 = lambda ap: ap.unsqueeze(2).to_broadcast([128, 64, 4])
            j4 = lambda ap: ap.rearrange("p (j e) -> p j e", e=4)
            op("dve", lambda e: e.tensor_tensor(j4(EQ1), j4(SEL), b4(M1), op=ALU.is_equal), R=["TMP4", "r_m1"], W=["TMP4b"])
            op("dve", lambda e: e.scalar_tensor_tensor(SEL2, EQ1, -1.0e9, SEL, op0=ALU.mult, op1=ALU.add),
               R=["TMP4b", "TMP4"], W=["TMP4"])
            op("dve", lambda e: e.tensor_reduce(M2, j4(SEL2), axis=AX.X, op=ALU.max), R=["TMP4"], W=["r_m2"])
            op("dve", lambda e: e.tensor_tensor(j4(EQ2), j4(SEL2), b4(M2), op=ALU.is_equal), R=["TMP4", "r_m2"], W=["TMP4b"])
            op("dve", lambda e: e.tensor_tensor(GS, M1, M2, op=ALU.add), R=["r_m1", "r_m2"], W=["r_gs"])
            op("dve", lambda e: e.tensor_reduce(GM, GS.rearrange("p (i g) -> p i g", g=4), axis=AX.X, op=ALU.max),
               R=["r_gs"], W=["r_gm"])
            op("dve", lambda e: e.tensor_tensor(ING.rearrange("p (i g) -> p i g", g=4), GS.rearrange("p (i g) -> p i g", g=4),
                                                GM.unsqueeze(2).to_broadcast([128, 16, 4]), op=ALU.is_equal),
               R=["r_gs", "r_gm"], W=["r_ing"])
            op("dve", lambda e: e.tensor_tensor(EQ1, EQ1, EQ2, op=ALU.add), R=["TMP4b"], W=["TMP4b"])
            op("dve", lambda e: e.tensor_tensor(j4(EQ1), j4(EQ1), b4(ING), op=ALU.mult), R=["TMP4b", "r_ing"], W=["TMP4b"])
            op("dve", lambda e: e.tensor_tensor(EQ1, EQ1, PR, op=ALU.mult), R=["TMP4b", "TMP4"], W=["TMP4b"])
            op("dve", lambda e: e.tensor_reduce(SM_, v3(EQ1), axis=AX.X, op=ALU.add), R=["TMP4b"], W=["r_sm"])
            op("dve", lambda e: e.reciprocal(SM_, SM_), R=["r_sm"], W=["r_sm"])
            op("dve", lambda e: e.tensor_tensor(v3(CMBb), v3(EQ1), b3(SM_), op=ALU.mult), R=["TMP4b", "r_sm"], W=["TMP4b"])
            if debug is not None and debug[0] == f"comb{l}":
                op("dve", lambda e: e.tensor_tensor(v3(CMB), v3(EQ1), b3(SM_), op=ALU.mult), R=["TMP4b", "r_sm"], W=["TMP4b"])
                dump(CMB, 256, F32, ["TMP4b"])
                return True
            CT = SM[0:16, 0:1024].bitcast(BF16)
            for i in range(NT):
                pb = 4 + (i // 8)
                op("pe", lambda e, i=i: e.transpose(psb(pb)[0:16, (i % 8) * 128:(i % 8 + 1) * 128],
                                                    CMBb[:, i * 16:(i + 1) * 16], ident_b[:]),
                   R=["TMP4b", "ident_b"], W=[("ps", pb)], sig=(i % 8 == 7))
            for hb in range(2):
                op("act", lambda e, hb=hb: e.activation(out=CT[:, hb * 1024:(hb + 1) * 1024], in_=psb(4 + hb)[0:16, :],
                                                        func=AF.Copy), R=[("ps", 4 + hb)], W=["CT"])
            return False

        def load_expert(l, e_):
            base = 3 * (e_ % 2)
            kb.dma("pool", ring(base).rearrange("p (k n) -> p k n", k=8),
                   w_exp_gate[l, e_].rearrange("(k p) n -> p k n", p=128), W=[("ring", base)], dkey=f"ring{base}")
            kb.dma("pool", ring(base + 1).rearrange("p (k n) -> p k n", k=8),
                   w_exp_up[l, e_].rearrange("(k p) n -> p k n", p=128), W=[("ring", base + 1)], dkey=f"ring{base + 1}")
            wd = ring(base + 2).rearrange("p (k n) -> p k n", k=4)
            kb.dma("pool", wd, w_exp_down[l, e_].rearrange("(k p) n -> p k n", p=128),
                   W=[("ring", base + 2)], dkey=f"ring{base + 2}")


        def moe_phase(l):
            CT = SM[0:16, 0:1024].bitcast(BF16)

            def scale_wd(e_):
                base = 3 * (e_ % 2)
                wd = ring(base + 2).rearrange("p (k n) -> p k n", k=4)
                op("pool", lambda e: e.tensor_tensor(wd, wd, MOD[:, 5, :].unsqueeze(1).to_broadcast([128, 4, 1024]),
                                                     op=ALU.mult), R=[("ring", base + 2), ("MOD", 5)], W=[("ring", base + 2)])

            def prep_piece(e2, tb):
                p2 = e2 % 2
                CB2 = (TMP4 if p2 == 0 else TMP4b)[:].bitcast(BF16)
                ck2 = "TMP4" if p2 == 0 else "TMP4b"
                if tb == 0:
                    op("dve", lambda e: e.tensor_copy(SELE[:, p2, :], ident_b[0:16, e2:e2 + 1].to_broadcast([16, 128])),
                       R=["ident_b"], W=[("SELE", p2)])
                op("pe", lambda e: e.matmul(ps[6][:], SELE[:, p2, :], CT[:, tb * 512:(tb + 1) * 512],
                                            start=True, stop=True), R=[("SELE", p2), "CT"], W=[("ps", 6)])
                op("act", lambda e: e.activation(out=CB2[:, tb * 512:(tb + 1) * 512], in_=ps[6][:], func=AF.Copy),
                   R=[("ps", 6)], W=[(ck2, tb)])

            gcnt = 0
            dcnt = 0
            for e_ in range(16):
                if e_ + 1 < 16:
                    load_expert(l, e_ + 1)
                base = 3 * (e_ % 2)
                WG = ring(base).rearrange("p (k n) -> p k n", k=8)
                WU = ring(base + 1).rearrange("p (k n) -> p k n", k=8)
                WD = ring(base + 2).rearrange("p (k n) -> p k n", k=4)
                es_ = e_ % 2
                CBC = (TMP4 if es_ == 0 else TMP4b)[:].bitcast(BF16)
                ckey = "TMP4" if es_ == 0 else "TMP4b"
                if e_ == 0:
                    for tb in range(4):
                        prep_piece(0, tb)
                for fc in range(4):
                    if fc == 2:
                        scale_wd(e_)
                    for tb in range(4):
                        g2 = gcnt % 2
                        gcnt += 1
                        pg, pu = g2, 2 + g2
                        hk = [("H2", i) for i in range(4 * tb, 4 * tb + 4)]
                        for k in range(8):
                            op("pe", lambda e, k=k: e.matmul(ps[pg][:], WG[:, k, fc * 128:(fc + 1) * 128],
                                                             H2T3[:, k, tb * 512:(tb + 1) * 512],
                                                             start=(k == 0), stop=(k == 7)),
                               R=[("ring", base)] + hk, W=[("ps", pg)], sig=(k == 7))
                        for k in range(8):
                            op("pe", lambda e, k=k: e.matmul(ps[pu][:], WU[:, k, fc * 128:(fc + 1) * 128],
                                                             H2T3[:, k, tb * 512:(tb + 1) * 512],
                                                             start=(k == 0), stop=(k == 7)),
                               R=[("ring", base + 1)] + hk, W=[("ps", pu)], sig=(k == 7))
                        SGt = SM[:, 1024 + 256 * g2:1280 + 256 * g2].bitcast(BF16)
                        Tt = SM[:, 1536 + 256 * g2:1792 + 256 * g2].bitcast(BF16)
                        op("act", lambda e: e.activation(out=SGt, in_=ps[pg][:], func=AF.Silu),
                           R=[("ps", pg)], W=[("SGt", g2)])
                        op("dve", lambda e: e.tensor_tensor(Tt, SGt, ps[pu][:], op=ALU.mult),
                           R=[("SGt", g2), ("ps", pu)], W=[("Tt", g2)])
                        op("pool", lambda e: e.tensor_tensor(ACTT[:, fc, tb * 512:(tb + 1) * 512], Tt,
                                                             CBC[:, tb * 512:(tb + 1) * 512], op=ALU.mult),
                           R=[("Tt", g2), (ckey, tb)], W=[("ACTT", fc, tb)])
                    if e_ + 1 < 16:
                        prep_piece(e_ + 1, fc)
                for i in range(NT):
                    for half in range(2):
                        pd = (4, 5, 7)[dcnt % 3]
                        dcnt += 1
                        for fc in range(4):
                            op("pe", lambda e, fc=fc: e.matmul(ps[pd][:], ACTT[:, fc, i * 128:(i + 1) * 128],
                                                               WD[:, fc, half * 512:(half + 1) * 512],
                                                               start=(fc == 0), stop=(fc == 3)),
                               R=[("ACTT", fc, i // 4), ("ring", base + 2)], W=[("ps", pd)], sig=(fc == 3))
                        xs_ = X[:, i, half * 512:(half + 1) * 512]
                        op("dve", lambda e, xs_=xs_: e.tensor_tensor(xs_, xs_, ps[pd][:], op=ALU.add),
                           R=[("ps", pd), ("X", i, half)], W=[("X", i, half)])

        def dump(ap, shape2d_cols, dt, keys):
            kb.dma("sp", dbg_d[:, 0:shape2d_cols], ap, R=keys, dkey="dbg")

        for l in range(depth):
            adaln(l)
            if debug is not None and debug[0] == f"mod{l}":
                dump(MOD[:].rearrange("p a n -> p (a n)"), 6 * D, BF16, [("MOD", j) for j in range(6)])
                break
            kb.barrier()
            layer_loads(l)
            norm_to_FT(l, 0, lambda i: FT3[:, :, i::16])
            if debug is not None and debug[0] == f"hT{l}":
                dump(FT[:], 8 * S, BF16, [("FT", i) for i in range(NT)])
                break
            u_proj(l)
            if debug is not None and debug[0] == f"ucm{l}":
                dump(UCM.rearrange("p j f -> p (j f)"), 16 * 512, BF16, [("UCM", j) for j in range(NT)])
                break
            kb.barrier()
            attention(l)
            if debug is not None and debug[0] == f"at{l}":
                dump(AT[:], 4 * S, BF16, [("AT", n) for n in range(NT)])
                break
            kb.barrier()
            if ssm_main(l):
                break
            if debug is not None and debug[0] == f"yg{l}":
                dump(UCM.rearrange("p j f -> p (j f)"), 16 * 512, BF16, [("UCM", j) for j in range(NT)])
                break
            kb.barrier()
            wout_load(l)
            glu_phase(l)
            if debug is not None and debug[0] == f"st{l}":
                dump(FT[:, 0:4 * S], 4 * S, BF16, [("ST", i) for i in range(NT)])
                break
            kb.barrier()
            wout_phase(l)
            if debug is not None and debug[0] == f"x1{l}":
                kb.barrier()
                dump(X[:].rearrange("p i d -> p (i d)"), NT * D, F32, [("X", i) for i in range(NT)])
                break
            kb.barrier()
            load_expert(l, 0)
            norm_to_FT(l, 1, lambda i: H2T3[:, :, i * 128:(i + 1) * 128], okey="H2", pbanks=(0, 1))
            if debug is not None and debug[0] == f"h2{l}":
                kb.barrier()
                dump(BIG[:, 4096:12288].bitcast(BF16), 8 * S, BF16, [("H2", i) for i in range(NT)])
                break
            kb.barrier()
            if router(l):
                break
            kb.barrier()
            moe_phase(l)
            kb.barrier()
            if debug is not None and debug[0] == f"x2{l}":
                dump(X[:].rearrange("p i d -> p (i d)"), NT * D, F32, [("X", i) for i in range(NT)])
                break

        if debug is None:
            y_v = y_d.rearrange("(c i) d -> c i d", i=NT)
            for q4 in range(4):
                kb.dma("sp", y_v[:, 4 * q4:4 * q4 + 4, :], X[:, 4 * q4:4 * q4 + 4, :],
                       R=[("X", i) for i in range(4 * q4, 4 * q4 + 4)], dkey=f"y{q4}")
        kb.final_wait("sp")
    return nc


_IN_NAMES = ["ada_w", "ada_b", "norm1_g", "w_in", "q_norm_g", "k_norm_g", "attn_sink", "lam_re", "lam_im",
             "ssm_b_re", "ssm_b_im", "ssm_c_re", "ssm_c_im", "ssm_d", "ssm_log_dt", "w_glu", "attn_out_g",
             "ssm_out_g", "w_out", "norm2_g", "w_router", "router_bias", "w_exp_gate", "w_exp_up", "w_exp_down"]


def make_in_maps(inputs, cores):
    maps = []
    shared = {k: np.ascontiguousarray(np.asarray(inputs[k], dtype=np.float32)) for k in _IN_NAMES}
    for b in cores:
        m = dict(shared)
        m["x"] = np.ascontiguousarray(np.asarray(inputs["x"][b], dtype=np.float32))
        m["c"] = np.ascontiguousarray(np.asarray(inputs["c"][b], dtype=np.float32).reshape(8, 128))
        m["positions"] = np.ascontiguousarray(np.asarray(inputs["positions"][b], dtype=np.int32))
        maps.append(m)
    return maps


def kernel(**inputs):
    nc = build()
    in_maps = make_in_maps(inputs, list(range(8)))
    res = run_bass_kernel_spmd(nc, in_maps, core_ids=list(range(8)))
    return np.stack([np.asarray(r["y"], dtype=np.float32) for r in res.results], axis=0)
```

```python
import math
import contextlib
import numpy as np
import concourse.bass as bass
import concourse.mybir as mybir
from concourse.bass_utils import run_bass_kernel_spmd

F32 = mybir.dt.float32
BF16 = mybir.dt.bfloat16
I32 = mybir.dt.int32
AF = mybir.ActivationFunctionType
ALU = mybir.AluOpType
AX = mybir.AxisListType

DEPTH = 2
S = 2048
D = 1024
NT = 16
EPS = 1e-6
NEG = -30000.0
TWO_PI = 2.0 * math.pi
import os
SELF_SYNC = set(os.environ.get("KSELF", "dve").split(","))


class KB:
    def __init__(self, nc, es):
        self.nc = nc
        self.es = es
        self.E = {"pe": nc.tensor, "act": nc.scalar, "dve": nc.vector, "pool": nc.gpsimd, "sp": nc.sync}
        self.sems = {}
        self.val = {}
        self.seen = {e: {} for e in self.E}
        self.lastw = {}
        self.readers = {}
        for e in ("pe", "act", "dve", "pool"):
            self._sem(e)

    def _sem(self, name):
        if name not in self.sems:
            self.sems[name] = self.es.enter_context(self.nc.semaphore("s_" + name.replace(":", "_")))
            self.val[name] = 0
        return self.sems[name]

    def _deps(self, R, W):
        deps = {}

        def need(d):
            if d is None:
                return
            s, v = d
            if v > deps.get(s, 0):
                deps[s] = v

        for k in R:
            need(self.lastw.get(k))
        for k in W:
            need(self.lastw.get(k))
            for r in self.readers.get(k, ()):
                need(r)
        return deps

    def _wait(self, eng, deps):
        E = self.E[eng]
        for s, v in deps.items():
            if s == eng and (eng == "pe" or eng not in SELF_SYNC):
                continue
            if self.seen[eng].get(s, 0) >= v:
                continue
            E.wait_ge(self.sems[s], v)
            self.seen[eng][s] = v

    def _record(self, tok, R, W):
        for k in R:
            self.readers.setdefault(k, []).append(tok)
        for k in W:
            self.lastw[k] = tok
            self.readers[k] = []

    def op(self, eng, fn, R=(), W=(), sig=True):
        self._wait(eng, self._deps(R, W))
        inst = fn(self.E[eng])
        if sig:
            self.val[eng] += 1
            inst.then_inc(self.sems[eng], 1)
            tok = (eng, self.val[eng])
        else:
            tok = (eng, self.val[eng] + 1)
        self._record(tok, R, W)
        return inst

    def dma(self, q, out, in_, R=(), W=(), dkey=None, **kw):
        name = "d:" + dkey
        self._sem(name)
        self._wait(q, self._deps(R, W))
        inst = self.E[q].dma_start(out=out, in_=in_, **kw)
        self.val[name] += 16
        inst.then_inc(self.sems[name], 16)
        self._record((name, self.val[name]), R, W)
        return inst

    def barrier(self):
        for eng in self.E:
            deps = {s: v for s, v in self.val.items() if v > 0}
            self._wait(eng, deps)
        self.lastw.clear()
        self.readers.clear()

    def final_wait(self, eng="sp"):
        deps = {s: v for s, v in self.val.items() if v > 0 and s.startswith("d:")}
        self._wait(eng, deps)


def run_rr(gens, width, lag=0):
    it = iter(gens)
    active = []
    steps_newest = 10 ** 9
    done = False
    while True:
        if not done and len(active) < width and steps_newest >= lag:
            try:
                active.append(next(it))
                steps_newest = 0
            except StopIteration:
                done = True
        if not active:
            if done:
                break
            steps_newest = 10 ** 9
            continue
        for g in list(active):
            try:
                next(g)
            except StopIteration:
                active.remove(g)
        steps_newest += 1


def build(depth=DEPTH, debug=None):
    nc = bass.Bass("TRN2", target_bir_lowering=False)

    def din(name, shape, dt=F32):
        return nc.dram_tensor(name, list(shape), dt, kind="ExternalInput").ap()

    x_d = din("x", [S, D])
    c_d = din("c", [8, 128])
    pos_d = din("positions", [S], I32)
    ada_w = din("ada_w", [DEPTH, D, 6 * D])
    ada_b = din("ada_b", [DEPTH, 6 * D])
    norm1_g = din("norm1_g", [DEPTH, D])
    w_in = din("w_in", [DEPTH, D, 1280])
    q_norm_g = din("q_norm_g", [DEPTH, 64])
    k_norm_g = din("k_norm_g", [DEPTH, 64])
    attn_sink = din("attn_sink", [DEPTH, 8])
    lam_re = din("lam_re", [DEPTH, 32, 64])
    lam_im = din("lam_im", [DEPTH, 32, 64])
    ssm_b_re = din("ssm_b_re", [DEPTH, 32, 64, 16])
    ssm_b_im = din("ssm_b_im", [DEPTH, 32, 64, 16])
    ssm_c_re = din("ssm_c_re", [DEPTH, 32, 16, 64])
    ssm_c_im = din("ssm_c_im", [DEPTH, 32, 16, 64])
    ssm_d = din("ssm_d", [DEPTH, 512])
    ssm_log_dt = din("ssm_log_dt", [DEPTH, 32])
    w_glu = din("w_glu", [DEPTH, 512, 512])
    attn_out_g = din("attn_out_g", [DEPTH, 512])
    ssm_out_g = din("ssm_out_g", [DEPTH, 512])
    w_out = din("w_out", [DEPTH, D, D])
    norm2_g = din("norm2_g", [DEPTH, D])
    w_router = din("w_router", [D, 16])
    router_bias = din("router_bias", [16])
    w_exp_gate = din("w_exp_gate", [DEPTH, 16, D, 512])
    w_exp_up = din("w_exp_up", [DEPTH, 16, D, 512])
    w_exp_down = din("w_exp_down", [DEPTH, 16, 512, D])
    y_d = nc.dram_tensor("y", [S, D], F32, kind="ExternalOutput").ap()
    dbg_d = None
    if debug is not None:
        dbg_d = nc.dram_tensor("dbg", [128, debug[1]], debug[2], kind="ExternalOutput").ap()

    with contextlib.ExitStack() as es:
        kb = KB(nc, es)
        op = kb.op

        def sb(name, shape, dt):
            return es.enter_context(nc.sbuf_tensor(name, list(shape), dt))

        X = sb("X", [128, NT, D], F32)
        FT = sb("FT", [128, 8 * S], BF16)
        AT = sb("AT", [128, 4 * S], BF16)
        BIG = sb("BIG", [128, 12288], F32)
        MOD = sb("MOD", [128, 6, D], BF16)
        SM = sb("SM", [128, 2560], F32)
        ident_f = sb("ident_f", [128, 128], F32)
        ident_b = sb("ident_b", [128, 128], BF16)
        colI = sb("colI", [128, 128], F32)
        rowI = sb("rowI", [128, 128], F32)
        MBp = sb("MBp", [128, 4, 128], BF16)
        MBc = sb("MBc", [128, 4, 128], BF16)
        COS = sb("COS", [128, NT, 32], F32)
        SIN = sb("SIN", [128, NT, 32], F32)
        SCrep = sb("SCrep", [128, 8, 128], BF16)
        TMP4 = sb("TMP4", [128, 1024], F32)
        TMP4b = sb("TMP4b", [128, 1024], F32)
        SS = sb("SS", [128, 64], F32)
        NHALF = sb("NHALF", [128, 64], F32)
        GQK = sb("GQK", [128, 10, 64], BF16)
        ESINK = sb("ESINK", [128, 8], F32)
        GROW = sb("GROW", [128, 8], F32)
        MASKM = sb("MASKM", [128, 256], BF16)
        DIAG0 = sb("DIAG0", [128, 256], BF16)
        REP16 = sb("REP16", [16, 128], F32)
        SGN = sb("SGN", [128, 2], F32)
        DCOL = sb("DCOL", [128, 32], F32)
        LAB = sb("LAB", [128, 2, 2, 2, 16], F32)
        WR = sb("WR", [128, 8, 16], BF16)
        RB = sb("RB", [128, 16], F32)
        SELE = sb("SELE", [16, 2, 128], BF16)

        PSALL = es.enter_context(nc.psum_tensor("psall", [128, 4096], F32))
        ps = [PSALL[:, 512 * i:512 * (i + 1)] for i in range(8)]

        def psb(i):
            return ps[i].bitcast(BF16)

        def big_bf(w0, w1):
            return BIG[:, w0:w1].bitcast(BF16)

        FT3 = FT[:].rearrange("p (k t) -> p k t", k=8)
        AT3 = AT[:].rearrange("p (k t) -> p k t", k=4)

        op("pool", lambda e: e.iota(colI[:], [[1, 128]], base=0, channel_multiplier=0,
                                    allow_small_or_imprecise_dtypes=True), W=["colI"])
        op("pool", lambda e: e.iota(rowI[:], [[0, 128]], base=0, channel_multiplier=1,
                                    allow_small_or_imprecise_dtypes=True), W=["rowI"])
        op("pool", lambda e: e.memset(NHALF[:], -0.5), W=["NHALF"])
        op("dve", lambda e: e.tensor_tensor(ident_f[:], colI[:], rowI[:], op=ALU.is_equal),
           R=["colI", "rowI"], W=["ident_f"])
        op("dve", lambda e: e.tensor_copy(ident_b[:], ident_f[:]), R=["ident_f"], W=["ident_b"])
        op("dve", lambda e: e.tensor_tensor(TMP4[:, 0:128], colI[:], rowI[:], op=ALU.is_ge),
           R=["colI", "rowI"], W=["TMP4"])
        op("dve", lambda e: e.tensor_scalar(MBp[:], TMP4[:, 0:128].unsqueeze(1).to_broadcast([128, 4, 128]),
                                            NEG, None, op0=ALU.mult), R=["TMP4"], W=["MBp"])
        op("dve", lambda e: e.tensor_tensor(TMP4[:, 128:256], colI[:], rowI[:], op=ALU.is_lt),
           R=["colI", "rowI"], W=["TMP4"])
        op("dve", lambda e: e.tensor_scalar(MBc[:], TMP4[:, 128:256].unsqueeze(1).to_broadcast([128, 4, 128]),
                                            NEG, None, op0=ALU.mult), R=["TMP4"], W=["MBc"])


        def _ssm_consts():
            PI_ = SM[:, 340:341].bitcast(I32)
            JJF = SM[:, 341:342]
            HPF = SM[:, 342:343]
            TI = SM[:, 344:345].bitcast(I32)
            IIF = TMP4[:, 0:256]
            HF = TMP4[:, 256:512]
            DLF = TMP4[:, 512:768]
            E1 = TMP4b[:, 0:256]
            E2 = TMP4b[:, 256:512]
            op("pool", lambda e: e.iota(PI_, [[0, 1]], base=0, channel_multiplier=1), W=["c_pi"])
            op("dve", lambda e: e.tensor_single_scalar(TI, PI_, 4, op=ALU.arith_shift_right), R=["c_pi"], W=["c_ti"])
            op("dve", lambda e: e.tensor_copy(JJF, TI), R=["c_ti"], W=["c_jj"])
            op("dve", lambda e: e.tensor_single_scalar(TI, PI_, 15, op=ALU.bitwise_and), R=["c_pi", "c_jj"], W=["c_ti"])
            op("dve", lambda e: e.tensor_copy(HPF, TI), R=["c_ti"], W=["c_hp"])
            op("pool", lambda e: e.iota(IIF.rearrange("p (a b c) -> p a b c", a=2, b=8), [[0, 2], [1, 8], [0, 16]],
                                        base=0, channel_multiplier=0, allow_small_or_imprecise_dtypes=True),
               W=["TMP4"])
            op("pool", lambda e: e.iota(HF.rearrange("p (a b c) -> p a b c", a=2, b=8), [[0, 2], [0, 8], [1, 16]],
                                        base=0, channel_multiplier=0, allow_small_or_imprecise_dtypes=True),
               W=["TMP4"])
            op("pool", lambda e: e.iota(DLF.rearrange("p (a b c) -> p a b c", a=2, b=8), [[1, 2], [0, 8], [0, 16]],
                                        base=0, channel_multiplier=0, allow_small_or_imprecise_dtypes=True),
               W=["TMP4"])
            op("dve", lambda e: e.tensor_scalar(E1, IIF, JJF, None, op0=ALU.is_ge), R=["TMP4", "c_jj"], W=["TMP4b"])
            op("dve", lambda e: e.tensor_tensor(MASKM[:], E1, DLF, op=ALU.max), R=["TMP4b", "TMP4"], W=["MASKM"])
            op("dve", lambda e: e.tensor_scalar(E1, IIF, JJF, None, op0=ALU.is_equal), R=["TMP4", "c_jj", "MASKM"],
               W=["TMP4b"])
            op("dve", lambda e: e.tensor_scalar(E2, HF, HPF, None, op0=ALU.is_equal), R=["TMP4", "c_hp"], W=["TMP4b"])
            op("dve", lambda e: e.tensor_tensor(E1, E1, E2, op=ALU.mult), R=["TMP4b"], W=["TMP4b"])
            op("dve", lambda e: e.tensor_scalar(E2, DLF, -1.0, 1.0, op0=ALU.mult, op1=ALU.add), R=["TMP4"], W=["TMP4b"])
            op("dve", lambda e: e.tensor_tensor(DIAG0[:], E1, E2, op=ALU.mult), R=["TMP4b"], W=["DIAG0"])
            op("dve", lambda e: e.tensor_copy(REP16[:].rearrange("k (j h) -> k j h", j=8),
                                              ident_f[0:16, 0:16].unsqueeze(1).to_broadcast([16, 8, 16])),
               R=["ident_f"], W=["REP16"])
            op("pool", lambda e: e.memset(SGN[0:64, 0:1], 1.0), W=["SGN"])
            op("pool", lambda e: e.memset(SGN[64:128, 0:1], -1.0), W=["SGN"])
            op("pool", lambda e: e.memset(SGN[0:64, 1:2], -1.0), W=["SGN"])
            op("pool", lambda e: e.memset(SGN[64:128, 1:2], 1.0), W=["SGN"])

        _ssm_consts()
        kb.dma("pool", WR[:], w_router.rearrange("(k p) e -> p k e", p=128), W=["WR"], dkey="wr")
        kb.dma("sp", RB[:], router_bias.rearrange("(o e) -> o e", o=1).partition_broadcast(128).rearrange("p o e -> p (o e)"),
               W=["RB"], dkey="rb")

        x_v = x_d.rearrange("(c i) d -> c i d", i=NT)
        for q4 in range(4):
            kb.dma("sp", X[:, 4 * q4:4 * q4 + 4, :], x_v[:, 4 * q4:4 * q4 + 4, :],
                   W=[("X", i) for i in range(4 * q4, 4 * q4 + 4)], dkey=f"x{q4}")

        POSI = SM[:, 0:16].bitcast(I32)
        with nc.allow_non_contiguous_dma(reason="tiny position load"):
            kb.dma("sp", POSI, pos_d.rearrange("(n p) -> p n", p=128), W=["POSI"], dkey="pos")
        POSF = SM[:, 16:32]
        FREQ = SM[:, 32:64]
        ANG = TMP4[:, 0:512].rearrange("p (n k) -> p n k", k=32)
        ANG2 = TMP4[:, 512:1024].rearrange("p (n k) -> p n k", k=32)
        KI = TMP4b[:, 0:512].bitcast(I32).rearrange("p (n k) -> p n k", k=32)
        KF = TMP4b[:, 512:1024].rearrange("p (n k) -> p n k", k=32)
        op("dve", lambda e: e.tensor_copy(POSF, POSI), R=["POSI"], W=["POSF"])
        op("pool", lambda e: e.iota(FREQ, [[1, 32]], base=0, channel_multiplier=0,
                                    allow_small_or_imprecise_dtypes=True), W=["FREQ"])
        op("act", lambda e: e.activation(out=FREQ, in_=FREQ, func=AF.Exp, scale=-math.log(10000.0) / 32.0),
           R=["FREQ"], W=["FREQ"])
        op("dve", lambda e: e.tensor_tensor(ANG, POSF.unsqueeze(2).to_broadcast([128, NT, 32]),
                                            FREQ.unsqueeze(1).to_broadcast([128, NT, 32]), op=ALU.mult),
           R=["POSF", "FREQ"], W=["TMP4"])

        def sin_table(dst, src_ang, shift, keyname):
            op("dve", lambda e: e.tensor_scalar(ANG2, src_ang, shift, None, op0=ALU.add), R=["TMP4"], W=["TMP4"])
            op("dve", lambda e: e.tensor_scalar(KF, ANG2, 1.0 / TWO_PI, None, op0=ALU.mult), R=["TMP4"], W=["TMP4b"])
            op("dve", lambda e: e.tensor_copy(KI, KF), R=["TMP4b"], W=["TMP4b"])
            op("dve", lambda e: e.tensor_copy(KF, KI), R=["TMP4b"], W=["TMP4b"])
            op("dve", lambda e: e.scalar_tensor_tensor(ANG2, KF, -TWO_PI, ANG2, op0=ALU.mult, op1=ALU.add),
               R=["TMP4b", "TMP4"], W=["TMP4"])
            op("dve", lambda e: e.tensor_scalar(ANG2, ANG2, -3.1415925, 3.1415925, op0=ALU.max, op1=ALU.min),
               R=["TMP4"], W=["TMP4"])
            op("act", lambda e: e.activation(out=dst, in_=ANG2, func=AF.Sin), R=["TMP4"], W=[keyname])

        sin_table(SIN[:], ANG, 0.0, "SIN")
        sin_table(COS[:], ANG, math.pi / 2.0, "COS")

        C8 = SM[0:8, 64:192]
        kb.dma("sp", C8, c_d, W=["C8"], dkey="c8")
        op("pe", lambda e: e.matmul(ps[0][:, 0:8], C8, ident_f[0:8, 0:8], start=True, stop=True),
           R=["C8", "ident_f"], W=[("ps", 0)])
        SCT = SM[:, 192:200]
        op("act", lambda e: e.activation(out=SCT, in_=ps[0][:, 0:8], func=AF.Silu), R=[("ps", 0)], W=["SCT"])
        op("dve", lambda e: e.tensor_copy(SCrep[:], SCT.unsqueeze(2).to_broadcast([128, 8, 128])),
           R=["SCT"], W=["SCrep"])

        def ring_slot(s):
            return big_bf(2048 * s, 2048 * (s + 1))

        def adaln(l):
            for blk in range(6):
                if blk in (1, 4):
                    ng = (norm1_g if blk == 1 else norm2_g)
                    kb.dma("sp", TMP4b[:], ng[l:l + 1, :].partition_broadcast(128).rearrange("p o n -> p (o n)"),
                           W=["TMP4b"], dkey="ngbc")
                BIASB = TMP4[:] if blk % 2 == 0 else SM[:, 1024:2048]
                bkey = "TMP4" if blk % 2 == 0 else "SMbias"
                kb.dma("sp", BIASB, ada_b[l:l + 1, blk * D:(blk + 1) * D].partition_broadcast(128)
                       .rearrange("p o n -> p (o n)"), W=[bkey], dkey="adb%d" % (blk % 2))
                for half in range(2):
                    s = (blk * 2 + half) % 4
                    slot = ring_slot(s).rearrange("p (k n) -> p k n", k=8)
                    col0 = blk * D + half * 512
                    kb.dma("pool", slot, ada_w[l, :, col0:col0 + 512].rearrange("(k p) n -> p k n", p=128),
                           W=[("ring", s)], dkey=f"ring{s}")
                    pb = 2 + (s % 2)
                    for k in range(8):
                        op("pe", lambda e, k=k: e.matmul(ps[pb][:], SCrep[:, k, :], slot[:, k, :],
                                                         start=(k == 0), stop=(k == 7)),
                           R=["SCrep", ("ring", s)], W=[("ps", pb)], sig=(k == 7))
                    dst = MOD[:, blk, half * 512:(half + 1) * 512]
                    bias = BIASB[:, half * 512:(half + 1) * 512]
                    if blk in (1, 4):
                        tmp = SM[:, 512:1024]
                        op("dve", lambda e: e.tensor_tensor(tmp, ps[pb][:], bias, op=ALU.add),
                           R=[("ps", pb), bkey], W=["SMtmp"])
                        op("dve", lambda e: e.scalar_tensor_tensor(dst, tmp, 1.0, TMP4b[:, half * 512:(half + 1) * 512],
                                                                   op0=ALU.add, op1=ALU.mult),
                           R=["SMtmp", "TMP4b"], W=[("MOD", blk)])
                    else:
                        op("dve", lambda e: e.tensor_tensor(dst, ps[pb][:], bias, op=ALU.add),
                           R=[("ps", pb), bkey], W=[("MOD", blk)])

        def norm_to_FT(l, which, pos_of_tile, okey="FT", pbanks=(0, 1), part="both"):
            a_i, b_i = (1, 0) if which == 0 else (4, 3)
            junk = SM[:, 2048:2560].bitcast(BF16)
            if part in ("both", "stats"):
                for i in range(NT):
                    op("act", lambda e, i=i: e.activation(out=junk, in_=X[:, i, :], func=AF.Square,
                                                          accum_out=SS[:, i:i + 1]),
                       R=[("X", i)], W=["junk", ("SS", i)])
                op("dve", lambda e: e.tensor_scalar(SS[:, 16:32], SS[:, 0:16], 1.0 / D, EPS, op0=ALU.mult, op1=ALU.add),
                   R=[("SS", i) for i in range(NT)], W=["SSb"])
                op("pool", lambda e: e.tensor_tensor(SS[:, 32:48], SS[:, 16:32], NHALF[:, 0:16], op=ALU.pow),
                   R=["SSb", "NHALF"], W=["RSTD"])
            if part == "stats":
                return
            def tile(i):
                xs = i % 2
                t32 = TMP4[:] if xs == 0 else TMP4b[:]
                tkey = "TMP4" if xs == 0 else "TMP4b"
                xn = SM[:, 1024 + 512 * xs:1024 + 512 * (xs + 1)].bitcast(BF16)
                op("dve", lambda e, i=i: e.scalar_tensor_tensor(t32, X[:, i, :], SS[:, 32 + i:33 + i], MOD[:, a_i, :],
                                                                op0=ALU.mult, op1=ALU.mult),
                   R=[("X", i), "RSTD", ("MOD", a_i)], W=[tkey])
                yield
                op("dve", lambda e: e.tensor_tensor(xn, t32, MOD[:, b_i, :], op=ALU.add),
                   R=[tkey, ("MOD", b_i)], W=[("xn", xs)])
                yield
                pb = pbanks[xs]
                for k in range(8):
                    op("pe", lambda e, k=k: e.transpose(psb(pb)[:, k * 128:(k + 1) * 128],
                                                        xn[:, k * 128:(k + 1) * 128], ident_b[:]),
                       R=[("xn", xs), "ident_b"], W=[("ps", pb)], sig=(k == 7))
                    yield
                dst = pos_of_tile(i)
                op("act", lambda e: e.activation(out=dst, in_=psb(pb).rearrange("p (k c) -> p k c", k=8),
                                                 func=AF.Copy),
                   R=[("ps", pb)], W=[(okey, i)])
                yield

            run_rr([tile(i) for i in range(NT)], 2, 5)

        WIN3 = big_bf(0, 5120).rearrange("p (k n) -> p k n", k=8)
        UCM = big_bf(8192, 12288).rearrange("p (j f) -> p j f", j=16)
        UCMg = big_bf(8192, 12288).rearrange("p (g j h) -> p g j h", g=32, j=16)

        def layer_loads(l):
            kb.dma("pool", WIN3, w_in[l].rearrange("(k p) n -> p k n", p=128), W=["WIN"], dkey="win")
            QG = SM[:, 200:264]
            KG = SM[:, 264:328]
            SK = SM[:, 328:336]
            kb.dma("sp", QG, q_norm_g[l:l + 1, :].partition_broadcast(128).rearrange("p o n -> p (o n)"),
                   W=["QG"], dkey="qg")
            kb.dma("sp", KG, k_norm_g[l:l + 1, :].partition_broadcast(128).rearrange("p o n -> p (o n)"),
                   W=["KG"], dkey="kg")
            kb.dma("sp", SK, attn_sink[l:l + 1, :].partition_broadcast(128).rearrange("p o n -> p (o n)"),
                   W=["SK"], dkey="sk")
            op("dve", lambda e: e.tensor_copy(GQK[:, 0:8, :], QG.unsqueeze(1).to_broadcast([128, 8, 64])),
               R=["QG"], W=["GQK"])
            op("dve", lambda e: e.tensor_copy(GQK[:, 8:10, :], KG.unsqueeze(1).to_broadcast([128, 2, 64])),
               R=["KG"], W=["GQK"])
            op("act", lambda e: e.activation(out=ESINK[:], in_=SK, func=AF.Exp), R=["SK"], W=["ESINK"])
            with nc.allow_non_contiguous_dma(reason="tiny gain vectors"):
                kb.dma("sp", GROW[:, 0:4], attn_out_g[l].rearrange("(k p) -> p k", p=128), W=["GROW"], dkey="grow")
                kb.dma("sp", GROW[:, 4:8], ssm_out_g[l].rearrange("(k p) -> p k", p=128), W=["GROW"], dkey="grow")

        def u_proj(l):
            for j in range(NT):
                pb = 2 + (j % 2)
                for k in range(8):
                    op("pe", lambda e, k=k: e.matmul(ps[pb][:], FT3[:, k, j::16], WIN3[:, k, 768:1280],
                                                     start=(k == 0), stop=(k == 7)),
                       R=[("FT", j), "WIN"], W=[("ps", pb)], sig=(k == 7))
                eng = "act" if j % 2 == 0 else "dve"
                if eng == "act":
                    op("act", lambda e: e.activation(out=UCMg[:, :, j, :],
                                                     in_=ps[pb][:].rearrange("p (g h) -> p g h", g=32), func=AF.Copy),
                       R=[("ps", pb)], W=[("UCM", j)])
                else:
                    op("dve", lambda e: e.tensor_copy(UCMg[:, :, j, :], ps[pb][:].rearrange("p (g h) -> p g h", g=32)),
                       R=[("ps", pb)], W=[("UCM", j)])

        def attention(l):
            KT = big_bf(5120, 7168)[0:64, :].rearrange("p (n h t) -> p n h t", n=16, h=2)
            PT = [big_bf(7168 + 256 * j, 7168 + 256 * (j + 1)) for j in range(4)]
            QK = SM[:, 0:640]
            T1 = SM[:, 640:1280]
            T2 = SM[:, 1280:1920]
            QKN = SM[:, 1920:2240].bitcast(BF16)
            ST10 = SM[:, 2240:2250]
            RS10 = SM[:, 2250:2260]
            DEN = SM[:, 2260:2268]
            RDEN = SM[:, 2268:2276]
            ASS = SM[:, 2276:2277]
            ARS = SM[:, 2278:2279]
            QT = [TMP4[0:64, 512 * j:512 * (j + 1)].bitcast(BF16) for j in range(2)]
            ATT = TMP4b[:, 0:512]
            AN = TMP4b[:, 512:768].bitcast(BF16)
            VA = [TMP4b[:, 768 + 66 * j:768 + 66 * (j + 1)].bitcast(BF16).rearrange("p (h d) -> p h d", h=2)
                  for j in range(3)]
            ALLFT = [("FT", i) for i in range(NT)]
            for j in range(3):
                op("pool", lambda e, j=j: e.memset(VA[j][:, :, 64:66], 1.0), W=[("VA1", j)])
            T1v = T1.rearrange("p (h t d) -> p h t d", h=10, t=2)
            T2v = T2.rearrange("p (h t d) -> p h t d", h=10, t=2)
            def front(n):
                qs = n % 2
                for k in range(8):
                    op("pe", lambda e, k=k: e.matmul(ps[4][:], FT3[:, k, 128 * n:128 * n + 128], WIN3[:, k, 0:512],
                                                     start=(k == 0), stop=(k == 7)),
                       R=ALLFT + ["WIN"], W=[("ps", 4)], sig=(k == 7))
                    yield
                for k in range(8):
                    op("pe", lambda e, k=k: e.matmul(ps[5][:, 0:256], FT3[:, k, 128 * n:128 * n + 128],
                                                     WIN3[:, k, 512:768], start=(k == 0), stop=(k == 7)),
                       R=ALLFT + ["WIN"], W=[("ps", 5)], sig=(k == 7))
                    yield
                op("act", lambda e: e.activation(out=QK[:, 0:512], in_=ps[4][:], func=AF.Copy),
                   R=[("ps", 4)], W=["QK"])
                yield
                op("act", lambda e: e.activation(out=QK[:, 512:640], in_=ps[5][:, 0:128], func=AF.Copy),
                   R=[("ps", 5)], W=["QK"])
                yield
                op("act", lambda e: e.activation(out=VA[n % 3][:, :, 0:64],
                                                 in_=ps[5][:, 128:256].rearrange("p (h d) -> p h d", h=2),
                                                 func=AF.Copy),
                   R=[("ps", 5)], W=[("VA", n % 3)])
                yield
                junkq = T2[:, 0:32].bitcast(BF16)
                for h in range(10):
                    op("act", lambda e, h=h: e.activation(out=junkq, in_=QK[:, h * 64:(h + 1) * 64], func=AF.Square,
                                                          accum_out=ST10[:, h:h + 1]), R=["QK"], W=["T2", ("ST10", h)])
                    yield
                op("pool", lambda e: e.tensor_scalar(ST10, ST10, 1.0 / 64.0, EPS, op0=ALU.mult, op1=ALU.add),
                   R=[("ST10", h) for h in range(10)], W=["ST10"])
                yield
                op("pool", lambda e: e.tensor_tensor(RS10, ST10, NHALF[:, 0:10], op=ALU.pow),
                   R=["ST10", "NHALF"], W=["RS10"])
                yield
                op("pool", lambda e: e.tensor_tensor(T1, QK, GQK[:].rearrange("p h d -> p (h d)"), op=ALU.mult),
                   R=["QK", "GQK"], W=["T1"])
                yield
                sin_b = SIN[:, n, :].unsqueeze(1).to_broadcast([128, 10, 32])
                cos_b = COS[:, n, :].unsqueeze(1).unsqueeze(1).to_broadcast([128, 10, 2, 32])
                op("pool", lambda e: e.tensor_tensor(T2v[:, :, 0, :], T1v[:, :, 1, :], sin_b, op=ALU.mult),
                   R=["T1", "SIN"], W=["T2"])
                yield
                op("pool", lambda e: e.tensor_tensor(T2v[:, :, 1, :], T1v[:, :, 0, :], sin_b, op=ALU.mult),
                   R=["T1", "SIN"], W=["T2"])
                yield
                op("pool", lambda e: e.tensor_tensor(T1v, T1v, cos_b, op=ALU.mult), R=["T1", "COS", "T2"], W=["T1"])
                yield
                op("pool", lambda e: e.tensor_tensor(T1v[:, :, 0, :], T1v[:, :, 0, :], T2v[:, :, 0, :],
                                                     op=ALU.subtract), R=["T1", "T2"], W=["T1"])
                yield
                op("pool", lambda e: e.tensor_tensor(T1v[:, :, 1, :], T1v[:, :, 1, :], T2v[:, :, 1, :],
                                                     op=ALU.add), R=["T1", "T2"], W=["T1"])
                yield
                op("pool", lambda e: e.tensor_tensor(QKN.rearrange("p (h d) -> p h d", h=10),
                                                     T1.rearrange("p (h d) -> p h d", h=10),
                                                     RS10.unsqueeze(2).to_broadcast([128, 10, 64]), op=ALU.mult),
                   R=["T1", "RS10"], W=["QKN"])
                yield
                for h in range(8):
                    op("pe", lambda e, h=h: e.transpose(psb(0)[0:64, h * 128:(h + 1) * 128],
                                                        QKN[:, h * 64:(h + 1) * 64], ident_b[:]),
                       R=["QKN", "ident_b"], W=[("ps", 0)], sig=(h == 7))
                    yield
                for h in range(2):
                    op("pe", lambda e, h=h: e.transpose(psb(0)[64:128, h * 128:(h + 1) * 128],
                                                        QKN[:, 512 + h * 64:512 + (h + 1) * 64], ident_b[:]),
                       R=["QKN", "ident_b"], W=[("ps", 0)], sig=(h == 1))
                    yield
                op("act", lambda e: e.activation(out=QT[qs], in_=psb(0)[0:64, :], func=AF.Copy),
                   R=[("ps", 0)], W=[("QT", qs)])
                yield
                op("act", lambda e: e.activation(out=KT[:, n, :, :],
                                                 in_=psb(0)[64:128, 0:256].rearrange("p (h t) -> p h t", h=2),
                                                 func=AF.Copy),
                   R=[("ps", 0)], W=[("KT", n)])
                yield

            def back(n):
                qs = n % 2
                QT3 = QT[qs].rearrange("p (h t) -> p h t", h=8)
                halves = ([(n - 1, MBp)] if n > 0 else []) + [(n, MBc)]
                for kvh in range(2):
                    for hi, (nb, MB) in enumerate(halves):
                        cidx = 2 * kvh + hi
                        sbk = 2 + (cidx % 2)
                        pj = cidx % 4
                        op("pe", lambda e: e.matmul(ps[sbk][:], KT[:, nb, kvh, :],
                                                    QT3[:, 4 * kvh:4 * kvh + 4, :], start=True, stop=False),
                           R=[("KT", nb), ("QT", qs)], W=[("ps", sbk)], sig=False)
                        yield
                        op("pe", lambda e: e.matmul(ps[sbk][:], ident_b[:], MB[:].rearrange("p h q -> p (h q)"),
                                                    start=False, stop=True),
                           R=["ident_b", "MB"], W=[("ps", sbk)])
                        yield
                        op("act", lambda e: e.activation(out=PT[pj], in_=ps[sbk][:], func=AF.Exp, scale=0.125),
                           R=[("ps", sbk)], W=[("PT", pj)])
                        yield
                        for h in range(4):
                            op("pe", lambda e, h=h: e.matmul(ps[6 + kvh][:, h * 65:h * 65 + 65],
                                                             PT[pj][:, h * 128:(h + 1) * 128],
                                                             VA[nb % 3][:, kvh, 0:65],
                                                             start=(hi == 0 and h == 0), stop=(hi == len(halves) - 1 and h == 3),
                                                             skip_group_check=True),
                               R=[("PT", pj), ("VA", nb % 3), ("VA1", nb % 3)], W=[("ps", 6 + kvh)], sig=(h == 3))
                            yield
                pv2 = PSALL[:, 3072:4096].rearrange("p (b x) -> p b x", b=2)[:, :, 0:260].rearrange(
                    "p b (h d) -> p b h d", h=4)
                k67 = [("ps", 6), ("ps", 7)]
                op("dve", lambda e: e.tensor_tensor(DEN.rearrange("p (b h o) -> p b h o", b=2, o=1), pv2[:, :, :, 64:65],
                                                    ESINK[:].rearrange("p (b h o) -> p b h o", b=2, o=1), op=ALU.add),
                   R=k67 + ["ESINK"], W=["DEN"])
                yield
                op("dve", lambda e: e.reciprocal(RDEN, DEN), R=["DEN"], W=["RDEN"])
                yield
                op("dve", lambda e: e.tensor_tensor(
                    ATT.rearrange("p (b h d) -> p b h d", b=2, h=4), pv2[:, :, :, 0:64],
                    RDEN.rearrange("p (b h) -> p b h", b=2).unsqueeze(3).to_broadcast([128, 2, 4, 64]), op=ALU.mult),
                   R=k67 + ["RDEN"], W=["ATT"])
                yield
                op("dve", lambda e: e.scalar_tensor_tensor(AN, ATT, 1.0, ATT, op0=ALU.mult, op1=ALU.mult, accum_out=ASS),
                   R=["ATT"], W=["AN", "ASS"])
                yield
                op("dve", lambda e: e.tensor_scalar(ASS, ASS, 1.0 / 512.0, EPS, op0=ALU.mult, op1=ALU.add),
                   R=["ASS"], W=["ASS"])
                yield
                op("pool", lambda e: e.tensor_tensor(ARS, ASS, NHALF[:, 0:1], op=ALU.pow),
                   R=["ASS", "NHALF"], W=["ARS"])
                yield
                op("dve", lambda e: e.tensor_scalar(AN, ATT, ARS, None, op0=ALU.mult), R=["ATT", "ARS"], W=["AN"])
                yield
                for k in range(4):
                    op("pe", lambda e, k=k: e.transpose(psb(1)[:, 512 + k * 128:512 + (k + 1) * 128],
                                                        AN[:, k * 128:(k + 1) * 128], ident_b[:]),
                       R=["AN", "ident_b"], W=[("ps", 1)], sig=(k == 3))
                    yield
                op("act", lambda e: e.activation(out=AT3[:, :, 128 * n:128 * n + 128],
                                                 in_=psb(1)[:, 512:1024].rearrange("p (k t) -> p k t", k=4),
                                                 func=AF.Copy),
                   R=[("ps", 1)], W=[("AT", n)])
                yield

            run_rr([front(0)], 1)
            for n in range(NT):
                gs = [back(n)] + ([front(n + 1)] if n + 1 < NT else [])
                run_rr(gs, 2)

        FTf = FT[:].bitcast(F32)
        BZCL = big_bf(0, 4096).rearrange("p (g a m) -> p g a m", g=32, a=2)
        MW = big_bf(4096, 8192).rearrange("p (g a m) -> p g a m", g=32, a=2)
        UT = FT[:, 0:8192].rearrange("p (g a c) -> p g a c", g=32, a=2)
        ZS = FTf[:, 4096:8192].rearrange("p (r g c) -> p r g c", r=2, g=16)
        XSB = SM[:, 0:2048].bitcast(BF16).rearrange("p (g c) -> p g c", g=32)
        S34 = TMP4b[:].rearrange("p (gh t g h) -> p gh t g h", gh=2, t=2, g=16)

        def sincos(dst_s, dst_c, ang, t_a, t_k, t_ki, keys_ang, key_a, key_k, wkey):
            for dst, shift in ((dst_c, math.pi / 2.0), (dst_s, 0.0)):
                op("dve", lambda e: e.tensor_scalar(t_a, ang, shift, None, op0=ALU.add), R=keys_ang, W=[key_a])
                op("dve", lambda e: e.tensor_scalar(t_k, t_a, 1.0 / TWO_PI, None, op0=ALU.mult), R=[key_a], W=[key_k])
                op("dve", lambda e: e.tensor_copy(t_ki, t_k), R=[key_k], W=[key_k])
                op("dve", lambda e: e.tensor_copy(t_k, t_ki), R=[key_k], W=[key_k])
                op("dve", lambda e: e.scalar_tensor_tensor(t_a, t_k, -TWO_PI, t_a, op0=ALU.mult, op1=ALU.add),
                   R=[key_k, key_a], W=[key_a])
                op("dve", lambda e: e.tensor_scalar(t_a, t_a, -3.1415925, 3.1415925, op0=ALU.max, op1=ALU.min),
                   R=[key_a], W=[key_a])
                op("act", lambda e, dst=dst: e.activation(out=dst, in_=t_a, func=AF.Sin), R=[key_a], W=[wkey])

        def ssm_tables(l, gh, taus, PC, PS, tmpE, tmpA, tmpK, AB, ADT, ldsm):
            T = sum(c for _, _, c in taus)
            g0 = 16 * gh
            LL = ldsm[0:16, 0:256]
            kb.dma("sp", LL[:, 0:64], lam_re[l, g0:g0 + 16, :], W=["ldsm"], dkey="ll")
            kb.dma("sp", LL[:, 64:128], lam_re[l, g0:g0 + 16, :], W=["ldsm"], dkey="ll")
            kb.dma("sp", LL[:, 128:192], lam_im[l, g0:g0 + 16, :], W=["ldsm"], dkey="ll")
            kb.dma("sp", LL[:, 192:256], lam_im[l, g0:g0 + 16, :], W=["ldsm"], dkey="ll")
            op("pe", lambda e: e.matmul(ps[6][:, 0:16], LL[:, 0:128], ident_f[0:16, 0:16], start=True, stop=False),
               R=["ldsm", "ident_f"], W=[("ps", 6)])
            op("pe", lambda e: e.matmul(ps[6][:, 16:32], LL[:, 128:256], ident_f[0:16, 0:16], start=False, stop=True),
               R=["ldsm", "ident_f"], W=[("ps", 6)])
            op("act", lambda e: e.activation(out=AB[:, 0:32], in_=ps[6][:, 0:32], func=AF.Copy), R=[("ps", 6)], W=["AB"])
            DT = ADT[:, 32:48]
            kb.dma("sp", DT, ssm_log_dt[l:l + 1, g0:g0 + 16].partition_broadcast(128).rearrange("p o n -> p (o n)"),
                   W=["ADT"], dkey="dt")
            op("act", lambda e: e.activation(out=DT, in_=DT, func=AF.Exp), R=["ADT"], W=["ADT"])
            op("dve", lambda e: e.tensor_tensor(ADT[:, 0:16], AB[:, 0:16], DT, op=ALU.mult), R=["AB", "ADT"], W=["ADT"])
            op("dve", lambda e: e.tensor_tensor(ADT[:, 16:32], AB[:, 16:32], DT, op=ALU.mult), R=["AB", "ADT"], W=["ADT"])
            TAU = ADT[:, 48:48 + T]
            o = 0
            for (b0, st, c) in taus:
                op("pool", lambda e, o=o, b0=b0, st=st, c=c: e.iota(TAU[:, o:o + c], [[st, c]], base=b0,
                                                                     channel_multiplier=0,
                                                                     allow_small_or_imprecise_dtypes=True),
                   W=["ADT"])
                o += c
            sh = [128, 16, T]
            op("dve", lambda e: e.tensor_tensor(tmpE, ADT[:, 0:16].unsqueeze(2).to_broadcast(sh),
                                                TAU.unsqueeze(1).to_broadcast(sh), op=ALU.mult), R=["ADT"], W=["tmpE"])
            op("act", lambda e: e.activation(out=tmpE, in_=tmpE, func=AF.Exp), R=["tmpE"], W=["tmpE"])
            op("dve", lambda e: e.tensor_tensor(PS, ADT[:, 16:32].unsqueeze(2).to_broadcast(sh),
                                                TAU.unsqueeze(1).to_broadcast(sh), op=ALU.mult), R=["ADT"], W=["PS"])
            sincos(PS, PC, PS, tmpA, tmpK, tmpK.bitcast(I32), ["PS"], "tmpA", "tmpK", "PCS")
            op("dve", lambda e: e.tensor_tensor(PC, PC, tmpE, op=ALU.mult), R=["PCS", "tmpE"], W=["PC"])
            op("dve", lambda e: e.tensor_tensor(PS, PS, tmpE, op=ALU.mult), R=["PCS", "tmpE", "PS"], W=["PS"])

        def stack_load(l, gh, src_r, src_i, ld, ldkey, dst, dstkey, kind, psbank, scale_col):
            g0 = 16 * gh
            if kind == "C":
                L4 = ld[0:16, :].rearrange("g (h r p) -> g h r p", h=16, r=2)
                kb.dma("sp", L4[:, :, 0, :], src_r[l, g0:g0 + 16, :, :], W=[ldkey], dkey=ldkey)
                kb.dma("sp", L4[:, :, 1, :], src_i[l, g0:g0 + 16, :, :], W=[ldkey], dkey=ldkey)
                for h in range(16):
                    op("pe", lambda e, h=h: e.matmul(ps[psbank][:, h * 16:(h + 1) * 16],
                                                     L4[:, h, :, :].rearrange("g r p -> g (r p)"),
                                                     ident_f[0:16, 0:16], start=(h == 0), stop=(h == 15)),
                       R=[ldkey, "ident_f"], W=[("ps", psbank)], sig=(h == 15))
            else:
                L4 = ld[0:16, :].rearrange("g (r p h) -> g r p h", r=2, p=64)
                kb.dma("sp", L4[:, 0, :, :], src_r[l, g0:g0 + 16, :, :], W=[ldkey], dkey=ldkey)
                kb.dma("sp", L4[:, 1, :, :], src_i[l, g0:g0 + 16, :, :], W=[ldkey], dkey=ldkey)
                for h in range(16):
                    op("pe", lambda e, h=h: e.matmul(ps[psbank][:, h * 16:(h + 1) * 16],
                                                     L4[:, :, :, h].rearrange("g r p -> g (r p)"),
                                                     ident_f[0:16, 0:16], start=(h == 0), stop=(h == 15)),
                       R=[ldkey, "ident_f"], W=[("ps", psbank)], sig=(h == 15))
            op("dve", lambda e: e.tensor_scalar(dst, ps[psbank][:, 0:256].rearrange("p (h g) -> p g h", h=16),
                                                scale_col, None, op0=ALU.mult),
               R=[("ps", psbank), "SGN"], W=[dstkey])

        def outer_combine(dst_bf, PCv, PSv, Sa, Sb, nt, keysR, wkey, pbanks):
            sh = [128, 16, nt, 16]
            tmp = PSALL[:, 512 * pbanks:512 * pbanks + 16 * nt * 16].rearrange("p (g t h) -> p g t h", g=16, t=nt)
            pk = [("ps", pbanks + j) for j in range((16 * nt * 16 + 511) // 512)]
            op("dve", lambda e: e.tensor_tensor(tmp, PCv.unsqueeze(3).to_broadcast(sh),
                                                Sa.unsqueeze(2).to_broadcast(sh), op=ALU.mult), R=keysR, W=pk)
            op("pool", lambda e: e.tensor_tensor(dst_bf, PSv.unsqueeze(3).to_broadcast(sh),
                                                 Sb.unsqueeze(2).to_broadcast(sh), op=ALU.mult), R=keysR, W=[wkey])
            op("dve", lambda e: e.tensor_tensor(dst_bf, tmp, dst_bf, op=ALU.add), R=pk + [wkey], W=[wkey])

        def ssm_gen_A(l, gh):
            g0 = 16 * gh
            ldA = FTf[:, 0:2048]
            ldB = FTf[:, 2048:4096]
            ST = FTf[:, 4096:5632].rearrange("p (s g h) -> p s g h", s=6, g=16)
            BzT = FT[:, 2 * 5632:2 * 7680].rearrange("p (g a m) -> p g a m", g=16, a=2)
            CF = FT[:, 0:4096].rearrange("p (g a m) -> p g a m", g=16, a=2)
            PC = SM[:, 0:512].rearrange("p (g t) -> p g t", g=16)
            PS = SM[:, 512:1024].rearrange("p (g t) -> p g t", g=16)
            tmpE = SM[:, 1024:1536].rearrange("p (g t) -> p g t", g=16)
            tmpA = SM[:, 1536:2048].rearrange("p (g t) -> p g t", g=16)
            tmpK = SM[:, 2048:2560].rearrange("p (g t) -> p g t", g=16)
            AB = TMP4[:, 0:32]
            ADT = TMP4[:, 32:160]
            KAP = TMP4[:, 160:320]
            ssm_tables(l, gh, [(15, -1, 16), (-7, 1, 16)], PC, PS, tmpE, tmpA, tmpK, AB, ADT, TMP4[:, 512:768])
            S3p = S34[:, gh, 0, :, :]
            S4p = S34[:, gh, 1, :, :]
            stack_load(l, gh, ssm_c_re, ssm_c_im, ldA, "ldA", S3p, "S34", "C", 7, SGN[:, 0:1])
            stack_load(l, gh, ssm_c_im, ssm_c_re, ldB, "ldB", ST[:, 1], "ST1", "C", 6, -1.0)
            op("pool", lambda e: e.tensor_copy(S4p, ST[:, 1]), R=["ST1"], W=["S34"])
            stack_load(l, gh, ssm_b_re, ssm_b_im, ldA, "ldA", ST[:, 2], "ST2", "B", 7, 1.0)
            stack_load(l, gh, ssm_b_im, ssm_b_re, ldB, "ldB", ST[:, 3], "ST3", "B", 6, SGN[:, 1:2])
            a_ = AB[:, 0:16]
            b_ = AB[:, 16:32]
            L1r = PC[:, :, 24]
            L1i = PS[:, :, 24]
            nr, den, t1, t2, kr, ki = [KAP[:, 16 * j:16 * (j + 1)] for j in range(6)]
            op("dve", lambda e: e.tensor_scalar(nr, L1r, -1.0, None, op0=ALU.add), R=["PC"], W=["KAP"])
            op("dve", lambda e: e.tensor_tensor(den, a_, a_, op=ALU.mult), R=["AB"], W=["KAP"])
            op("dve", lambda e: e.tensor_tensor(t1, b_, b_, op=ALU.mult), R=["AB"], W=["KAP"])
            op("dve", lambda e: e.tensor_tensor(den, den, t1, op=ALU.add), R=["KAP"], W=["KAP"])
            op("dve", lambda e: e.reciprocal(den, den), R=["KAP"], W=["KAP"])
            op("dve", lambda e: e.tensor_tensor(t1, nr, a_, op=ALU.mult), R=["KAP", "AB"], W=["KAP"])
            op("dve", lambda e: e.tensor_tensor(t2, L1i, b_, op=ALU.mult), R=["PS", "AB"], W=["KAP"])
            op("dve", lambda e: e.tensor_tensor(t1, t1, t2, op=ALU.add), R=["KAP"], W=["KAP"])
            op("dve", lambda e: e.tensor_tensor(kr, t1, den, op=ALU.mult), R=["KAP"], W=["KAP"])
            op("dve", lambda e: e.tensor_tensor(t1, L1i, a_, op=ALU.mult), R=["PS", "AB", "KAP"], W=["KAP"])
            op("dve", lambda e: e.tensor_tensor(t2, nr, b_, op=ALU.mult), R=["KAP", "AB"], W=["KAP"])
            op("dve", lambda e: e.tensor_tensor(t1, t1, t2, op=ALU.subtract), R=["KAP"], W=["KAP"])
            op("dve", lambda e: e.tensor_tensor(ki, t1, den, op=ALU.mult), R=["KAP"], W=["KAP"])
            sh3 = [128, 16, 16]
            krb = kr.unsqueeze(2).to_broadcast(sh3)
            kib = ki.unsqueeze(2).to_broadcast(sh3)
            op("dve", lambda e: e.tensor_tensor(ST[:, 4], ST[:, 2], krb, op=ALU.mult), R=["ST2", "KAP"], W=["ST4"])
            op("dve", lambda e: e.tensor_tensor(ST[:, 0], ST[:, 3], kib, op=ALU.mult), R=["ST3", "KAP"], W=["ST0"])
            op("dve", lambda e: e.tensor_tensor(ST[:, 4], ST[:, 4], ST[:, 0], op=ALU.add), R=["ST4", "ST0"], W=["ST4"])
            op("dve", lambda e: e.tensor_tensor(ST[:, 5], ST[:, 3], krb, op=ALU.mult), R=["ST3", "KAP"], W=["ST5"])
            op("dve", lambda e: e.tensor_tensor(ST[:, 0], ST[:, 2], kib, op=ALU.mult), R=["ST2", "KAP", "ST4"], W=["ST0"])
            op("dve", lambda e: e.tensor_tensor(ST[:, 5], ST[:, 5], ST[:, 0], op=ALU.subtract), R=["ST5", "ST0"], W=["ST5"])
            outer_combine(BzT.rearrange("p g a (j h) -> p g (a j) h", h=16), PC[:, :, 0:16], PS[:, :, 0:16],
                          ST[:, 4], ST[:, 5], 16, ["PC", "PS", "ST4", "ST5"], "BzT", 0)
            outer_combine(CF.rearrange("p g a (j h) -> p g (a j) h", h=16), PC[:, :, 16:32], PS[:, :, 16:32],
                          S3p, S4p, 16, ["PC", "PS", "S34", "ldA", "ldB"], "ldA", 0)
            for gp in range(8):
                pb = 4 + (gp % 2)
                for j in range(4):
                    g, jb = 2 * gp + j // 2, j % 2
                    op("pe", lambda e, g=g, jb=jb, j=j: e.matmul(ps[pb][:, j * 128:(j + 1) * 128], BzT[:, g, jb, :],
                                                               ident_b[:], start=(j == 0), stop=(j == 3)),
                       R=["BzT", "ident_b"], W=[("ps", pb)], sig=(j == 3))
                op("act", lambda e: e.activation(
                    out=BZCL[:, g0 + 2 * gp:g0 + 2 * gp + 2, :, :].rearrange("p g a m -> p (g a m)"),
                    in_=ps[pb][:], func=AF.Copy), R=[("ps", pb)], W=["BZCL"])
            for g in range(16):
                pb = 6 + (g % 2)
                op("pe", lambda e, g=g: e.matmul(ps[pb][:, 0:256], BzT[:, g, 1, :],
                                                 CF[:, g, :, :].rearrange("p a m -> p (a m)"), start=True, stop=True),
                   R=["BzT", "ldA"], W=[("ps", pb)])
                tm = TMP4[:, 768:1024].bitcast(BF16)[:, 0:256] if g % 2 == 0 else TMP4[:, 896:1024].bitcast(BF16)
                tkey = "tmM0" if g % 2 == 0 else "tmM1"
                tm = TMP4[:, 768:896].bitcast(BF16) if g % 2 == 0 else TMP4[:, 896:1024].bitcast(BF16)
                op("dve", lambda e: e.tensor_tensor(tm, ps[pb][:, 0:256], MASKM[:], op=ALU.mult),
                   R=[("ps", pb), "MASKM"], W=[tkey])
                op("dve", lambda e, g=g: e.scalar_tensor_tensor(
                    MW[:, g0 + g, :, :].rearrange("p a m -> p (a m)"), DIAG0[:], DCOL[:, g0 + g:g0 + g + 1], tm,
                    op0=ALU.mult, op1=ALU.add), R=["DIAG0", "DCOL", tkey], W=["MW"])

        def ssm_gen_B(l, gh):
            g0 = 16 * gh
            PC = TMP4[:, 0:256].rearrange("p (g t) -> p g t", g=16)
            PS = TMP4[:, 256:512].rearrange("p (g t) -> p g t", g=16)
            tmpE = TMP4[:, 512:768].rearrange("p (g t) -> p g t", g=16)
            tmpA = TMP4[:, 768:1024].rearrange("p (g t) -> p g t", g=16)
            tmpK = SM[:, 2048:2304].rearrange("p (g t) -> p g t", g=16)
            AB = SM[:, 2304:2336]
            ADT = SM[:, 2336:2432]
            ssm_tables(l, gh, [(1, 1, 16)], PC, PS, tmpE, tmpA, tmpK, AB, ADT, SM[:, 0:256])
            outer_combine(BZCL[:, g0:g0 + 16, :, :].rearrange("p g a (j h) -> p g (a j) h", h=16), PC, PS,
                          S34[:, gh, 0, :, :], S34[:, gh, 1, :, :], 16, ["PC", "PS", "S34"], "BZCL", 0)

        PWR = SM[:, 512:768].rearrange("p (g t) -> p g t", g=16)
        PWI = SM[:, 768:1024].rearrange("p (g t) -> p g t", g=16)

        def ssm_scan_coefs(l):
            LL = SM[0:16, 0:256]
            kb.dma("sp", LL[:, 0:128].rearrange("g (a p) -> g a p", a=2),
                   lam_re[l].rearrange("(a g) p -> g a p", a=2), W=["ldsm"], dkey="scl")
            kb.dma("sp", LL[:, 128:256].rearrange("g (a p) -> g a p", a=2),
                   lam_im[l].rearrange("(a g) p -> g a p", a=2), W=["ldsm"], dkey="scl")
            op("pe", lambda e: e.matmul(ps[6][:, 32:48], LL[:, 0:128], ident_f[0:16, 0:16], start=True, stop=False),
               R=["ldsm", "ident_f"], W=[("ps", 6)])
            op("pe", lambda e: e.matmul(ps[6][:, 48:64], LL[:, 128:256], ident_f[0:16, 0:16], start=False, stop=True),
               R=["ldsm", "ident_f"], W=[("ps", 6)])
            A_ = TMP4[:, 0:16]
            B_ = TMP4[:, 16:32]
            DT = TMP4[:, 32:48]
            TAU = TMP4[:, 48:64]
            g3 = lambda ap: ap.rearrange("p (g t) -> p g t", g=16)
            EA = g3(TMP4[:, 64:320])
            AN = g3(TMP4[:, 320:576])
            tA = g3(TMP4[:, 576:832])
            tK = g3(SM[:, 256:512])
            op("act", lambda e: e.activation(out=TMP4[:, 0:32], in_=ps[6][:, 32:64], func=AF.Copy), R=[("ps", 6)], W=["TMP4"])
            for a in range(2):
                kb.dma("sp", DT[64 * a:64 * a + 64, :],
                       ssm_log_dt[l:l + 1, 16 * a:16 * a + 16].partition_broadcast(64).rearrange("p o n -> p (o n)"),
                       W=["TMP4"], dkey="dts")
            op("act", lambda e: e.activation(out=DT, in_=DT, func=AF.Exp), R=["TMP4"], W=["TMP4"])
            op("pool", lambda e: e.iota(TAU, [[16, 16]], base=16, channel_multiplier=0,
                                        allow_small_or_imprecise_dtypes=True), R=["TMP4"], W=["TMP4"])
            op("dve", lambda e: e.tensor_tensor(A_, A_, DT, op=ALU.mult), R=["TMP4"], W=["TMP4"])
            op("dve", lambda e: e.tensor_tensor(B_, B_, DT, op=ALU.mult), R=["TMP4"], W=["TMP4"])
            sh = [128, 16, 16]
            op("dve", lambda e: e.tensor_tensor(EA, A_.unsqueeze(2).to_broadcast(sh), TAU.unsqueeze(1).to_broadcast(sh),
                                                op=ALU.mult), R=["TMP4"], W=["TMP4"])
            op("act", lambda e: e.activation(out=EA, in_=EA, func=AF.Exp), R=["TMP4"], W=["TMP4"])
            op("dve", lambda e: e.tensor_tensor(AN, B_.unsqueeze(2).to_broadcast(sh), TAU.unsqueeze(1).to_broadcast(sh),
                                                op=ALU.mult), R=["TMP4"], W=["TMP4"])
            sincos(PWI, PWR, AN, tA, tK, tK.bitcast(I32), ["TMP4"], "TMP4", "tKc", "PW")
            op("dve", lambda e: e.tensor_tensor(PWR, PWR, EA, op=ALU.mult), R=["PW", "TMP4"], W=["PW"])
            op("dve", lambda e: e.tensor_tensor(PWI, PWI, EA, op=ALU.mult), R=["PW", "TMP4"], W=["PW"])
            for j, t in ((0, 0), (1, 15)):
                op("dve", lambda e, j=j, t=t: e.tensor_copy(LAB[:, j, 0, 0, :], PWR[:, :, t]), R=["PW"], W=["LAB"])
                op("dve", lambda e, j=j, t=t: e.tensor_copy(LAB[:, j, 0, 1, :], PWR[:, :, t]), R=["PW"], W=["LAB"])
                op("dve", lambda e, j=j, t=t: e.tensor_copy(LAB[:, j, 1, 1, :], PWI[:, :, t]), R=["PW"], W=["LAB"])
                op("dve", lambda e, j=j, t=t: e.tensor_scalar(LAB[:, j, 1, 0, :], PWI[:, :, t], -1.0, None, op0=ALU.mult),
                   R=["PW"], W=["LAB"])

        def ssm_main(l):
            DT_ = SS[0:16, 0:32]
            with nc.allow_non_contiguous_dma(reason="tiny D load"):
                kb.dma("sp", DT_, ssm_d[l].rearrange("(g h) -> h g", h=16), W=["dT"], dkey="dT")
            op("pe", lambda e: e.matmul(ps[5][:, 0:32], REP16[:], DT_, start=True, stop=True),
               R=["REP16", "dT"], W=[("ps", 5)])
            op("act", lambda e: e.activation(out=DCOL[:], in_=ps[5][:, 0:32], func=AF.Copy), R=[("ps", 5)], W=["DCOL"])
            ssm_gen_A(l, 0)
            if debug is not None and debug[0] == f"tab{l}":
                kb.barrier()
                kb.dma("sp", dbg_d[:, 0:1024], SM[:, 0:1024], dkey="dbg")
                kb.dma("sp", dbg_d[:, 1024:2560], FTf[:, 4096:5632], dkey="dbg")
                kb.dma("sp", dbg_d[:, 2560:2880], TMP4[:, 0:320], dkey="dbg")
                kb.dma("sp", dbg_d[:, 2880:3904], TMP4b[:, 0:1024], dkey="dbg")
                return True
            ssm_gen_A(l, 1)
            if debug is not None and debug[0] == f"bz{l}":
                dump(BIG[:, 0:8192], 8192, F32, ["BZCL", "MW"])
                return True
            kb.barrier()
            ALLU = [("UCM", j) for j in range(NT)]
            for b8 in range(8):
                pb = b8 % 2
                for j in range(8):
                    g, jb = 4 * b8 + j // 2, j % 2
                    op("pe", lambda e, g=g, jb=jb, j=j: e.transpose(
                        psb(pb)[:, j * 128:(j + 1) * 128], UCMg[:, g, 8 * jb:8 * jb + 8, :].rearrange("p j h -> p (j h)"),
                        ident_b[:]),
                       R=ALLU + ["ident_b"], W=[("ps", pb)], sig=(j == 7))
                eng = "act" if b8 % 2 == 0 else "dve"
                dst = UT[:, 4 * b8:4 * b8 + 4, :, :].rearrange("p g a c -> p (g a c)")
                if eng == "act":
                    op("act", lambda e: e.activation(out=dst, in_=psb(pb), func=AF.Copy), R=[("ps", pb)], W=["UT"])
                else:
                    op("dve", lambda e: e.tensor_copy(dst, psb(pb)), R=[("ps", pb)], W=["UT"])
            for g4 in range(8):
                pb = 2 + (g4 % 2)
                for j in range(4):
                    g = 4 * g4 + j
                    for jb in range(2):
                        op("pe", lambda e, g=g, jb=jb, j=j: e.matmul(ps[pb][:, j * 128:(j + 1) * 128], BZCL[:, g, jb, :],
                                                                   UT[:, g, jb, :], start=(j == 0 and jb == 0),
                                                                   stop=(j == 3 and jb == 1)),
                           R=["BZCL", "UT"], W=[("ps", pb)], sig=(j == 3 and jb == 1))
                ghh, gl0 = g4 // 4, 4 * (g4 % 4)
                pv = ps[pb][:].rearrange("p (g c) -> p g c", g=4)
                op("act", lambda e: e.activation(out=ZS[64 * ghh:64 * ghh + 64, 0, gl0:gl0 + 4, :], in_=pv[0:64],
                                                 func=AF.Copy), R=[("ps", pb)], W=["ZS"])
                op("dve", lambda e: e.tensor_copy(ZS[64 * ghh:64 * ghh + 64, 1, gl0:gl0 + 4, :], pv[64:128]),
                   R=[("ps", pb)], W=["ZS"])
            if debug is not None and debug[0] == f"z0{l}":
                dump(FTf[:, 4096:8192], 4096, F32, ["ZS"])
                return True
            ssm_scan_coefs(l)
            kb.barrier()
            ssm_gen_B(l, 0)
            ssm_gen_B(l, 1)
            ZS5 = FTf[:, 4096:8192].rearrange("p (r g b s) -> p r g b s", r=2, g=16, b=8)
            SCb = SM[:, 1024:1536].rearrange("p (a r g b) -> p a r g b", a=2, r=2, g=16)
            sh4 = [128, 2, 16, 8]
            sh3 = [128, 16, 8]
            for s_ in range(1, 16):
                prev = ZS5[:, :, :, :, s_ - 1]
                cur = ZS5[:, :, :, :, s_]
                op("pool", lambda e: e.tensor_tensor(SCb[:, 0], prev, LAB[:, 0, 0].unsqueeze(3).to_broadcast(sh4),
                                                     op=ALU.mult), R=["ZS", "LAB"], W=["SC0"])
                op("pool", lambda e: e.tensor_tensor(SCb[:, 1, 0], prev[:, 1], LAB[:, 0, 1, 0, :].unsqueeze(2).to_broadcast(sh3),
                                                     op=ALU.mult), R=["ZS", "LAB"], W=["SC1"])
                op("pool", lambda e: e.tensor_tensor(SCb[:, 1, 1], prev[:, 0], LAB[:, 0, 1, 1, :].unsqueeze(2).to_broadcast(sh3),
                                                     op=ALU.mult), R=["ZS", "LAB"], W=["SC1"])
                op("pool", lambda e: e.tensor_tensor(SCb[:, 0], SCb[:, 0], SCb[:, 1], op=ALU.add), R=["SC0", "SC1"], W=["SC0"])
                op("pool", lambda e: e.tensor_tensor(cur, cur, SCb[:, 0], op=ALU.add), R=["ZS", "SC0"], W=["ZS"])
            SC2 = SM[:, 1536:1600].rearrange("p (a r g) -> p a r g", a=2, r=2)
            for bk in range(1, 8):
                prev = ZS5[:, :, :, bk - 1, 15]
                cur = ZS5[:, :, :, bk, 15]
                op("pool", lambda e: e.tensor_tensor(SC2[:, 0], prev, LAB[:, 1, 0], op=ALU.mult), R=["ZS", "LAB"], W=["SC20"])
                op("pool", lambda e: e.tensor_tensor(SC2[:, 1, 0, :], prev[:, 1, :], LAB[:, 1, 1, 0, :], op=ALU.mult),
                   R=["ZS", "LAB"], W=["SC21"])
                op("pool", lambda e: e.tensor_tensor(SC2[:, 1, 1, :], prev[:, 0, :], LAB[:, 1, 1, 1, :], op=ALU.mult),
                   R=["ZS", "LAB"], W=["SC21"])
                op("pool", lambda e: e.tensor_tensor(SC2[:, 0], SC2[:, 0], SC2[:, 1], op=ALU.add), R=["SC20", "SC21"], W=["SC20"])
                op("pool", lambda e: e.tensor_tensor(cur, cur, SC2[:, 0], op=ALU.add), R=["ZS", "SC20"], W=["ZS"])
            F_ = [SM[:, 1600:1840].rearrange("p (g s) -> p g s", g=16),
                  SM[:, 1024:1264].rearrange("p (g s) -> p g s", g=16)]
            sh15 = [128, 16, 15]
            pr = PWR[:, :, 0:15]
            pi_ = PWI[:, :, 0:15]
            for bk in range(1, 8):
                cr = ZS5[:, 0, :, bk - 1, 15].unsqueeze(2).to_broadcast(sh15)
                ci = ZS5[:, 1, :, bk - 1, 15].unsqueeze(2).to_broadcast(sh15)
                re = ZS5[:, 0, :, bk, 0:15]
                im = ZS5[:, 1, :, bk, 0:15]
                for (dst, pa, ca, pb_, cb, sub) in ((re, pr, cr, pi_, ci, True), (im, pr, ci, pi_, cr, False)):
                    op("pool", lambda e, pa=pa, ca=ca: e.tensor_tensor(F_[0], pa, ca, op=ALU.mult), R=["PW", "ZS"], W=["F0"])
                    op("pool", lambda e, pb_=pb_, cb=cb: e.tensor_tensor(F_[1], pb_, cb, op=ALU.mult), R=["PW", "ZS"], W=["SC0"])
                    op("pool", lambda e, dst=dst: e.tensor_tensor(dst, dst, F_[0], op=ALU.add), R=["F0", "ZS"], W=["ZS"])
                    op("pool", lambda e, dst=dst, sub=sub: e.tensor_tensor(dst, dst, F_[1],
                                                                      op=(ALU.subtract if sub else ALU.add)),
                       R=["SC0", "ZS"], W=["ZS"])
            if debug is not None and debug[0] == f"zs{l}":
                dump(FTf[:, 4096:8192], 4096, F32, ["ZS"])
                return True
            op("pool", lambda e: e.memset(XSB[:, :, 0:1], 0.0), R=["ZS"], W=["XSB", "ldsm"])
            for ghh in range(2):
                for r in range(2):
                    eng = "act" if r == 0 else "dve"
                    src = ZS[64 * ghh:64 * ghh + 64, r, :, 0:127]
                    dst = XSB[64 * r:64 * r + 64, 16 * ghh:16 * ghh + 16, 1:128]
                    if eng == "act":
                        op("act", lambda e: e.activation(out=dst, in_=src, func=AF.Copy), R=["ZS"], W=["XSB", "ldsm"])
                    else:
                        op("dve", lambda e: e.tensor_copy(dst, src), R=["ZS"], W=["XSB", "ldsm"])
            for gp in range(16):
                pb = 4 + (gp % 4)
                for j in range(2):
                    g = 2 * gp + j
                    o = j * 256
                    op("pe", lambda e, g=g, o=o, j=j: e.matmul(ps[pb][:, o:o + 256], UT[:, g, 0, :],
                                                          MW[:, g, :, :].rearrange("p a m -> p (a m)"),
                                                          start=(j == 0), stop=False, skip_group_check=True),
                       R=["UT", "MW"], W=[("ps", pb)], sig=False)
                    op("pe", lambda e, g=g, o=o: e.matmul(ps[pb][:, o + 128:o + 256], UT[:, g, 1, :], MW[:, g, 0, :],
                                                     start=False, stop=False, skip_group_check=True),
                       R=["UT", "MW"], W=[("ps", pb)], sig=False)
                    op("pe", lambda e, g=g, o=o: e.matmul(ps[pb][:, o:o + 256], XSB[:, g, :],
                                                     BZCL[:, g, :, :].rearrange("p a m -> p (a m)"),
                                                     start=False, stop=(j == 1), skip_group_check=True),
                       R=["XSB", "BZCL"], W=[("ps", pb)], sig=(j == 1))
                dst = UCM[:, :, 32 * gp:32 * gp + 32].rearrange("p i (g h) -> p g i h", g=2)
                op("act", lambda e: e.activation(out=dst, in_=ps[pb][:].rearrange("p (g i h) -> p g i h", g=2, i=16),
                                                 func=AF.Gelu_apprx_tanh), R=[("ps", pb)], W=ALLU)

        def glu_phase(l):
            WGLU = TMP4[:].bitcast(BF16).rearrange("p (k n) -> p k n", k=4)
            kb.dma("pool", WGLU, w_glu[l].rearrange("(k p) n -> p k n", p=128), W=["TMP4"], dkey="wglu")
            ZSS = SS[:, 0:16]
            def tile(i):
                s2 = i % 2
                pb = s2
                YGT = SM[:, 2048 + 256 * s2:2048 + 256 * (s2 + 1)].bitcast(BF16)
                SG = SM[:, 1024 * s2:1024 * s2 + 512]
                ZF = SM[:, 1024 * s2 + 512:1024 * s2 + 1024]
                ZN = SG.bitcast(BF16)[:, 0:512]
                for fc in range(4):
                    op("pe", lambda e, fc=fc: e.transpose(psb(pb)[:, fc * 128:(fc + 1) * 128],
                                                          UCM[:, i, fc * 128:(fc + 1) * 128], ident_b[:]),
                       R=[("UCM", i), "ident_b"], W=[("ps", pb)], sig=(fc == 3))
                    yield
                op("act", lambda e: e.activation(out=YGT, in_=psb(pb)[:, 0:512], func=AF.Copy),
                   R=[("ps", pb)], W=[("YGT", s2)])
                yield
                pg = 2 + s2
                for fc in range(4):
                    op("pe", lambda e, fc=fc: e.matmul(ps[pg][:], YGT[:, fc * 128:(fc + 1) * 128], WGLU[:, fc, :],
                                                       start=(fc == 0), stop=(fc == 3)),
                       R=[("YGT", s2), "TMP4"], W=[("ps", pg)], sig=(fc == 3))
                    yield
                op("act", lambda e: e.activation(out=SG, in_=ps[pg][:], func=AF.Sigmoid), R=[("ps", pg)], W=[("SG", s2)])
                yield
                op("dve", lambda e: e.tensor_tensor(ZF, UCM[:, i, :], SG, op=ALU.mult),
                   R=[("UCM", i), ("SG", s2)], W=[("ZF", s2)])
                yield
                op("dve", lambda e: e.scalar_tensor_tensor(ZN, ZF, 1.0, ZF, op0=ALU.mult, op1=ALU.mult,
                                                           accum_out=ZSS[:, i:i + 1]),
                   R=[("ZF", s2), ("SG", s2)], W=[("SG", s2), ("ZSS", i)])
                yield
                op("dve", lambda e: e.tensor_scalar(SS[:, 16 + i:17 + i], ZSS[:, i:i + 1], 1.0 / 512.0, EPS,
                                                    op0=ALU.mult, op1=ALU.add), R=[("ZSS", i)], W=[("ZSb", i)])
                yield
                op("pool", lambda e: e.tensor_tensor(SS[:, 32 + i:33 + i], SS[:, 16 + i:17 + i], NHALF[:, 0:1],
                                                     op=ALU.pow), R=[("ZSb", i), "NHALF"], W=[("ZRS", i)])
                yield
                op("dve", lambda e: e.tensor_scalar(ZN, ZF, SS[:, 32 + i:33 + i], None, op0=ALU.mult),
                   R=[("ZF", s2), ("ZRS", i), ("SG", s2)], W=[("SG", s2)])
                yield
                pt = 4 + s2
                for fc in range(4):
                    op("pe", lambda e, fc=fc: e.transpose(psb(pt)[:, fc * 128:(fc + 1) * 128],
                                                          ZN[:, fc * 128:(fc + 1) * 128], ident_b[:]),
                       R=[("SG", s2), "ident_b"], W=[("ps", pt)], sig=(fc == 3))
                    yield
                op("act", lambda e: e.activation(out=FT3[:, 0:4, i::16],
                                                 in_=psb(pt)[:, 0:512].rearrange("p (k c) -> p k c", k=4),
                                                 func=AF.Copy), R=[("ps", pt)], W=[("ST", i)])
                yield

            run_rr([tile(i) for i in range(NT)], 2, 8)

        WOUT = big_bf(0, 4096).rearrange("p (k n) -> p k n", k=8)

        def wout_load(l):
            kb.dma("pool", WOUT, w_out[l].rearrange("(k p) n -> p k n", p=128), W=["WOUT"], dkey="wout")
            for k in range(8):
                op("dve", lambda e, k=k: e.scalar_tensor_tensor(WOUT[:, k, :], WOUT[:, k, :], GROW[:, k:k + 1],
                                                                MOD[:, 2, :], op0=ALU.mult, op1=ALU.mult),
                   R=["WOUT", "GROW", ("MOD", 2)], W=["WOUT"])

        def wout_phase(l):
            ALLAT = [("AT", n) for n in range(NT)]
            ALLST = [("ST", n) for n in range(NT)]
            cnt = 0
            for i in range(NT):
                for half in range(2):
                    pb = 4 + (cnt % 4)
                    cnt += 1
                    for k in range(8):
                        lhs = AT3[:, k, i::16] if k < 4 else FT3[:, k - 4, i::16]
                        op("pe", lambda e, k=k, lhs=lhs: e.matmul(ps[pb][:], lhs, WOUT[:, k, half * 512:(half + 1) * 512],
                                                                 start=(k == 0), stop=(k == 7)),
                           R=ALLAT + ALLST + ["WOUT"], W=[("ps", pb)], sig=(k == 7))
                    xs_ = X[:, i, half * 512:(half + 1) * 512]
                    op("dve", lambda e, xs_=xs_: e.tensor_tensor(xs_, xs_, ps[pb][:], op=ALU.add),
                       R=[("ps", pb), ("X", i)], W=[("X", i)])

        H2T3 = big_bf(4096, 12288).rearrange("p (k t) -> p k t", k=8)
        ACTT = big_bf(0, 4096).rearrange("p (k t) -> p k t", k=4)

        def ring(sl):
            if sl < 4:
                return FT[:, sl * 4096:(sl + 1) * 4096]
            return AT[:, (sl - 4) * 4096:(sl - 3) * 4096]

        def router(l):
            ALLH = [("H2", i) for i in range(NT)]
            for i in range(NT):
                for k in range(8):
                    op("pe", lambda e, k=k: e.matmul(ps[6][:, i * 16:(i + 1) * 16], H2T3[:, k, i * 128:(i + 1) * 128],
                                                     WR[:, k, :], start=(i == 0 and k == 0), stop=(i == NT - 1 and k == 7),
                                                     skip_group_check=True),
                       R=[("H2", i), "WR"], W=[("ps", 6)], sig=(i == NT - 1 and k == 7))
            v3 = lambda ap: ap.rearrange("p (i e) -> p i e", i=16)
            v4 = lambda ap: ap.rearrange("p (i g e) -> p i g e", i=16, g=4)
            LG = TMP4[:, 0:256]
            PR = TMP4[:, 256:512]
            SEL = TMP4[:, 512:768]
            SEL2 = TMP4[:, 768:1024]
            EQ1 = TMP4b[:, 0:256]
            EQ2 = TMP4b[:, 256:512]
            CMB = TMP4b[:, 512:768]
            CMBb = TMP4b[:, 768:896].bitcast(BF16)
            MX = SS[:, 0:16]
            SM_ = SS[:, 16:32]
            M1 = SM[:, 2048:2112]
            M2 = SM[:, 2112:2176]
            GS = SM[:, 2176:2240]
            GM = SS[:, 32:48]
            ING = SM[:, 2240:2304]
            b3 = lambda ap: ap.unsqueeze(2).to_broadcast([128, 16, 16])
            op("dve", lambda e: e.tensor_reduce(MX, v3(ps[6][:, 0:256]), axis=AX.X, op=ALU.max), R=[("ps", 6)], W=["r_mx"])
            op("dve", lambda e: e.tensor_tensor(v3(LG), v3(ps[6][:, 0:256]), b3(MX), op=ALU.subtract),
               R=[("ps", 6), "r_mx"], W=["TMP4"])
            op("act", lambda e: e.activation(out=LG, in_=LG, func=AF.Exp), R=["TMP4"], W=["TMP4"])
            op("dve", lambda e: e.tensor_reduce(SM_, v3(LG), axis=AX.X, op=ALU.add), R=["TMP4"], W=["r_sm"])
            op("dve", lambda e: e.reciprocal(SM_, SM_), R=["r_sm"], W=["r_sm"])
            op("dve", lambda e: e.tensor_tensor(v3(PR), v3(LG), b3(SM_), op=ALU.mult), R=["TMP4", "r_sm"], W=["TMP4"])
            op("dve", lambda e: e.tensor_tensor(v3(SEL), v3(PR), RB[:].unsqueeze(1).to_broadcast([128, 16, 16]),
                                                op=ALU.add), R=["TMP4", "RB"], W=["TMP4"])
            op("dve", lambda e: e.tensor_reduce(M1, SEL.rearrange("p (j e) -> p j e", e=4), axis=AX.X, op=ALU.max),
               R=["TMP4"], W=["r_m1"])
            b4 = lambda ap: ap.unsqueeze(2).to_broadcast([128, 64, 4])
            j4 = lambda ap: ap.rearrange("p (j e) -> p j e", e=4)
            op("dve", lambda e: e.tensor_tensor(j4(EQ1), j4(SEL), b4(M1), op=ALU.is_equal), R=["TMP4", "r_m1"], W=["TMP4b"])
            op("dve", lambda e: e.scalar_tensor_tensor(SEL2, EQ1, -1.0e9, SEL, op0=ALU.mult, op1=ALU.add),
               R=["TMP4b", "TMP4"], W=["TMP4"])
            op("dve", lambda e: e.tensor_reduce(M2, j4(SEL2), axis=AX.X, op=ALU.max), R=["TMP4"], W=["r_m2"])
            op("dve", lambda e: e.tensor_tensor(j4(EQ2), j4(SEL2), b4(M2), op=ALU.is_equal), R=["TMP4", "r_m2"], W=["TMP4b"])
            op("dve", lambda e: e.tensor_tensor(GS, M1, M2, op=ALU.add), R=["r_m1", "r_m2"], W=["r_gs"])
            op("dve", lambda e: e.tensor_reduce(GM, GS.rearrange("p (i g) -> p i g", g=4), axis=AX.X, op=ALU.max),
               R=["r_gs"], W=["r_gm"])
            op("dve", lambda e: e.tensor_tensor(ING.rearrange("p (i g) -> p i g", g=4), GS.rearrange("p (i g) -> p i g", g=4),
                                                GM.unsqueeze(2).to_broadcast([128, 16, 4]), op=ALU.is_equal),
               R=["r_gs", "r_gm"], W=["r_ing"])
            op("dve", lambda e: e.tensor_tensor(EQ1, EQ1, EQ2, op=ALU.add), R=["TMP4b"], W=["TMP4b"])
            op("dve", lambda e: e.tensor_tensor(j4(EQ1), j4(EQ1), b4(ING), op=ALU.mult), R=["TMP4b", "r_ing"], W=["TMP4b"])
            op("dve", lambda e: e.tensor_tensor(EQ1, EQ1, PR, op=ALU.mult), R=["TMP4b", "TMP4"], W=["TMP4b"])
            op("dve", lambda e: e.tensor_reduce(SM_, v3(EQ1), axis=AX.X, op=ALU.add), R=["TMP4b"], W=["r_sm"])
            op("dve", lambda e: e.reciprocal(SM_, SM_), R=["r_sm"], W=["r_sm"])
            op("dve", lambda e: e.tensor_tensor(v3(CMBb), v3(EQ1), b3(SM_), op=ALU.mult), R=["TMP4b", "r_sm"], W=["TMP4b"])
            if debug is not None and debug[0] == f"comb{l}":
                op("dve", lambda e: e.tensor_tensor(v3(CMB), v3(EQ1), b3(SM_), op=ALU.mult), R=["TMP4b", "r_sm"], W=["TMP4b"])
                dump(CMB, 256, F32, ["TMP4b"])
                return True
            CT = SM[0:16, 0:1024].bitcast(BF16)
            for i in range(NT):
                pb = 4 + (i // 8)
                op("pe", lambda e, i=i: e.transpose(psb(pb)[0:16, (i % 8) * 128:(i % 8 + 1) * 128],
                                                    CMBb[:, i * 16:(i + 1) * 16], ident_b[:]),
                   R=["TMP4b", "ident_b"], W=[("ps", pb)], sig=(i % 8 == 7))
            for hb in range(2):
                op("act", lambda e, hb=hb: e.activation(out=CT[:, hb * 1024:(hb + 1) * 1024], in_=psb(4 + hb)[0:16, :],
                                                        func=AF.Copy), R=[("ps", 4 + hb)], W=["CT"])
            return False

        def load_expert(l, e_):
            base = 3 * (e_ % 2)
            kb.dma("pool", ring(base).rearrange("p (k n) -> p k n", k=8),
                   w_exp_gate[l, e_].rearrange("(k p) n -> p k n", p=128), W=[("ring", base)], dkey=f"ring{base}")
            kb.dma("pool", ring(base + 1).rearrange("p (k n) -> p k n", k=8),
                   w_exp_up[l, e_].rearrange("(k p) n -> p k n", p=128), W=[("ring", base + 1)], dkey=f"ring{base + 1}")
            wd = ring(base + 2).rearrange("p (k n) -> p k n", k=4)
            kb.dma("pool", wd, w_exp_down[l, e_].rearrange("(k p) n -> p k n", p=128),
                   W=[("ring", base + 2)], dkey=f"ring{base + 2}")


        def moe_phase(l):
            CT = SM[0:16, 0:1024].bitcast(BF16)

            def scale_wd(e_):
                base = 3 * (e_ % 2)
                wd = ring(base + 2).rearrange("p (k n) -> p k n", k=4)
                op("pool", lambda e: e.tensor_tensor(wd, wd, MOD[:, 5, :].unsqueeze(1).to_broadcast([128, 4, 1024]),
                                                     op=ALU.mult), R=[("ring", base + 2), ("MOD", 5)], W=[("ring", base + 2)])

            def prep_piece(e2, tb):
                p2 = e2 % 2
                CB2 = (TMP4 if p2 == 0 else TMP4b)[:].bitcast(BF16)
                ck2 = "TMP4" if p2 == 0 else "TMP4b"
                if tb == 0:
                    op("dve", lambda e: e.tensor_copy(SELE[:, p2, :], ident_b[0:16, e2:e2 + 1].to_broadcast([16, 128])),
                       R=["ident_b"], W=[("SELE", p2)])
                op("pe", lambda e: e.matmul(ps[6][:], SELE[:, p2, :], CT[:, tb * 512:(tb + 1) * 512],
                                            start=True, stop=True), R=[("SELE", p2), "CT"], W=[("ps", 6)])
                op("act", lambda e: e.activation(out=CB2[:, tb * 512:(tb + 1) * 512], in_=ps[6][:], func=AF.Copy),
                   R=[("ps", 6)], W=[(ck2, tb)])

            gcnt = 0
            dcnt = 0
            for e_ in range(16):
                if e_ + 1 < 16:
                    load_expert(l, e_ + 1)
                base = 3 * (e_ % 2)
                WG = ring(base).rearrange("p (k n) -> p k n", k=8)
                WU = ring(base + 1).rearrange("p (k n) -> p k n", k=8)
                WD = ring(base + 2).rearrange("p (k n) -> p k n", k=4)
                es_ = e_ % 2
                CBC = (TMP4 if es_ == 0 else TMP4b)[:].bitcast(BF16)
                ckey = "TMP4" if es_ == 0 else "TMP4b"
                if e_ == 0:
                    for tb in range(4):
                        prep_piece(0, tb)
                for fc in range(4):
                    if fc == 2:
                        scale_wd(e_)
                    for tb in range(4):
                        g2 = gcnt % 2
                        gcnt += 1
                        pg, pu = g2, 2 + g2
                        hk = [("H2", i) for i in range(4 * tb, 4 * tb + 4)]
                        for k in range(8):
                            op("pe", lambda e, k=k: e.matmul(ps[pg][:], WG[:, k, fc * 128:(fc + 1) * 128],
                                                             H2T3[:, k, tb * 512:(tb + 1) * 512],
                                                             start=(k == 0), stop=(k == 7)),
                               R=[("ring", base)] + hk, W=[("ps", pg)], sig=(k == 7))
                        for k in range(8):
                            op("pe", lambda e, k=k: e.matmul(ps[pu][:], WU[:, k, fc * 128:(fc + 1) * 128],
                                                             H2T3[:, k, tb * 512:(tb + 1) * 512],
                                                             start=(k == 0), stop=(k == 7)),
                               R=[("ring", base + 1)] + hk, W=[("ps", pu)], sig=(k == 7))
                        SGt = SM[:, 1024 + 256 * g2:1280 + 256 * g2].bitcast(BF16)
                        Tt = SM[:, 1536 + 256 * g2:1792 + 256 * g2].bitcast(BF16)
                        op("act", lambda e: e.activation(out=SGt, in_=ps[pg][:], func=AF.Silu),
                           R=[("ps", pg)], W=[("SGt", g2)])
                        op("dve", lambda e: e.tensor_tensor(Tt, SGt, ps[pu][:], op=ALU.mult),
                           R=[("SGt", g2), ("ps", pu)], W=[("Tt", g2)])
                        op("pool", lambda e: e.tensor_tensor(ACTT[:, fc, tb * 512:(tb + 1) * 512], Tt,
                                                             CBC[:, tb * 512:(tb + 1) * 512], op=ALU.mult),
                           R=[("Tt", g2), (ckey, tb)], W=[("ACTT", fc, tb)])
                    if e_ + 1 < 16:
                        prep_piece(e_ + 1, fc)
                for i in range(NT):
                    for half in range(2):
                        pd = (4, 5, 7)[dcnt % 3]
                        dcnt += 1
                        for fc in range(4):
                            op("pe", lambda e, fc=fc: e.matmul(ps[pd][:], ACTT[:, fc, i * 128:(i + 1) * 128],
                                                               WD[:, fc, half * 512:(half + 1) * 512],
                                                               start=(fc == 0), stop=(fc == 3)),
                               R=[("ACTT", fc, i // 4), ("ring", base + 2)], W=[("ps", pd)], sig=(fc == 3))
                        xs_ = X[:, i, half * 512:(half + 1) * 512]
                        op("dve", lambda e, xs_=xs_: e.tensor_tensor(xs_, xs_, ps[pd][:], op=ALU.add),
                           R=[("ps", pd), ("X", i, half)], W=[("X", i, half)])

        def dump(ap, shape2d_cols, dt, keys):
            kb.dma("sp", dbg_d[:, 0:shape2d_cols], ap, R=keys, dkey="dbg")

        for l in range(depth):
            norm_to_FT(l, 0, None, part="stats")
            adaln(l)
            if debug is not None and debug[0] == f"mod{l}":
                dump(MOD[:].rearrange("p a n -> p (a n)"), 6 * D, BF16, [("MOD", j) for j in range(6)])
                break
            kb.barrier()
            layer_loads(l)
            norm_to_FT(l, 0, lambda i: FT3[:, :, i::16], part="tiles")
            if debug is not None and debug[0] == f"hT{l}":
                dump(FT[:], 8 * S, BF16, [("FT", i) for i in range(NT)])
                break
            u_proj(l)
            if debug is not None and debug[0] == f"ucm{l}":
                dump(UCM.rearrange("p j f -> p (j f)"), 16 * 512, BF16, [("UCM", j) for j in range(NT)])
                break
            kb.barrier()
            attention(l)
            if debug is not None and debug[0] == f"at{l}":
                dump(AT[:], 4 * S, BF16, [("AT", n) for n in range(NT)])
                break
            kb.barrier()
            if ssm_main(l):
                break
            if debug is not None and debug[0] == f"yg{l}":
                dump(UCM.rearrange("p j f -> p (j f)"), 16 * 512, BF16, [("UCM", j) for j in range(NT)])
                break
            kb.barrier()
            wout_load(l)
            glu_phase(l)
            if debug is not None and debug[0] == f"st{l}":
                dump(FT[:, 0:4 * S], 4 * S, BF16, [("ST", i) for i in range(NT)])
                break
            kb.barrier()
            wout_phase(l)
            if debug is not None and debug[0] == f"x1{l}":
                kb.barrier()
                dump(X[:].rearrange("p i d -> p (i d)"), NT * D, F32, [("X", i) for i in range(NT)])
                break
            kb.barrier()
            load_expert(l, 0)
            norm_to_FT(l, 1, lambda i: H2T3[:, :, i * 128:(i + 1) * 128], okey="H2", pbanks=(0, 1))
            if debug is not None and debug[0] == f"h2{l}":
                kb.barrier()
                dump(BIG[:, 4096:12288].bitcast(BF16), 8 * S, BF16, [("H2", i) for i in range(NT)])
                break
            kb.barrier()
            if router(l):
                break
            kb.barrier()
            moe_phase(l)
            kb.barrier()
            if debug is not None and debug[0] == f"x2{l}":
                dump(X[:].rearrange("p i d -> p (i d)"), NT * D, F32, [("X", i) for i in range(NT)])
                break

        if debug is None:
            y_v = y_d.rearrange("(c i) d -> c i d", i=NT)
            for q4 in range(4):
                kb.dma("sp", y_v[:, 4 * q4:4 * q4 + 4, :], X[:, 4 * q4:4 * q4 + 4, :],
                       R=[("X", i) for i in range(4 * q4, 4 * q4 + 4)], dkey=f"y{q4}")
        kb.final_wait("sp")
    return nc


_IN_NAMES = ["ada_w", "ada_b", "norm1_g", "w_in", "q_norm_g", "k_norm_g", "attn_sink", "lam_re", "lam_im",
             "ssm_b_re", "ssm_b_im", "ssm_c_re", "ssm_c_im", "ssm_d", "ssm_log_dt", "w_glu", "attn_out_g",
             "ssm_out_g", "w_out", "norm2_g", "w_router", "router_bias", "w_exp_gate", "w_exp_up", "w_exp_down"]


def make_in_maps(inputs, cores):
    maps = []
    shared = {k: np.ascontiguousarray(np.asarray(inputs[k], dtype=np.float32)) for k in _IN_NAMES}
    for b in cores:
        m = dict(shared)
        m["x"] = np.ascontiguousarray(np.asarray(inputs["x"][b], dtype=np.float32))
        m["c"] = np.ascontiguousarray(np.asarray(inputs["c"][b], dtype=np.float32).reshape(8, 128))
        m["positions"] = np.ascontiguousarray(np.asarray(inputs["positions"][b], dtype=np.int32))
        maps.append(m)
    return maps


def kernel(**inputs):
    nc = build()
    in_maps = make_in_maps(inputs, list(range(8)))
    res = run_bass_kernel_spmd(nc, in_maps, core_ids=list(range(8)))
    return np.stack([np.asarray(r["y"], dtype=np.float32) for r in res.results], axis=0)
```
